# Optimizing a Trainium2 kernel written in Bass

```python
import jax, jax.numpy as jnp
from jax import lax
import numpy as np

D_MODEL = 1024
BATCH = 16
SEQ = 4096
DEPTH = 4

N_MIXERS = 2
CHUNK = 64
EPS = 1e-6
D_FF = 2816

GLA_HEADS = 4
GLA_DK = 128
GLA_DV = 256
GLA_KEY = GLA_HEADS * GLA_DK
GLA_VAL = GLA_HEADS * GLA_DV
GLA_GATE_RANK = 16
GLA_GATE_NORM = 16.0
GLA_IN = 2 * GLA_KEY + 2 * GLA_VAL + GLA_GATE_RANK

GDN_HEADS = 8
GDN_DK = 128
GDN_DV = 128
GDN_KEY = GDN_HEADS * GDN_DK
GDN_VAL = GDN_HEADS * GDN_DV
CONV_K = 4
GDN_CONV_CH = 2 * GDN_KEY + GDN_VAL
GDN_IN = GDN_CONV_CH + GDN_VAL + 2 * GDN_HEADS

N_GLA = (DEPTH + N_MIXERS - 1) // N_MIXERS
N_GDN = DEPTH // N_MIXERS

kernel_name = "hybrid_gla_gated_deltanet_macaron"

F32 = jnp.float32


def rmsnorm(x, w):
    xf = x.astype(F32)
    y = xf * lax.rsqrt(jnp.mean(xf * xf, axis=-1, keepdims=True) + EPS)
    return (y * w.astype(F32)).astype(x.dtype)


def swiglu(h, w_in, w_out):
    g, u = jnp.split(h @ w_in, 2, axis=-1)
    return (jax.nn.silu(g) * u) @ w_out


def to_chunks(t, n_heads):
    b, s, hd = t.shape
    return t.reshape(b, s // CHUNK, CHUNK, n_heads, hd // n_heads).transpose(0, 3, 1, 2, 4)


def heads_to_chunks(t):
    b, s, h = t.shape
    return t.reshape(b, s // CHUNK, CHUNK, h).transpose(0, 3, 1, 2)


def from_chunks(t):
    b, h, n, c, d = t.shape
    return t.transpose(0, 2, 3, 1, 4).reshape(b, n * c, h * d)


def head_norm_gate(o, gate, w):
    o = o * lax.rsqrt(jnp.mean(o * o, axis=-1, keepdims=True) + EPS) * w.astype(F32)
    return from_chunks(o).astype(gate.dtype) * jax.nn.silu(gate)


def l2norm(t):
    return t * lax.rsqrt(jnp.sum(t * t, axis=-1, keepdims=True) + EPS)


def causal_conv(x, w):
    c = x.shape[-1]
    return lax.conv_general_dilated(
        x, w[:, None, :].astype(x.dtype), window_strides=(1,), padding=[(CONV_K - 1, 0)],
        dimension_numbers=("NWC", "WIO", "NWC"), feature_group_count=c)


def gla_chunked(q, k, v, g):
    scale = q.shape[-1] ** -0.5
    b = jnp.cumsum(g, axis=3)
    b_last = b[:, :, :, -1:, :]
    q_dec = q * jnp.exp(b) * scale
    k_inv = k * jnp.exp(-b)
    k_end = k * jnp.exp(b_last - b)
    causal = jnp.tril(jnp.ones((CHUNK, CHUNK), dtype=bool))
    a = jnp.where(causal, jnp.einsum("bhnid,bhnjd->bhnij", q_dec, k_inv), 0.0)
    o_intra = jnp.einsum("bhnij,bhnjv->bhniv", a, v)

    def step(state, xs):
        qd, ke, vv, dl = xs
        o = jnp.einsum("bhcd,bhdv->bhcv", qd, state)
        state = dl[..., None] * state + jnp.einsum("bhcd,bhcv->bhdv", ke, vv)
        return state, o

    s0 = jnp.zeros(q.shape[:2] + (q.shape[-1], v.shape[-1]), q.dtype)
    xs = tuple(jnp.moveaxis(t, 2, 0) for t in (q_dec, k_end, v, jnp.exp(b_last[:, :, :, 0])))
    _, o_inter = lax.scan(step, s0, xs)
    return o_intra + jnp.moveaxis(o_inter, 0, 2)


def gdn_chunked(q, k, v, beta, g):
    dv = v.shape[-1]
    q = q * (q.shape[-1] ** -0.5)
    d = jnp.cumsum(g, axis=-1)
    d_last = d[..., -1]
    causal = jnp.tril(jnp.ones((CHUNK, CHUNK), dtype=bool))
    strict = jnp.tril(jnp.ones((CHUNK, CHUNK), dtype=bool), k=-1)
    decay = jnp.where(causal, jnp.exp(jnp.minimum(d[..., :, None] - d[..., None, :], 0.0)), 0.0)
    kb = k * beta[..., None]
    m = jnp.where(strict, jnp.einsum("bhnid,bhnjd->bhnij", kb, k) * decay, 0.0)
    a = m + jnp.eye(CHUNK, dtype=m.dtype)
    rhs = jnp.concatenate([v * beta[..., None], kb * jnp.exp(d)[..., None]], axis=-1)
    sol = lax.linalg.triangular_solve(a, rhs, left_side=True, lower=True, unit_diagonal=True)
    u, w = sol[..., :dv], sol[..., dv:]
    qk = jnp.einsum("bhnid,bhnjd->bhnij", q, k) * decay
    q_dec = q * jnp.exp(d)[..., None]
    k_end = k * jnp.exp(d_last[..., None] - d)[..., None]

    def step(state, xs):
        qk_c, qd, ke, u_c, w_c, dl = xs
        v_new = u_c - jnp.einsum("bhcd,bhdv->bhcv", w_c, state)
        o = jnp.einsum("bhcd,bhdv->bhcv", qd, state) + jnp.einsum("bhij,bhjv->bhiv", qk_c, v_new)
        state = dl[..., None, None] * state + jnp.einsum("bhcd,bhcv->bhdv", ke, v_new)
        return state, o

    s0 = jnp.zeros(q.shape[:2] + (q.shape[-1], dv), q.dtype)
    xs = tuple(jnp.moveaxis(t, 2, 0) for t in (qk, q_dec, k_end, u, w, jnp.exp(d_last)))
    _, o = lax.scan(step, s0, xs)
    return jnp.moveaxis(o, 0, 2)


def gla_mixer(h, w_in, w_gk, b_gk, w_norm, w_out):
    p = h @ w_in
    q, k, v, r, z = jnp.split(
        p, [GLA_KEY, 2 * GLA_KEY, 2 * GLA_KEY + GLA_VAL, 2 * GLA_KEY + 2 * GLA_VAL], axis=-1)
    gk = jax.nn.log_sigmoid((z @ w_gk + b_gk).astype(F32)) / GLA_GATE_NORM
    o = gla_chunked(to_chunks(q.astype(F32), GLA_HEADS), to_chunks(k.astype(F32), GLA_HEADS),
                    to_chunks(v.astype(F32), GLA_HEADS), to_chunks(gk, GLA_HEADS))
    return head_norm_gate(o, r, w_norm) @ w_out


def gdn_mixer(h, w_in, conv_w, a_log, dt_bias, w_norm, w_out):
    p = h @ w_in
    qkv, r, ab = jnp.split(p, [GDN_CONV_CH, GDN_CONV_CH + GDN_VAL], axis=-1)
    qkv = jax.nn.silu(causal_conv(qkv, conv_w))
    q, k, v = jnp.split(qkv.astype(F32), [GDN_KEY, 2 * GDN_KEY], axis=-1)
    a, bl = jnp.split(ab.astype(F32), 2, axis=-1)
    beta = jax.nn.sigmoid(bl)
    g = -jnp.exp(a_log.astype(F32)) * jax.nn.softplus(a + dt_bias.astype(F32))
    o = gdn_chunked(l2norm(to_chunks(q, GDN_HEADS)), l2norm(to_chunks(k, GDN_HEADS)),
                    to_chunks(v, GDN_HEADS), heads_to_chunks(beta), heads_to_chunks(g))
    return head_norm_gate(o, r, w_norm) @ w_out


def setup_inputs(seed: int = 0) -> dict:
    key = jax.random.key(seed)
    ks = jax.random.split(key, 20)
    nrm = lambda k, shape, fan_in: jax.random.normal(k, shape, F32) * (fan_in ** -0.5)
    x = jax.random.normal(ks[0], (BATCH, SEQ, D_MODEL), F32)
    norm_w = 1.0 + 0.02 * jax.random.normal(ks[1], (DEPTH, 3, D_MODEL), F32)
    ffn_w_in = nrm(ks[2], (DEPTH, 2, D_MODEL, 2 * D_FF), D_MODEL)
    ffn_w_out = nrm(ks[3], (DEPTH, 2, D_FF, D_MODEL), D_FF)
    gla_w_in = nrm(ks[4], (N_GLA, D_MODEL, GLA_IN), D_MODEL)
    gla_w_gk = nrm(ks[5], (N_GLA, GLA_GATE_RANK, GLA_KEY), GLA_GATE_RANK)
    gla_b_gk = 0.1 * jax.random.normal(ks[6], (N_GLA, GLA_KEY), F32)
    gla_norm_w = 1.0 + 0.02 * jax.random.normal(ks[7], (N_GLA, GLA_DV), F32)
    gla_w_out = nrm(ks[8], (N_GLA, GLA_VAL, D_MODEL), GLA_VAL)
    gdn_w_in = nrm(ks[9], (N_GDN, D_MODEL, GDN_IN), D_MODEL)
    gdn_conv_w = nrm(ks[10], (N_GDN, CONV_K, GDN_CONV_CH), CONV_K)
    gdn_a_log = jnp.log(jax.random.uniform(ks[11], (N_GDN, GDN_HEADS), F32, minval=1.0, maxval=16.0))
    dt = jnp.exp(jax.random.uniform(ks[12], (N_GDN, GDN_HEADS), F32,
                                    minval=float(np.log(1e-3)), maxval=float(np.log(1e-1))))
    gdn_dt_bias = jnp.log(jnp.expm1(dt))
    gdn_norm_w = 1.0 + 0.02 * jax.random.normal(ks[13], (N_GDN, GDN_DV), F32)
    gdn_w_out = nrm(ks[14], (N_GDN, GDN_VAL, D_MODEL), GDN_VAL)
    final_norm_w = 1.0 + 0.02 * jax.random.normal(ks[15], (D_MODEL,), F32)
    return {"x": x, "norm_w": norm_w, "ffn_w_in": ffn_w_in, "ffn_w_out": ffn_w_out,
            "gla_w_in": gla_w_in, "gla_w_gk": gla_w_gk, "gla_b_gk": gla_b_gk,
            "gla_norm_w": gla_norm_w, "gla_w_out": gla_w_out,
            "gdn_w_in": gdn_w_in, "gdn_conv_w": gdn_conv_w, "gdn_a_log": gdn_a_log,
            "gdn_dt_bias": gdn_dt_bias, "gdn_norm_w": gdn_norm_w, "gdn_w_out": gdn_w_out,
            "final_norm_w": final_norm_w}


def reference(x, norm_w, ffn_w_in, ffn_w_out, gla_w_in, gla_w_gk, gla_b_gk, gla_norm_w, gla_w_out,
              gdn_w_in, gdn_conv_w, gdn_a_log, gdn_dt_bias, gdn_norm_w, gdn_w_out, final_norm_w):
    for i in range(DEPTH):
        x = x + 0.5 * swiglu(rmsnorm(x, norm_w[i, 0]), ffn_w_in[i, 0], ffn_w_out[i, 0])
        h = rmsnorm(x, norm_w[i, 1])
        j = i // N_MIXERS
        if i % N_MIXERS == 0:
            x = x + gla_mixer(h, gla_w_in[j], gla_w_gk[j], gla_b_gk[j], gla_norm_w[j], gla_w_out[j])
        else:
            x = x + gdn_mixer(h, gdn_w_in[j], gdn_conv_w[j], gdn_a_log[j], gdn_dt_bias[j],
                              gdn_norm_w[j], gdn_w_out[j])
        x = x + 0.5 * swiglu(rmsnorm(x, norm_w[i, 2]), ffn_w_in[i, 1], ffn_w_out[i, 1])
    return rmsnorm(x, final_norm_w)
```

```python
import contextlib
import numpy as np
import concourse.bass as bass
import concourse.mybir as mybir
from concourse.bass_utils import run_bass_kernel_spmd

F32 = mybir.dt.float32
BF16 = mybir.dt.bfloat16
AF = mybir.ActivationFunctionType
ALU = mybir.AluOpType

D = 1024
SEQ = 4096
T = 512
EPS = 1e-6
DFF = 2816
ENGS = ("pe", "act", "dve", "pool", "sp")
EPOCH = 30000


class Res:
    __slots__ = ("name", "last_w", "readers", "parent", "strict")

    def __init__(self, name, parent=None, strict=False):
        self.name = name
        self.last_w = None
        self.readers = []
        self.parent = parent
        self.strict = strict


class Op:
    __slots__ = ("eng", "fn", "waits", "signal", "is_dma", "key", "sig")

    def __init__(self, eng, fn, is_dma=False, key=None):
        self.eng = eng
        self.fn = fn
        self.waits = []
        self.signal = False
        self.is_dma = is_dma
        self.key = key
        self.sig = None


class Prog:
    def __init__(self, nc):
        self.nc = nc
        self.ops = {e: [] for e in ENGS}
        self.all_ops = []
        self.dma_keys = {}

    def _dep(self, op, prod, strict=False):
        if prod is None or prod is op:
            return
        if prod.eng == op.eng and not prod.is_dma and not strict:
            return
        op.waits.append(prod)
        prod.signal = True

    def _track(self, op, reads, writes):
        extra = [r.parent for r in list(reads) + list(writes) if r.parent is not None]
        if extra:
            reads = list(reads) + [p for p in extra if p not in reads and p not in writes]
        for r in reads:
            self._dep(op, r.last_w, r.strict)
        for w in writes:
            self._dep(op, w.last_w, w.strict)
            for rd in w.readers:
                self._dep(op, rd)
        for r in reads:
            r.readers.append(op)
        for w in writes:
            w.last_w = op
            w.readers = []

    def op(self, eng, fn, reads=(), writes=()):
        o = Op(eng, fn)
        self._track(o, reads, writes)
        self.ops[eng].append(o)
        self.all_ops.append(o)
        return o

    def dma(self, eng, fn, reads=(), writes=(), key="d"):
        o = Op(eng, fn, is_dma=True, key=key)
        o.signal = True
        self._track(o, reads, writes)
        self.ops[eng].append(o)
        self.all_ops.append(o)
        self.dma_keys.setdefault(key, 0)
        return o

    def emit(self):
        nc = self.nc
        cnt = {e: 0 for e in ENGS}
        dcnt = {k: 0 for k in self.dma_keys}
        for o in self.all_ops:
            if o.is_dma:
                dcnt[o.key] += 16
                o.sig = ("dma", o.key, dcnt[o.key])
            elif o.signal:
                cnt[o.eng] += 1
                ep, v = divmod(cnt[o.eng] - 1, EPOCH)
                o.sig = ("eng", (o.eng, ep), v + 1)
        sem_names = []
        for e in ENGS:
            for ep in range(max(1, (cnt[e] + EPOCH - 1) // EPOCH)):
                sem_names.append(("eng", (e, ep)))
        for k in self.dma_keys:
            sem_names.append(("dma", k))
        with contextlib.ExitStack() as st:
            sems = {}
            for i, sn in enumerate(sem_names):
                sems[sn] = st.enter_context(nc.semaphore("s%d" % i))
            block = st.enter_context(nc.Block())
            prog = self

            def make(ename):
                def body(eng):
                    waited = {}
                    for o in prog.ops[ename]:
                        need = {}
                        for p in o.waits:
                            kind, k, v = p.sig
                            sn = (kind, k)
                            if need.get(sn, 0) < v:
                                need[sn] = v
                        for sn, v in need.items():
                            if waited.get(sn, 0) >= v:
                                continue
                            eng.wait_ge(sems[sn], v)
                            waited[sn] = v
                        ins = o.fn(eng)
                        if o.is_dma:
                            ins.then_inc(sems[("dma", o.key)], 16)
                        elif o.signal:
                            ins.then_inc(sems[(o.sig[0], o.sig[1])], 1)
                    if ename == "sp":
                        for k, tot in dcnt.items():
                            if tot > 0 and waited.get(("dma", k), 0) < tot:
                                eng.wait_ge(sems[("dma", k)], tot)
                return body

            block.tensor(make("pe"))
            block.scalar(make("act"))
            block.vector(make("dve"))
            block.gpsimd(make("pool"))
            block.sync(make("sp"))


C_ID, C_LI, C_US, C_LS, C_CE, C_CO, C_ONE = 0, 128, 256, 384, 512, 640, 768
NCF = 896
B_ID, B_LI, B_LS4, B_TRB, B_TR2, B_O1024, B_O256, B_O1, B_LI4 = 0, 128, 256, 768, 896, 1024, 1152, 1280, 1408
NCB = 1920

S_NORM = 0
S_FNORM = 96
S_GLAN = 104
S_CONV = 108
S_ALOG = 300
S_DTB = 316
S_GDNN = 332
S_WZ = 588
S_WAB = 844
S_WGK = 1100
S_BGK = 2124
NSM = 3148


def _consts():
    cf = np.zeros((128, NCF), np.float32)
    m = np.arange(128)
    same = (m[:, None] // 64) == (m[None, :] // 64)
    li = ((m[:, None] <= m[None, :]) & same).astype(np.float32)
    ls = ((m[:, None] < m[None, :]) & same).astype(np.float32)
    us = ((m[:, None] > m[None, :]) & same).astype(np.float32)
    cf[:, C_ID:C_ID + 128] = np.eye(128)
    cf[:, C_LI:C_LI + 128] = li
    cf[:, C_US:C_US + 128] = us
    cf[:, C_LS:C_LS + 128] = ls
    cf[:, C_CE:C_CE + 128] = (m[:, None] < 64).astype(np.float32) * np.ones((1, 128), np.float32)
    cf[:, C_CO:C_CO + 128] = (m[:, None] >= 64).astype(np.float32) * np.ones((1, 128), np.float32)
    cf[:, C_ONE:C_ONE + 128] = 1.0
    cb = np.zeros((128, NCB), np.float32)
    cb[:, B_ID:B_ID + 128] = np.eye(128)
    cb[:, B_LI:B_LI + 128] = li
    cb[:, B_LS4:B_LS4 + 512] = np.tile(ls, (1, 4))
    cb[:, B_TRB:B_TRB + 128] = -li / 16.0
    cb[:, B_TR2:B_TR2 + 128] = -us / 16.0
    cb[:, B_O1024:B_O1024 + 128] = 1.0 / 1024.0
    cb[:, B_O256:B_O256 + 128] = 1.0 / 256.0
    cb[:, B_O1:B_O1 + 128] = 1.0
    cb[:, B_LI4:B_LI4 + 512] = np.tile(li, (1, 4))
    return cf, cb


def _kblocks(W, col_lists):
    out = []
    for cols in col_lists:
        sub = W[:, cols]
        out.append(sub.reshape(8, 128, 512).transpose(1, 0, 2).reshape(128, 4096))
    return out


_SWAP = [False]


def _layer_is_gla(l):
    return (l % 2 == 0) != _SWAP[0]


def _prep_weights(inp, depth):
    wk = []
    ar = np.arange
    for l in range(depth):
        j = l // 2
        for which in range(2):
            if which == 1:
                pass
        def ffn_blocks(i):
            W = inp["ffn_w_in"][l, i]
            lists = [np.concatenate([ar(b * 256, (b + 1) * 256), DFF + ar(b * 256, (b + 1) * 256)]) for b in range(11)]
            wk.extend(_kblocks(W, lists))
            Wo = inp["ffn_w_out"][l, i]
            for half in range(2):
                for g in range(3):
                    nf = 8 if g < 2 else 6
                    sub = Wo[g * 1024:g * 1024 + nf * 128, half * 512:(half + 1) * 512]
                    blk = np.zeros((128, 8, 512), np.float32)
                    blk[:, 0:nf, :] = sub.reshape(nf, 128, 512).transpose(1, 0, 2)
                    wk.append(blk.reshape(128, 4096))
        ffn_blocks(0)
        if _layer_is_gla(l):
            W = inp["gla_w_in"][j]
            lists = [ar(512, 1024), ar(0, 512), ar(1024, 1536), ar(1536, 2048), ar(2048, 2560), ar(2560, 3072)]
            wk.extend(_kblocks(W, lists))
            wk.extend(_kblocks(inp["gla_w_out"][j], [ar(0, 512), ar(512, 1024)]))
        else:
            W = inp["gdn_w_in"][j]
            lists = [ar(b * 512, (b + 1) * 512) for b in range(8)]
            wk.extend(_kblocks(W, lists))
            wk.extend(_kblocks(inp["gdn_w_out"][j], [ar(0, 512), ar(512, 1024)]))
        ffn_blocks(1)
    sm = np.zeros((128, NSM), np.float32)
    sm[:, S_NORM:S_NORM + 96] = inp["norm_w"].reshape(12, 8, 128).transpose(2, 0, 1).reshape(128, 96)
    sm[:, S_FNORM:S_FNORM + 8] = inp["final_norm_w"].reshape(8, 128).T
    sm[:, S_GLAN:S_GLAN + 4] = inp["gla_norm_w"].reshape(2, 2, 128).transpose(2, 0, 1).reshape(128, 4)
    sm[:, S_CONV:S_CONV + 192] = inp["gdn_conv_w"].reshape(2, 4, 24, 128).transpose(3, 0, 1, 2).reshape(128, 192)
    sm[:, S_ALOG:S_ALOG + 16] = inp["gdn_a_log"].reshape(1, 16)
    sm[:, S_DTB:S_DTB + 16] = inp["gdn_dt_bias"].reshape(1, 16)
    sm[:, S_GDNN:S_GDNN + 256] = inp["gdn_norm_w"].reshape(1, 256)
    for j in range(2):
        wz = inp["gla_w_in"][j][:, 3072:3088]
        sm[:, S_WZ + j * 128:S_WZ + (j + 1) * 128] = wz.reshape(8, 128, 16).transpose(1, 0, 2).reshape(128, 128)
        wab = inp["gdn_w_in"][j][:, 4096:4112]
        sm[:, S_WAB + j * 128:S_WAB + (j + 1) * 128] = wab.reshape(8, 128, 16).transpose(1, 0, 2).reshape(128, 128)
        sm[0:16, S_WGK + j * 512:S_WGK + (j + 1) * 512] = inp["gla_w_gk"][j]
        sm[0:1, S_BGK + j * 512:S_BGK + (j + 1) * 512] = inp["gla_b_gk"][j][None, :]
    return np.ascontiguousarray(np.stack(wk)), sm


class Builder:
    def __init__(self, depth=4, nseq=2, ntile=8, stop_after=None):
        self.depth, self.nseq, self.ntile = depth, nseq, ntile
        self.stop_after = stop_after
        self.nblk_layer = [42 if _layer_is_gla(l) else 44 for l in range(depth)]
        self.NB = sum(self.nblk_layer)

    def mm(self, out, lhsT, rhs, start=True, stop=True, r=(), w=()):
        self.P.op("pe", lambda e: e.matmul(out, lhsT, rhs, start=start, stop=stop, skip_group_check=True), reads=r, writes=w)

    def tr(self, out, in_, ident, r=(), w=()):
        self.P.op("pe", lambda e: e.transpose(out, in_, ident), reads=r, writes=w)

    def act(self, out, in_, func, r=(), w=(), bias=None, scale=None, accum=None, eng="act"):
        kw = {}
        if bias is not None:
            kw["bias"] = bias
        if scale is not None:
            kw["scale"] = scale
        if accum is not None:
            kw["accum_out"] = accum
        self.P.op("act", lambda e: e.activation(out=out, in_=in_, func=func, **kw), reads=r, writes=w)

    def tt(self, eng, out, in0, in1, op, r=(), w=()):
        self.P.op(eng, lambda e: e.tensor_tensor(out=out, in0=in0, in1=in1, op=op), reads=r, writes=w)

    def ts(self, eng, out, in0, s1, s2, op0, op1=None, r=(), w=()):
        if op1 is None:
            self.P.op(eng, lambda e: e.tensor_scalar(out=out, in0=in0, scalar1=s1, scalar2=None, op0=op0), reads=r, writes=w)
        else:
            self.P.op(eng, lambda e: e.tensor_scalar(out=out, in0=in0, scalar1=s1, scalar2=s2, op0=op0, op1=op1), reads=r, writes=w)

    def stt(self, eng, out, in0, scalar, in1, op0, op1, r=(), w=()):
        self.P.op(eng, lambda e: e.scalar_tensor_tensor(out=out, in0=in0, scalar=scalar, in1=in1, op0=op0, op1=op1), reads=r, writes=w)

    def cp(self, eng, out, in_, r=(), w=()):
        if eng == "act":
            self.P.op("act", lambda e: e.activation(out=out, in_=in_, func=AF.Copy), reads=r, writes=w)
        else:
            self.P.op(eng, lambda e: e.tensor_copy(out=out, in_=in_), reads=r, writes=w)

    def memset(self, eng, ap, val, w=()):
        self.P.op(eng, lambda e: e.memset(ap, val), writes=w)

    def bank(self, pool=None):
        pool = pool or (0, 1, 2, 3, 4, 5, 6, 7)
        k = self._bctr.get(pool, 0)
        self._bctr[pool] = k + 1
        b = pool[k % len(pool)]
        return self.pbank[b], self.rbank[b]

    def load_wk(self):
        i = self.wk_i
        self.wk_i += 1
        slot = i % self.NWK
        blk = self.wk_next
        self.wk_next += 1
        src = self.wkb[blk]
        dst = self.wring[slot]
        layer_res = self.r_wkb[self.blk_layer[blk]]
        self.P.dma("sp", lambda e: e.dma_start(out=dst, in_=src), reads=[layer_res], writes=[self.r_wring[slot]], key="wk%d" % slot)
        return self.wring3[slot], self.r_wring[slot]

    def arena_switch(self):
        cell = self.cell
        self.P.op("dve", lambda e: e.memset(cell[:], 0.0), writes=[self.r_arena])

    def rmsnorm(self, wcol):
        xT, hT, sq = self.xT, self.hT, self.sq
        pb, rb = self.bank()
        for c in range(8):
            self.act(sq[:, c % 4, :], xT[:, c, :], AF.Square, r=[self.r_x], w=[self.r_sqs[c % 4]])
            self.mm(pb, self.cb[:, B_O1024:B_O1024 + 128], sq[:, c % 4, :], start=(c == 0), stop=(c == 7), r=[self.r_sqs[c % 4], self.r_c], w=[rb])
        rstd = self.rstd
        self.act(rstd, pb, AF.Ln, r=[rb, self.r_c], w=[self.r_rstd], bias=self.epsb[:])
        self.act(rstd, rstd, AF.Exp, r=[self.r_rstd], w=[self.r_rstd], scale=-0.5)
        for c in range(8):
            self.stt("dve", hT[:, c, :], xT[:, c, :], self.sm[:, wcol + c:wcol + c + 1], rstd, ALU.mult, ALU.mult,
                     r=[self.r_x, self.r_rstd, self.r_c], w=[self.r_h])

    def ffn(self, l, i):
        self.rmsnorm(S_NORM + (l * 3 + 2 * i) * 8)
        self.arena_switch()
        A = self.arena
        actb = A[:, 0:22 * 512].rearrange("p (a b) -> p a b", a=22)
        ra = [self.r_arena]
        r_act = self.r_av[0]
        hT = self.hT
        gu = (4, 5, 6, 7)
        for jb in range(11):
            w, rw = self.load_wk()
            for half in range(2):
                fc = jb * 2 + half
                pg, rg = self.bank(gu)
                pu, ru = self.bank(gu)
                for kc in range(8):
                    self.mm(pg, w[:, kc, half * 128:(half + 1) * 128], hT[:, kc, :], start=(kc == 0), stop=(kc == 7), r=[rw, self.r_h], w=[rg])
                for kc in range(8):
                    self.mm(pu, w[:, kc, 256 + half * 128:256 + (half + 1) * 128], hT[:, kc, :], start=(kc == 0), stop=(kc == 7), r=[rw, self.r_h], w=[ru])
                sg, rsg = self.sgbuf[fc % 2], self.r_sg[fc % 2]
                self.act(sg, pg, AF.Silu, r=[rg], w=[rsg])
                self.tt("dve", actb[:, fc, :], sg, pu, ALU.mult, r=[rsg, ru] + ra, w=[r_act])
        for half in range(2):
            pool = (0, 1, 2, 3) if half == 0 else (4, 5, 6, 7)
            bks = [self.bank(pool) for _ in range(4)]
            for g in range(3):
                wo, rwo = self.load_wk()
                for f in range(8 if g < 2 else 6):
                    fc = g * 8 + f
                    for d in range(4):
                        self.mm(bks[d][0], wo[:, f, d * 128:(d + 1) * 128], actb[:, fc, :], start=(fc == 0), stop=(fc == 21),
                                r=[rwo, r_act] + ra, w=[bks[d][1]])
            for d in range(4):
                dc = half * 4 + d
                self.stt("dve", self.xT[:, dc, :], bks[d][0], 0.5, self.xT[:, dc, :], ALU.mult, ALU.add, r=[bks[d][1], self.r_x], w=[self.r_x])

    def gla(self, l, j, first_tile):
        self.rmsnorm(S_NORM + (l * 3 + 1) * 8)
        self.arena_switch()
        A, ra = self.arena, [self.r_arena]
        hT, cb, cf = self.hT, self.cb, self.cf
        o = 0

        def carve(n, shape3=None):
            nonlocal o
            v = A[:, o:o + n]
            o += n
            return v
        qd = carve(2048).rearrange("p (a b) -> p a b", a=4)
        ki = carve(2048).rearrange("p (a b) -> p a b", a=4)
        ke = carve(2048).rearrange("p (a b) -> p a b", a=4)
        vt = carve(4096).rearrange("p (a b) -> p a b", a=4)
        lg = carve(2048).rearrange("p (a b) -> p a b", a=4)
        rs = carve(4096).rearrange("p (a b) -> p a b", a=8)
        og = carve(4096).rearrange("p (a b) -> p a b", a=8)
        zT = carve(512)
        AT = carve(1024).rearrange("p (a b) -> p a b", a=2)
        r_qd, r_ki, r_ke, r_vt, r_lg, r_rs, r_og, r_zT, r_oT, r_eb = self.r_av[0:10]
        r_AT = self.r_av[10:12]
        oT = self.f32a[:, 0:4096].rearrange("p (a b) -> p a b", a=8)
        eb = self.f32a[:, 4096:6144].rearrange("p (a b) -> p a b", a=4)
        S, Sb = self.Sg[j], self.Sgb[j]
        rS, rSb = self.r_Sg[j], self.r_Sgb[j]
        sm = self.sm
        if first_tile:
            self.memset("pool", S[:], 0.0, w=[rS])
            self.memset("pool", Sb[:], 0.0, w=[rSb])
        pz, rz = self.bank()
        for kc in range(8):
            self.mm(pz[0:16, :], self.smb[:, j * 128 + kc * 16:j * 128 + kc * 16 + 16], hT[:, kc, :], start=(kc == 0), stop=(kc == 7),
                    r=[self.r_h, self.r_c], w=[rz])
        self.cp("act", zT[0:16, :], pz[0:16, :], r=[rz] + ra, w=[r_zT])
        for b in range(4):
            pl, rl = self.bank()
            self.mm(pl, zT[0:16, b * 128:(b + 1) * 128], self.wgkb[0:16, j * 512:(j + 1) * 512], start=True, stop=False, r=[r_zT, self.r_c] + ra, w=[rl])
            self.mm(pl, self.cb[0:1, B_O1:B_O1 + 128], self.bgkb[0:1, j * 512:(j + 1) * 512], start=False, stop=True, r=[self.r_c], w=[rl])
            t0, rt0 = self.tmpf[b % 2], self.r_tmpf[b % 2]
            self.act(t0, pl, AF.Exp, r=[rl], w=[rt0], scale=-1.0)
            self.act(lg[:, b, :], t0, AF.Ln, r=[rt0, self.r_c] + ra, w=[r_lg], bias=self.oneb[:])
        w, rw = self.load_wk()
        for b in range(4):
            pk, rk = self.bank()
            for kc in range(8):
                self.mm(pk, hT[:, kc, b * 128:(b + 1) * 128], w[:, kc, :], start=(kc == 0), stop=(kc == 7), r=[rw, self.r_h], w=[rk])
            pd, rd = self.bank()
            self.mm(pd, cb[:, B_TR2:B_TR2 + 128], lg[:, b, :], r=[r_lg, self.r_c] + ra, w=[rd])
            t0, rt0 = self.tmpf[b % 2], self.r_tmpf[b % 2]
            self.act(t0, pd, AF.Exp, r=[rd], w=[rt0])
            self.tt("dve", ke[:, b, :], pk, t0, ALU.mult, r=[rk, rt0] + ra, w=[r_ke])
        for h in range(4):
            pk, rk = self.bank()
            for kc in range(8):
                self.mm(pk, w[:, kc, h * 128:(h + 1) * 128], hT[:, kc, :], start=(kc == 0), stop=(kc == 7), r=[rw, self.r_h], w=[rk])
            pb, rb = self.bank()
            for b in range(4):
                self.mm(pb[:, b * 128:(b + 1) * 128], lg[:, b, h * 128:(h + 1) * 128], cb[:, B_TRB:B_TRB + 128], r=[r_lg, self.r_c] + ra, w=[rb])
            t0, rt0 = self.tmpf[h % 2], self.r_tmpf[h % 2]
            self.act(t0, pb, AF.Exp, r=[rb], w=[rt0], scale=-1.0)
            self.act(eb[:, h, :], pb, AF.Exp, r=[rb], w=[r_eb])
            self.tt("dve", ki[:, h, :], pk, t0, ALU.mult, r=[rk, rt0] + ra, w=[r_ki])
        w, rw = self.load_wk()
        for h in range(4):
            pq, rq = self.bank()
            for kc in range(8):
                self.mm(pq, w[:, kc, h * 128:(h + 1) * 128], hT[:, kc, :], start=(kc == 0), stop=(kc == 7), r=[rw, self.r_h], w=[rq])
            self.stt("dve", qd[:, h, :], pq, 128.0 ** -0.5, eb[:, h, :], ALU.mult, ALU.mult, r=[rq, r_eb] + ra, w=[r_qd])
        for vb in range(2):
            w, rw = self.load_wk()
            for b in range(4):
                pv, rv = self.bank()
                for kc in range(8):
                    self.mm(pv, hT[:, kc, b * 128:(b + 1) * 128], w[:, kc, :], start=(kc == 0), stop=(kc == 7), r=[rw, self.r_h], w=[rv])
                self.cp("act" if b % 2 == 0 else "dve", vt[:, b, vb * 512:(vb + 1) * 512], pv, r=[rv] + ra, w=[r_vt])
        for rb_ in range(2):
            w, rw = self.load_wk()
            for cc in range(4):
                pr, rr = self.bank()
                for kc in range(8):
                    self.mm(pr, w[:, kc, cc * 128:(cc + 1) * 128], hT[:, kc, :], start=(kc == 0), stop=(kc == 7), r=[rw, self.r_h], w=[rr])
                self.act(rs[:, rb_ * 4 + cc, :], pr, AF.Silu, r=[rr] + ra, w=[r_rs])
        SP = (3, 4, 5, 6, 7)
        for b in range(4):
            bc = slice(b * 128, (b + 1) * 128)
            pA, rA = self.bank((0,))
            for h in range(4):
                self.mm(pA[:, h * 128:(h + 1) * 128], ki[:, h, bc], qd[:, h, bc], r=[r_ki, r_qd] + ra, w=[rA])
            at, rat = AT[:, b % 2, :], r_AT[b % 2]
            self.tt("dve", at, pA, cb[:, B_LI4:B_LI4 + 512], ALU.mult, r=[rA, self.r_c] + ra, w=[rat])
            po = [self.bank((1,)), self.bank((2,))]
            for pob, rob in po:
                self.memset("dve", pob, 0.0, w=[rob])
            for e in range(2):
                cs = slice(b * 128 + e * 64, b * 128 + (e + 1) * 64)
                last = b * 128 + e * 64 + 63
                for h in range(4):
                    pob, rob = po[h // 2]
                    for half in range(2):
                        c0 = (h % 2) * 256 + half * 128 + e * 64
                        self.mm(pob[:, c0:c0 + 64], Sb[:, h, half * 128:(half + 1) * 128], qd[:, h, cs], start=False, stop=False,
                                r=[rSb, r_qd] + ra, w=[rob])
                    pS, rpS = self.bank(SP)
                    self.mm(pS[:, 0:256], ke[e * 64:(e + 1) * 64, b, h * 128:(h + 1) * 128], vt[e * 64:(e + 1) * 64, b, h * 256:(h + 1) * 256],
                            r=[r_ke, r_vt] + ra, w=[rpS])
                    self.stt("dve", S[:, h, :], S[:, h, :], eb[:, h, last:last + 1], pS[:, 0:256], ALU.mult, ALU.add, r=[rS, r_eb, rpS], w=[rS])
                    self.cp("act", Sb[:, h, :], S[:, h, :], r=[rS], w=[rSb])
            for h in range(4):
                pob, rob = po[h // 2]
                for half in range(2):
                    c0 = (h % 2) * 256 + half * 128
                    self.mm(pob[:, c0:c0 + 128], vt[:, b, h * 256 + half * 128:h * 256 + (half + 1) * 128], at[:, h * 128:(h + 1) * 128],
                            start=False, stop=True, r=[r_vt, rat] + ra, w=[rob])
            for h2 in range(2):
                pob, rob = po[h2]
                self.cp("act", oT[:, h2 * 4:(h2 + 1) * 4, bc], pob.rearrange("p (a b) -> p a b", a=4), r=[rob], w=[r_oT])
        sq = self.sq
        for h in range(4):
            pn, rn = self.bank()
            for half in range(2):
                k4 = (h * 2 + half) % 4
                self.act(sq[:, k4, :], oT[:, h * 2 + half, :], AF.Square, r=[r_oT], w=[self.r_sqs[k4]])
                self.mm(pn, cb[:, B_O256:B_O256 + 128], sq[:, k4, :], start=(half == 0), stop=(half == 1), r=[self.r_sqs[k4], self.r_c], w=[rn])
            t0, rt0 = self.tmpf[h % 2], self.r_tmpf[h % 2]
            self.act(t0, pn, AF.Ln, r=[rn, self.r_c], w=[rt0], bias=self.epsb[:])
            self.act(t0, t0, AF.Exp, r=[rt0], w=[rt0], scale=-0.5)
            for half in range(2):
                c = h * 2 + half
                self.stt("dve", oT[:, c, :], oT[:, c, :], sm[:, S_GLAN + j * 2 + half:S_GLAN + j * 2 + half + 1], t0, ALU.mult, ALU.mult,
                         r=[r_oT, rt0, self.r_c], w=[r_oT])
                self.tt("dve", og[:, c, :], oT[:, c, :], rs[:, c, :], ALU.mult, r=[r_oT, r_rs] + ra, w=[r_og])
        self.out_proj(og, r_og, ra)

    def out_proj(self, og, r_og, ra):
        for blk in range(2):
            w, rw = self.load_wk()
            for dq in range(4):
                dc = blk * 4 + dq
                py, ry = self.bank()
                for vc in range(8):
                    self.mm(py, w[:, vc, dq * 128:(dq + 1) * 128], og[:, vc, :], start=(vc == 0), stop=(vc == 7), r=[rw, r_og] + ra, w=[ry])
                self.tt("dve", self.xT[:, dc, :], py, self.xT[:, dc, :], ALU.add, r=[ry, self.r_x], w=[self.r_x])

    def gdn(self, l, j, first_tile):
        self.rmsnorm(S_NORM + (l * 3 + 1) * 8)
        self.arena_switch()
        A, ra = self.arena, [self.r_arena]
        hT, cb, cf, sm = self.hT, self.cb, self.cf, self.sm
        o = 0

        def carve(n):
            nonlocal o
            v = A[:, o:o + n]
            o += n
            return v
        xc2 = carve(2 * 516).rearrange("p (a b) -> p a b", a=2)
        qkv = carve(24 * 512).rearrange("p (a b) -> p a b", a=24)
        og = carve(4096).rearrange("p (a b) -> p a b", a=8)
        ogt = carve(1024)
        ktok = carve(1024).rearrange("p (a b) -> p a b", a=8)
        kdtok = carve(1024).rearrange("p (a b) -> p a b", a=8)
        vtok = carve(1024).rearrange("p (a b) -> p a b", a=8)
        Pm = [carve(1024).rearrange("p (a b) -> p a b", a=8) for _ in range(2)]
        PmT = [carve(1024).rearrange("p (a b) -> p a b", a=8) for _ in range(2)]
        Rm = [carve(1024).rearrange("p (a b) -> p a b", a=8) for _ in range(2)]
        qkT = carve(1024).rearrange("p (a b) -> p a b", a=8)
        w0T = carve(1024).rearrange("p (a b) -> p a b", a=8)
        vnew = carve(1024).rearrange("p (a b) -> p a b", a=8)
        (r_xc0, r_qkv, r_og, r_ogt, r_kt, r_kd, r_vt, r_P0, r_P1, r_PT0, r_PT1, r_R0, r_R1, r_qk, r_w0, r_vn, r_rt, r_u0, r_sc, r_gu, r_dt) = self.r_av[0:21]
        r_P, r_PT, r_R = [r_P0, r_P1], [r_PT0, r_PT1], [r_R0, r_R1]
        r_sc = self.r_scs
        r_xcs = [r_xc0, self.r_av[21]]
        f = self.f32a
        rtok = f[:, 0:1024]
        u0 = f[:, 1024:2048].rearrange("p (a b) -> p a b", a=8)
        gu = f[:, 2048:3072].rearrange("p (a b) -> p a b", a=8)
        dtm = f[:, 3072:4096].rearrange("p (a b) -> p a b", a=8)
        sc = f[:, 4096:4096 + 256]
        otok = f[:, 4608:5632].rearrange("p (a b) -> p a b", a=8)
        S, Sb = self.Sd[j], self.Sdb[j]
        rS, rSb = self.r_Sd[j], self.r_Sdb[j]
        ct, rct = self.ctail[j], self.r_ctail[j]
        if first_tile:
            self.memset("pool", S[:], 0.0, w=[rS])
            self.memset("pool", Sb[:], 0.0, w=[rSb])
            self.memset("pool", ct[:], 0.0, w=[rct])
        pab, rab = self.bank()
        for b in range(4):
            for kc in range(8):
                self.mm(pab[:, b * 16:(b + 1) * 16], hT[:, kc, b * 128:(b + 1) * 128], self.smb[:, 256 + j * 128 + kc * 16:256 + j * 128 + kc * 16 + 16],
                        start=(kc == 0), stop=(kc == 7), r=[self.r_h, self.r_c], w=[rab])
        pab3 = pab[:, 0:64].rearrange("p (b c) -> p b c", b=4)
        sc3 = sc.rearrange("p (b c) -> p b c", b=4)
        for b in range(4):
            self.tt("dve", sc3[:, b, 0:8], pab3[:, b, 0:8], sm[:, S_DTB + j * 8:S_DTB + j * 8 + 8], ALU.add, r=[rab, self.r_c], w=[r_sc])
        self.act(sc3[:, :, 0:8], sc3[:, :, 0:8], AF.Exp, r=[r_sc], w=[r_sc])
        self.act(sc3[:, :, 0:8], sc3[:, :, 0:8], AF.Ln, r=[r_sc, self.r_c], w=[r_sc], bias=self.oneb[:])
        for b in range(4):
            self.tt("dve", sc3[:, b, 8:16], sc3[:, b, 0:8], self.negA[:, j * 8:j * 8 + 8], ALU.mult, r=[r_sc, self.r_c], w=[r_sc])
        self.act(sc3[:, :, 16:24], pab3[:, :, 8:16], AF.Exp, r=[rab], w=[r_sc], scale=-1.0)
        self.act(sc3[:, :, 16:24], sc3[:, :, 16:24], AF.Ln, r=[r_sc, self.r_c], w=[r_sc], bias=self.oneb[:])
        self.act(sc3[:, :, 16:24], sc3[:, :, 16:24], AF.Exp, r=[r_sc], w=[r_sc], scale=-1.0)
        pdd, rdd = self.bank()
        for b in range(4):
            g_b = sc3[:, b, 8:16]
            self.mm(pdd[:, b * 16:b * 16 + 8], cf[:, C_LI:C_LI + 128], g_b, r=[r_sc, self.r_c], w=[rdd])
            self.mm(pdd[:, b * 16 + 8:b * 16 + 16], cf[:, C_US:C_US + 128], g_b, r=[r_sc, self.r_c], w=[rdd])
            self.mm(pdd[:, 64 + b * 16:64 + b * 16 + 8], cf[:, C_CE:C_CE + 128], g_b, r=[r_sc, self.r_c], w=[rdd])
            self.mm(pdd[:, 64 + b * 16 + 8:64 + b * 16 + 16], cf[:, C_CO:C_CO + 128], g_b, r=[r_sc, self.r_c], w=[rdd])
        pdd3 = pdd[:, 0:64].rearrange("p (b c) -> p b c", b=4)
        self.act(sc3[:, :, 40:56], pdd3, AF.Exp, r=[rdd], w=[r_sc])
        edl = self.edl
        self.act(edl, pdd[:, 64:128].rearrange("p (b c) -> p b c", b=4), AF.Exp, r=[rdd], w=[self.r_edl])
        for blk in range(6):
            w, rw = self.load_wk()
            for cq in range(4):
                cc = blk * 4 + cq
                pp, rp = self.bank()
                for kc in range(8):
                    self.mm(pp, w[:, kc, cq * 128:(cq + 1) * 128], hT[:, kc, :], start=(kc == 0), stop=(kc == 7), r=[rw, self.r_h], w=[rp])
                xc, r_xc = xc2[:, cc % 2, :], r_xcs[cc % 2]
                self.cp("pool", xc[:, 0:3], ct[:, cc, :], r=[rct] + ra, w=[r_xc])
                self.cp("act", xc[:, 3:515], pp, r=[rp] + ra, w=[r_xc])
                t0, rt0 = self.tmpf[cc % 2], self.r_tmpf[cc % 2]
                cw = S_CONV + (j * 4) * 24 + cc
                eng = "dve"
                self.ts(eng, t0, xc[:, 3:515], sm[:, cw + 3 * 24:cw + 3 * 24 + 1], None, ALU.mult, r=[r_xc, self.r_c] + ra, w=[rt0])
                for tap in (2, 1, 0):
                    self.stt(eng, t0, xc[:, tap:tap + 512], sm[:, cw + tap * 24:cw + tap * 24 + 1], t0, ALU.mult, ALU.add,
                             r=[r_xc, rt0, self.r_c] + ra, w=[rt0])
                self.cp("pool", ct[:, cc, :], xc[:, 512:515], r=[r_xc] + ra, w=[rct])
                if cc >= 16:
                    self.act(qkv[:, cc, :], t0, AF.Silu, r=[rt0] + ra, w=[r_qkv])
                else:
                    t1, rt1 = self.tmpg[cc % 2], self.r_tmpg[cc % 2]
                    self.act(t1, t0, AF.Silu, r=[rt0], w=[rt1])
                    self.act(self.sq[:, cc % 4, :], t1, AF.Square, r=[rt1], w=[self.r_sqs[cc % 4]])
                    pn, rn = self.bank()
                    self.mm(pn, cb[:, B_O1:B_O1 + 128], self.sq[:, cc % 4, :], r=[self.r_sqs[cc % 4], self.r_c], w=[rn])
                    t2, rt2 = self.tmph[cc % 2], self.r_tmph[cc % 2]
                    self.act(t2, pn, AF.Ln, r=[rn, self.r_c], w=[rt2], bias=self.epsb[:])
                    self.act(t2, t2, AF.Exp, r=[rt2], w=[rt2], scale=-0.5)
                    if cc < 8:
                        self.stt("dve", qkv[:, cc, :], t1, 128.0 ** -0.5, t2, ALU.mult, ALU.mult, r=[rt1, rt2] + ra, w=[r_qkv])
                    else:
                        self.tt("dve", qkv[:, cc, :], t1, t2, ALU.mult, r=[rt1, rt2] + ra, w=[r_qkv])
        self.dump("sc", sc, [r_sc], F32)
        self.dump("edl", edl, [self.r_edl], F32)
        self.dump("qkv", qkv, [r_qkv], BF16)
        wr = [self.load_wk(), self.load_wk()]
        idb = cb[:, B_ID:B_ID + 128]
        for b in range(4):
            bc = slice(b * 128, (b + 1) * 128)
            for rb_ in range(2):
                w, rw = wr[rb_]
                pr, rr = self.bank((6, 7))
                for kc in range(8):
                    self.mm(pr, hT[:, kc, bc], w[:, kc, :], start=(kc == 0), stop=(kc == 7), r=[rw, self.r_h], w=[rr])
                self.act(rtok[:, rb_ * 512:(rb_ + 1) * 512], pr, AF.Silu, r=[rr], w=[r_rt])
            for grp in range(2):
                hs = range(grp * 4, grp * 4 + 4)
                ptk, rtk = self.bank((6, 7))
                ptk_b = self.pbank_bf[self._last_bank_id((6, 7))]
                for h in hs:
                    self.tr(ptk_b[:, (h % 4) * 128:(h % 4 + 1) * 128], qkv[:, 8 + h, bc], idb, r=[r_qkv, self.r_c] + ra, w=[rtk])
                for h in hs:
                    self.ts("dve", ktok[:, h, :], ptk_b[:, (h % 4) * 128:(h % 4 + 1) * 128], sc3[:, b, 48 + h:49 + h], None, ALU.mult, r=[rtk, r_sc] + ra, w=[r_kt])
                    self.ts("dve", kdtok[:, h, :], ptk_b[:, (h % 4) * 128:(h % 4 + 1) * 128], sc3[:, b, 40 + h:41 + h], None, ALU.mult,
                            r=[rtk, r_sc] + ra, w=[r_kd])
                ptv, rtv = self.bank((6, 7))
                ptv_b = self.pbank_bf[self._last_bank_id((6, 7))]
                for h in hs:
                    self.tr(ptv_b[:, (h % 4) * 128:(h % 4 + 1) * 128], qkv[:, 16 + h, bc], idb, r=[r_qkv, self.r_c] + ra, w=[rtv])
                self.cp("act", vtok[:, grp * 4:grp * 4 + 4, :], ptv_b[:, 0:512].rearrange("p (a b) -> p a b", a=4), r=[rtv] + ra, w=[r_vt])
            for h in range(8):
                self.ts("pool", gu[:, h, :], cf[:, C_US:C_US + 128], sc3[:, b, 8 + h:9 + h], None, ALU.mult, r=[self.r_c, r_sc], w=[r_gu])
            for grp in range(2):
                pD, rD = self.bank((4, 5))
                for h4 in range(4):
                    h = grp * 4 + h4
                    self.mm(pD[:, h4 * 128:(h4 + 1) * 128], gu[:, h, :], cf[:, C_LI:C_LI + 128], r=[r_gu, self.r_c], w=[rD])
                t0, rt0 = self.tmpf[grp], self.r_tmpf[grp]
                self.act(t0, pD, AF.Exp, r=[rD], w=[rt0])
                self.tt("dve", dtm[:, grp * 4:grp * 4 + 4, :], t0.rearrange("p (a b) -> p a b", a=4),
                        cb[:, B_LI4:B_LI4 + 512].rearrange("p (a b) -> p a b", a=4),
                        ALU.mult, r=[rt0, self.r_c], w=[r_dt])
            for grp in range(2):
                pX, rX = self.bank((4, 5))
                for h4 in range(4):
                    h = grp * 4 + h4
                    self.mm(pX[:, h4 * 128:(h4 + 1) * 128], qkv[:, 8 + h, bc], qkv[:, 8 + h, bc], r=[r_qkv] + ra, w=[rX])
                for h4 in range(4):
                    h = grp * 4 + h4
                    self.stt("dve", self.xtmp[:, h4, :], pX[:, h4 * 128:(h4 + 1) * 128], sc3[:, b, 16 + h:17 + h], dtm[:, h, :], ALU.mult, ALU.mult,
                             r=[rX, r_sc, r_dt], w=[self.r_xtmp])
                self.stt("dve", Pm[0][:, grp * 4:grp * 4 + 4, :], self.xtmp[:, :, :], -1.0, cb[:, B_LS4:B_LS4 + 512].rearrange("p (a b) -> p a b", a=4),
                         ALU.mult, ALU.mult, r=[self.r_xtmp, self.r_c] + ra, w=[r_P[0]])
                pQ, rQ = self.bank((4, 5))
                for h4 in range(4):
                    h = grp * 4 + h4
                    self.mm(pQ[:, h4 * 128:(h4 + 1) * 128], qkv[:, 8 + h, bc], qkv[:, h, bc], r=[r_qkv] + ra, w=[rQ])
                self.tt("dve", qkT[:, grp * 4:grp * 4 + 4, :], pQ.rearrange("p (a b) -> p a b", a=4), dtm[:, grp * 4:grp * 4 + 4, :], ALU.mult,
                        r=[rQ, r_dt] + ra, w=[r_qk])
            self.dump("dtm", dtm, [r_dt], F32)
            self.dump("P0", Pm[0], [r_P[0]], BF16)
            self.dump("qkT", qkT, [r_qk], BF16)
            self.dump("ktok", ktok, [r_kt], BF16)
            self.dump("vtok", vtok, [r_vt], BF16)
            for grp in range(2):
                g4 = slice(grp * 4, grp * 4 + 4)
                pT, rT = self.bank((4, 5))
                pT_b = self.pbank_bf[self._last_bank_id((4, 5))]
                for h4 in range(4):
                    self.tr(pT_b[:, h4 * 128:(h4 + 1) * 128], Pm[0][:, grp * 4 + h4, :], idb, r=[r_P[0], self.r_c] + ra, w=[rT])
                self.cp("act", PmT[0][:, g4, :], pT_b[:, 0:512].rearrange("p (a b) -> p a b", a=4), r=[rT] + ra, w=[r_PT[0]])
                self.tt("pool", Rm[0][:, g4, :], Pm[0][:, g4, :], self.id4,
                        ALU.add, r=[r_P[0], self.r_c] + ra, w=[r_R[0]])
            for n in range(1, 6):
                cur, prv = n % 2, (n - 1) % 2
                for grp in range(2):
                    g4 = slice(grp * 4, grp * 4 + 4)
                    if n < 5:
                        pP, rP = self.bank((0, 1, 2, 3))
                        for h4 in range(4):
                            h = grp * 4 + h4
                            self.mm(pP[:, h4 * 128:(h4 + 1) * 128], PmT[prv][:, h, :], Pm[prv][:, h, :], r=[r_PT[prv], r_P[prv]] + ra, w=[rP])
                        self.cp("act", Pm[cur][:, g4, :], pP.rearrange("p (a b) -> p a b", a=4), r=[rP] + ra, w=[r_P[cur]])
                    pPT, rPT = self.bank((0, 1, 2, 3))
                    for h4 in range(4):
                        h = grp * 4 + h4
                        self.mm(pPT[:, h4 * 128:(h4 + 1) * 128], Pm[prv][:, h, :], PmT[prv][:, h, :], r=[r_PT[prv], r_P[prv]] + ra, w=[rPT])
                    self.cp("dve", PmT[cur][:, g4, :], pPT.rearrange("p (a b) -> p a b", a=4), r=[rPT] + ra, w=[r_PT[cur]])
                    pR, rR = self.bank((0, 1, 2, 3))
                    for h4 in range(4):
                        h = grp * 4 + h4
                        self.mm(pR[:, h4 * 128:(h4 + 1) * 128], PmT[cur][:, h, :], Rm[prv][:, h, :], start=True, stop=False, r=[r_PT[cur], r_R[prv]] + ra, w=[rR])
                        self.mm(pR[:, h4 * 128:(h4 + 1) * 128], idb, Rm[prv][:, h, :], start=False, stop=True, r=[self.r_c, r_R[prv]] + ra, w=[rR])
                    self.cp("act" if grp == 0 else "dve", Rm[cur][:, g4, :], pR.rearrange("p (a b) -> p a b", a=4), r=[rR] + ra, w=[r_R[cur]])
            Rf, r_Rf = Rm[1], r_R[1]
            self.dump("Rf", Rf, [r_Rf], BF16)
            for grp in range(2):
                g4 = slice(grp * 4, grp * 4 + 4)
                pU, rU = self.bank((0, 1, 2, 3))
                for h4 in range(4):
                    h = grp * 4 + h4
                    self.mm(pU[:, h4 * 128:(h4 + 1) * 128], Rf[:, h, :], vtok[:, h, :], r=[r_Rf, r_vt] + ra, w=[rU])
                self.cp("act", u0[:, g4, :], pU.rearrange("p (a b) -> p a b", a=4), r=[rU], w=[r_u0])
                pW, rW = self.bank((0, 1, 2, 3))
                for h4 in range(4):
                    h = grp * 4 + h4
                    self.mm(pW[:, h4 * 128:(h4 + 1) * 128], kdtok[:, h, :], Rf[:, h, :], r=[r_Rf, r_kd] + ra, w=[rW])
                self.cp("dve", w0T[:, g4, :], pW.rearrange("p (a b) -> p a b", a=4), r=[rW] + ra, w=[r_w0])
            self.dump("u0", u0, [r_u0], F32)
            self.dump("w0T", w0T, [r_w0], BF16)
            po = [self.bank((4,)), self.bank((5,))]
            for e in range(2):
                ps_ = slice(e * 64, (e + 1) * 64)
                cs = slice(b * 128 + e * 64, b * 128 + (e + 1) * 64)
                for grp in range(2):
                    pws, rws = self.bank((0, 1, 2, 3))
                    for h4 in range(4):
                        h = grp * 4 + h4
                        self.mm(pws[ps_, h4 * 128:(h4 + 1) * 128], w0T[:, h, e * 64:(e + 1) * 64], Sb[:, h, :], r=[r_w0, rSb] + ra, w=[rws])
                        self.mm(po[grp][0][ps_, h4 * 128:(h4 + 1) * 128], qkv[:, h, cs], Sb[:, h, :], r=[r_qkv, rSb] + ra, w=[po[grp][1]])
                    g4 = slice(grp * 4, grp * 4 + 4)
                    self.tt("dve", self.vtmp[ps_, :, :], u0[ps_, g4, :], pws[ps_, :].rearrange("p (a b) -> p a b", a=4), ALU.subtract,
                            r=[r_u0, rws], w=[self.r_vtmp])
                    for h4 in range(4):
                        h = grp * 4 + h4
                        self.ts("dve", vnew[ps_, h, :], self.vtmp[ps_, h4, :], sc3[ps_, b, 16 + h:17 + h], None, ALU.mult, r=[self.r_vtmp, r_sc] + ra, w=[r_vn])
                    for h4 in range(4):
                        h = grp * 4 + h4
                        pS, rpS = self.bank((6, 7))
                        self.mm(pS[:, 0:128], ktok[ps_, h, :], vnew[ps_, h, :], r=[r_kt, r_vn] + ra, w=[rpS])
                        self.stt("dve", S[:, h, :], S[:, h, :], edl[:, b, e * 8 + h:e * 8 + h + 1], pS[:, 0:128], ALU.mult, ALU.add, r=[rS, self.r_edl, rpS], w=[rS])
                        self.cp("act", Sb[:, h, :], S[:, h, :], r=[rS], w=[rSb])
            for grp in range(2):
                g4 = slice(grp * 4, grp * 4 + 4)
                for h4 in range(4):
                    h = grp * 4 + h4
                    self.act(otok[:, h, :], po[grp][0][:, h4 * 128:(h4 + 1) * 128], AF.Copy, r=[po[grp][1], r_sc], w=[self.r_otok], scale=sc3[:, b, 40 + h:41 + h])
                pI, rI = self.bank((0, 1, 2, 3))
                for h4 in range(4):
                    h = grp * 4 + h4
                    self.mm(pI[:, h4 * 128:(h4 + 1) * 128], qkT[:, h, :], vnew[:, h, :], r=[r_qk, r_vn] + ra, w=[rI])
                self.tt("dve", otok[:, g4, :], otok[:, g4, :], pI.rearrange("p (a b) -> p a b", a=4), ALU.add, r=[self.r_otok, rI], w=[self.r_otok])
            self.dump("vnew", vnew, [r_vn], BF16)
            self.dump("otok", otok, [self.r_otok], F32)
            if b == 3:
                self.dump("otok3", otok, [self.r_otok], F32)
            ssq = self.ssq
            osq = f[:, 5632:6656].rearrange("p (a b) -> p a b", a=8)
            self.act(osq, otok, AF.Square, r=[self.r_otok], w=[self.r_junk])
            self.P.op("dve", lambda e, o_=ssq[:, 0:8], i_=osq: e.reduce_sum(out=o_, in_=i_, axis=mybir.AxisListType.X), reads=[self.r_junk], writes=[self.r_ssq])
            self.ts("dve", ssq[:, 0:8], ssq[:, 0:8], 1.0 / 128.0, None, ALU.mult, r=[self.r_ssq], w=[self.r_ssq])
            self.act(ssq[:, 0:8], ssq[:, 0:8], AF.Ln, r=[self.r_ssq, self.r_c], w=[self.r_ssq], bias=self.epsb[:])
            self.act(ssq[:, 0:8], ssq[:, 0:8], AF.Exp, r=[self.r_ssq], w=[self.r_ssq], scale=-0.5)
            for h in range(8):
                self.stt("dve", otok[:, h, :], otok[:, h, :], ssq[:, h:h + 1], sm[:, S_GDNN + j * 128:S_GDNN + (j + 1) * 128], ALU.mult, ALU.mult,
                         r=[self.r_otok, self.r_ssq, self.r_c], w=[self.r_otok])
            self.tt("dve", ogt[:, :], otok.rearrange("p a b -> p (a b)"), rtok, ALU.mult, r=[self.r_otok, r_rt] + ra, w=[r_ogt])
            self.dump("ogt", ogt, [r_ogt], BF16)
            self.dump("rtok", rtok, [r_rt], F32)
            self.dump("ssq", self.ssq, [self.r_ssq], F32)
            for grp in range(2):
                pG, rG = self.bank((0, 1, 2, 3))
                pG_b = self.pbank_bf[self._last_bank_id((0, 1, 2, 3))]
                for h4 in range(4):
                    c = grp * 4 + h4
                    self.tr(pG_b[:, h4 * 128:(h4 + 1) * 128], ogt[:, c * 128:(c + 1) * 128], idb, r=[r_ogt, self.r_c] + ra, w=[rG])
                self.cp("act", og[:, grp * 4:grp * 4 + 4, bc], pG_b[:, 0:512].rearrange("p (a b) -> p a b", a=4), r=[rG] + ra, w=[r_og])
        self.dump("og", og, [r_og], BF16)
        self.dump("Sd", S, [rS], F32)
        self.out_proj(og, r_og, ra)

    def dump(self, name, ap, reads, dt):
        if not getattr(self, "debug", False) or name in self._dumped:
            return
        self._dumped.add(name)
        d = self.nc.dram_tensor("dbg_" + name, list(ap.shape), dt, kind="ExternalOutput").ap()
        self.P.dma("sp", lambda e: e.dma_start(out=d, in_=ap), reads=reads, key="dbg_" + name)

    def _last_bank_id(self, pool):
        k = self._bctr[pool] - 1
        return pool[k % len(pool)]

    def build(self):
        nc = bass.Bass("TRN2", target_bir_lowering=False)
        self.nc = nc
        depth, nseq, ntile = self.depth, self.nseq, self.ntile
        ntok = nseq * ntile * T
        x_d = nc.dram_tensor("x", [ntok, D], F32, kind="ExternalInput").ap()
        wk_d = nc.dram_tensor("wk", [self.NB, 128, 4096], F32, kind="ExternalInput").ap()
        sm_d = nc.dram_tensor("sm", [128, NSM], F32, kind="ExternalInput").ap()
        cf_d = nc.dram_tensor("cf", [128, NCF], F32, kind="ExternalInput").ap()
        cb_d = nc.dram_tensor("cb", [128, NCB], F32, kind="ExternalInput").ap()
        y_d = nc.dram_tensor("y", [ntok, D], F32, kind="ExternalOutput").ap()
        self.wkb = nc.dram_tensor("wkb", [self.NB, 128, 4096], BF16, kind="Internal").ap()
        self.blk_layer = []
        for l in range(depth):
            self.blk_layer += [l] * self.nblk_layer[l]
        with contextlib.ExitStack() as st:
            def sb(name, shape, dt):
                return st.enter_context(nc.sbuf_tensor("s_" + name, shape, dt))
            P = self.P = Prog(nc)
            self._bctr = {}
            self._dumped = set()
            pall = st.enter_context(nc.psum_tensor("pall", [128, 8 * 512], F32))
            self.pbank = [pall[:, k * 512:(k + 1) * 512] for k in range(8)]
            self.rbank = [Res("bank%d" % k) for k in range(8)]
            self.pbank_bf = [pall[:, k * 512:(k + 1) * 512].bitcast(BF16) for k in range(8)]
            self.xT = sb("xT", [128, 8, T], F32)
            self.hT = sb("hT", [128, 8, T], BF16)
            self.sq = sb("sq", [128, 4, T], BF16)
            self.rstd = sb("rstd", [128, T], F32)[:]
            self.r_x, self.r_h, self.r_rstd = Res("x"), Res("h"), Res("rstd")
            self.r_sqs = [Res("sq%d" % k) for k in range(4)]
            self.xT, self.hT, self.sq = self.xT[:], self.hT[:], self.sq[:]
            self.tmpf = [sb("tmpf%d" % k, [128, T], F32)[:] for k in range(2)]
            self.r_tmpf = [Res("tmpf%d" % k) for k in range(2)]
            self.tmpg = [sb("tmpg%d" % k, [128, T], F32)[:] for k in range(2)]
            self.r_tmpg = [Res("tmpg%d" % k) for k in range(2)]
            self.tmph = [sb("tmph%d" % k, [128, T], F32)[:] for k in range(1)] * 2
            self.r_tmph = [Res("tmph0")] * 2
            self.sgbuf = [sb("sg%d" % k, [128, T], F32)[:] for k in range(2)]
            self.r_sg = [Res("sg%d" % k) for k in range(2)]
            self.NWK = 4
            wring = [sb("wring%d" % k, [128, 4096], BF16) for k in range(self.NWK)]
            self.wring = [w_[:] for w_ in wring]
            self.wring3 = [w_[:].rearrange("p (a b) -> p a b", a=8) for w_ in wring]
            self.r_wring = [Res("wring%d" % k) for k in range(self.NWK)]
            self.wk_i = 0
            self.arena = sb("arena", [128, 2 * 516 + 24 * 512 + 4096 + 13 * 1024 + 64], BF16)[:]
            self.r_arena = Res("arena")
            self.r_av = [Res("av%d" % k, parent=self.r_arena) for k in range(24)]
            self.f32a = sb("f32a", [128, 6656], F32)[:]
            self.cell = sb("cell", [128, 2], F32)
            self.cf = sb("cf", [128, NCF], F32)[:]
            self.cb = sb("cb", [128, NCB], BF16)[:]
            self.sm = sb("sm", [128, S_WZ], F32)[:]
            self.smb = sb("smb", [128, 512], BF16)[:]
            self.wgkb = sb("wgkb", [16, 1024], BF16)[:]
            self.bgkb = sb("bgkb", [1, 1024], BF16)[:]
            self.epsb = sb("epsb", [128, 1], F32)
            self.oneb = sb("oneb", [128, 1], F32)
            self.negA = sb("negA", [128, 16], F32)[:]
            self.id4 = sb("id4", [128, 4, 128], BF16)[:]
            self.xtmp = sb("xtmp", [128, 4, 128], F32)[:]
            self.r_xtmp = Res("xtmp")
            self.vtmp = sb("vtmp", [128, 4, 128], F32)[:]
            self.r_vtmp = Res("vtmp")
            self.edl = sb("edl", [128, 4, 16], F32)[:]
            self.r_edl = Res("edl", strict=True)
            self.ssq = sb("ssq", [128, 8], F32)[:]
            self.r_ssq = Res("ssq", strict=True)
            self.r_scs = Res("scs", parent=self.r_arena, strict=True)
            self.r_junk = Res("junk")
            self.r_otok = Res("otok", parent=self.r_arena)
            self.r_c = Res("consts", strict=True)
            self.Sg = [sb("Sg%d" % k, [128, 4, 256], F32)[:] for k in range(2)]
            self.Sgb = [sb("Sgb%d" % k, [128, 4, 256], BF16)[:] for k in range(2)]
            self.Sd = [sb("Sd%d" % k, [128, 8, 128], F32)[:] for k in range(2)]
            self.Sdb = [sb("Sdb%d" % k, [128, 8, 128], BF16)[:] for k in range(2)]
            self.ctail = [sb("ct%d" % k, [128, 24, 3], BF16)[:] for k in range(2)]
            self.r_Sg = [Res("Sg%d" % k) for k in range(2)]
            self.r_Sgb = [Res("Sgb%d" % k) for k in range(2)]
            self.r_Sd = [Res("Sd%d" % k) for k in range(2)]
            self.r_Sdb = [Res("Sdb%d" % k) for k in range(2)]
            self.r_ctail = [Res("ct%d" % k) for k in range(2)]
            xin = [self.arena[:, k * 2048:(k + 1) * 2048].bitcast(F32) for k in range(2)]
            r_xin = [Res("xin%d" % k) for k in range(2)]
            rc = self.r_c
            P.dma("sp", lambda e: e.dma_start(out=self.cf, in_=cf_d), writes=[rc], key="c0")
            P.dma("sp", lambda e: e.dma_start(out=self.sm, in_=sm_d[:, 0:S_WZ]), writes=[rc], key="c0")
            P.dma("pool", lambda e: e.dma_start(out=self.cb, in_=cb_d), writes=[rc], key="c1")
            P.dma("pool", lambda e: e.dma_start(out=self.smb[:, 0:256], in_=sm_d[:, S_WZ:S_WZ + 256]), writes=[rc], key="c1")
            P.dma("pool", lambda e: e.dma_start(out=self.smb[:, 256:512], in_=sm_d[:, S_WAB:S_WAB + 256]), writes=[rc], key="c1")
            P.dma("pool", lambda e: e.dma_start(out=self.wgkb, in_=sm_d[0:16, S_WGK:S_WGK + 1024]), writes=[rc], key="c1")
            P.dma("pool", lambda e: e.dma_start(out=self.bgkb, in_=sm_d[0:1, S_BGK:S_BGK + 1024]), writes=[rc], key="c1")
            self.memset("dve", self.epsb[:], EPS, w=[rc])
            self.memset("dve", self.oneb[:], 1.0, w=[rc])
            for k in range(4):
                self.cp("dve", self.id4[:, k, :], self.cb[:, B_ID:B_ID + 128], r=[rc], w=[rc])
            self.act(self.negA, self.sm[:, S_ALOG:S_ALOG + 16], AF.Exp, r=[rc], w=[rc])
            self.ts("dve", self.negA, self.negA, -1.0, None, ALU.mult, r=[rc], w=[rc])
            self.r_wkb = [Res("wkb%d" % l) for l in range(depth)]
            bi = 0
            for l in range(depth):
                for k in range(self.nblk_layer[l]):
                    src, dst = wk_d[bi], self.wkb[bi]
                    P.dma("pool", lambda e, s_=src, d_=dst: e.dma_start(out=d_, in_=s_), writes=[self.r_wkb[l]], key="cwk%d" % l)
                    bi += 1
            idf = self.cf[:, C_ID:C_ID + 128]
            r_yout = [Res("yout%d" % k) for k in range(2)]
            for s in range(nseq):
                for t in range(ntile):
                    tok0 = (s * ntile + t) * T
                    self.wk_next = 0
                    self.arena_switch()
                    for b in range(4):
                        xi, rxi = xin[b % 2], r_xin[b % 2]
                        src = x_d[tok0 + b * 128:tok0 + (b + 1) * 128, :]
                        P.dma("sp", lambda e, s_=src, d_=xi: e.dma_start(out=d_, in_=s_), reads=[self.r_arena], writes=[rxi], key="xin%d" % (b % 2))
                        for half in range(2):
                            pt, rt = self.bank()
                            for c4 in range(4):
                                c = half * 4 + c4
                                self.tr(pt[:, c4 * 128:(c4 + 1) * 128], xi[:, c * 128:(c + 1) * 128], idf, r=[rxi, rc, self.r_arena], w=[rt])
                            self.cp("act" if half == 0 else "dve", self.xT[:, half * 4:half * 4 + 4, b * 128:(b + 1) * 128],
                                    pt.rearrange("p (a b) -> p a b", a=4), r=[rt], w=[self.r_x])
                    for l in range(depth):
                        j = l // 2
                        self.ffn(l, 0)
                        if self.stop_after == (l, 0):
                            break
                        if _layer_is_gla(l):
                            self.gla(l, j, t == 0)
                        else:
                            self.gdn(l, j, t == 0)
                        if self.stop_after == (l, 1):
                            break
                        self.ffn(l, 1)
                        if self.stop_after == (l, 2):
                            break
                    if self.stop_after is None:
                        self.rmsnorm_final()
                        src_T = self.f32a[:, 0:4096].rearrange("p (a b) -> p a b", a=8)
                        r_src = self.r_av[23]
                    else:
                        self.arena_switch()
                        src_T, r_src = self.xT, self.r_x
                    for b in range(4):
                        yo, ryo = xin[b % 2], r_xin[b % 2]
                        for half in range(2):
                            pt, rt = self.bank()
                            for c4 in range(4):
                                c = half * 4 + c4
                                self.tr(pt[:, c4 * 128:(c4 + 1) * 128], src_T[:, c, b * 128:(b + 1) * 128], idf, r=[r_src, rc], w=[rt])
                            self.cp("act" if half == 0 else "dve", yo[:, half * 512:(half + 1) * 512], pt, r=[rt, self.r_arena], w=[ryo])
                        dst = y_d[tok0 + b * 128:tok0 + (b + 1) * 128, :]
                        P.dma("sp", lambda e, s_=yo, d_=dst: e.dma_start(out=d_, in_=s_), reads=[ryo, self.r_arena], writes=[], key="xin%d" % (b % 2))
            P.emit()
        return nc

    def rmsnorm_final(self):
        xT, sq = self.xT, self.sq
        self.arena_switch()
        outT = self.f32a[:, 0:4096].rearrange("p (a b) -> p a b", a=8)
        r_out = self.r_av[23]
        pb, rb = self.bank()
        for c in range(8):
            self.act(sq[:, c % 4, :], xT[:, c, :], AF.Square, r=[self.r_x], w=[self.r_sqs[c % 4]])
            self.mm(pb, self.cb[:, B_O1024:B_O1024 + 128], sq[:, c % 4, :], start=(c == 0), stop=(c == 7), r=[self.r_sqs[c % 4], self.r_c], w=[rb])
        rstd = self.rstd
        self.act(rstd, pb, AF.Ln, r=[rb, self.r_c], w=[self.r_rstd], bias=self.epsb[:])
        self.act(rstd, rstd, AF.Exp, r=[self.r_rstd], w=[self.r_rstd], scale=-0.5)
        for c in range(8):
            self.stt("dve", outT[:, c, :], xT[:, c, :], self.sm[:, S_FNORM + c:S_FNORM + c + 1], rstd, ALU.mult, ALU.mult,
                     r=[self.r_x, self.r_rstd, self.r_c, self.r_arena], w=[r_out])


_CACHE = {}


def kernel(**inputs):
    inp = {k: np.asarray(v) for k, v in inputs.items()}
    depth, ncore = 4, 8
    wk, sm = _prep_weights(inp, depth)
    cf, cb = _consts()
    x = np.ascontiguousarray(inp["x"], dtype=np.float32).reshape(ncore, 2 * SEQ, D)
    if "nc" not in _CACHE:
        _CACHE["nc"] = Builder(depth=depth, nseq=2, ntile=SEQ // T).build()
    nc = _CACHE["nc"]
    in_maps = [{"x": x[c], "wk": wk, "sm": sm, "cf": cf, "cb": cb} for c in range(ncore)]
    res = run_bass_kernel_spmd(nc, in_maps, core_ids=list(range(ncore)))
    y = np.stack([np.asarray(res.results[c]["y"]) for c in range(ncore)])
    return y.reshape(16, SEQ, D).astype(np.float32)
```

```python
import contextlib
import numpy as np
import concourse.bass as bass
import concourse.mybir as mybir
from concourse.bass_utils import run_bass_kernel_spmd

F32 = mybir.dt.float32
BF16 = mybir.dt.bfloat16
AF = mybir.ActivationFunctionType
ALU = mybir.AluOpType

D = 1024
SEQ = 4096
T = 512
EPS = 1e-6
DFF = 2816
ENGS = ("pe", "act", "dve", "pool", "sp")
EPOCH = 30000


class Res:
    __slots__ = ("name", "last_w", "readers", "parent", "strict")

    def __init__(self, name, parent=None, strict=False):
        self.name = name
        self.last_w = None
        self.readers = []
        self.parent = parent
        self.strict = strict


class Op:
    __slots__ = ("eng", "fn", "waits", "signal", "is_dma", "key", "sig")

    def __init__(self, eng, fn, is_dma=False, key=None):
        self.eng = eng
        self.fn = fn
        self.waits = []
        self.signal = False
        self.is_dma = is_dma
        self.key = key
        self.sig = None


class Prog:
    def __init__(self, nc):
        self.nc = nc
        self.ops = {e: [] for e in ENGS}
        self.all_ops = []
        self.dma_keys = {}

    def _dep(self, op, prod, strict=False):
        if prod is None or prod is op:
            return
        if prod.eng == op.eng and not prod.is_dma and not strict:
            return
        op.waits.append(prod)
        prod.signal = True

    def _track(self, op, reads, writes):
        extra = [r.parent for r in list(reads) + list(writes) if r.parent is not None]
        if extra:
            reads = list(reads) + [p for p in extra if p not in reads and p not in writes]
        for r in reads:
            self._dep(op, r.last_w, r.strict)
        for w in writes:
            self._dep(op, w.last_w, w.strict)
            for rd in w.readers:
                self._dep(op, rd)
        for r in reads:
            if not op.is_dma:
                r.readers = [x for x in r.readers if x.is_dma or x.eng != op.eng]
            r.readers.append(op)
        for w in writes:
            w.last_w = op
            w.readers = []

    def op(self, eng, fn, reads=(), writes=()):
        o = Op(eng, fn)
        self._track(o, reads, writes)
        self.ops[eng].append(o)
        self.all_ops.append(o)
        return o

    def dma(self, eng, fn, reads=(), writes=(), key="d"):
        o = Op(eng, fn, is_dma=True, key=key)
        o.signal = True
        self._track(o, reads, writes)
        self.ops[eng].append(o)
        self.all_ops.append(o)
        self.dma_keys.setdefault(key, 0)
        return o

    def emit(self):
        nc = self.nc
        cnt = {e: 0 for e in ENGS}
        dcnt = {k: 0 for k in self.dma_keys}
        for o in self.all_ops:
            if o.is_dma:
                dcnt[o.key] += 16
                o.sig = ("dma", o.key, dcnt[o.key])
            elif o.signal:
                cnt[o.eng] += 1
                ep, v = divmod(cnt[o.eng] - 1, EPOCH)
                o.sig = ("eng", (o.eng, ep), v + 1)
        sem_names = []
        for e in ENGS:
            for ep in range(max(1, (cnt[e] + EPOCH - 1) // EPOCH)):
                sem_names.append(("eng", (e, ep)))
        for k in self.dma_keys:
            sem_names.append(("dma", k))
        with contextlib.ExitStack() as st:
            sems = {}
            for i, sn in enumerate(sem_names):
                sems[sn] = st.enter_context(nc.semaphore("s%d" % i))
            block = st.enter_context(nc.Block())
            prog = self

            def make(ename):
                def body(eng):
                    waited = {}
                    for o in prog.ops[ename]:
                        need = {}
                        for p in o.waits:
                            kind, k, v = p.sig
                            sn = (kind, k)
                            if need.get(sn, 0) < v:
                                need[sn] = v
                        for sn, v in need.items():
                            if waited.get(sn, 0) >= v:
                                continue
                            eng.wait_ge(sems[sn], v)
                            waited[sn] = v
                        ins = o.fn(eng)
                        if o.is_dma:
                            ins.then_inc(sems[("dma", o.key)], 16)
                        elif o.signal:
                            ins.then_inc(sems[(o.sig[0], o.sig[1])], 1)
                    if ename == "sp":
                        for k, tot in dcnt.items():
                            if tot > 0 and waited.get(("dma", k), 0) < tot:
                                eng.wait_ge(sems[("dma", k)], tot)
                return body

            block.tensor(make("pe"))
            block.scalar(make("act"))
            block.vector(make("dve"))
            block.gpsimd(make("pool"))
            block.sync(make("sp"))


C_ID, C_LI, C_US, C_LS, C_CE, C_CO, C_ONE = 0, 128, 256, 384, 512, 640, 768
NCF = 896
B_ID, B_LI, B_LS4, B_TRB, B_TR2, B_O1024, B_O256, B_O1, B_LI4 = 0, 128, 256, 768, 896, 1024, 1152, 1280, 1408
NCB = 1920

S_NORM = 0
S_FNORM = 96
S_GLAN = 104
S_CONV = 108
S_ALOG = 300
S_DTB = 316
S_GDNN = 332
S_WZ = 588
S_WAB = 844
S_WGK = 1100
S_BGK = 2124
NSM = 3148


def _consts():
    cf = np.zeros((128, NCF), np.float32)
    m = np.arange(128)
    same = (m[:, None] // 64) == (m[None, :] // 64)
    li = ((m[:, None] <= m[None, :]) & same).astype(np.float32)
    ls = ((m[:, None] < m[None, :]) & same).astype(np.float32)
    us = ((m[:, None] > m[None, :]) & same).astype(np.float32)
    cf[:, C_ID:C_ID + 128] = np.eye(128)
    cf[:, C_LI:C_LI + 128] = li
    cf[:, C_US:C_US + 128] = us
    cf[:, C_LS:C_LS + 128] = ls
    cf[:, C_CE:C_CE + 128] = (m[:, None] < 64).astype(np.float32) * np.ones((1, 128), np.float32)
    cf[:, C_CO:C_CO + 128] = (m[:, None] >= 64).astype(np.float32) * np.ones((1, 128), np.float32)
    cf[:, C_ONE:C_ONE + 128] = 1.0
    cb = np.zeros((128, NCB), np.float32)
    cb[:, B_ID:B_ID + 128] = np.eye(128)
    cb[:, B_LI:B_LI + 128] = li
    cb[:, B_LS4:B_LS4 + 512] = np.tile(ls, (1, 4))
    cb[:, B_TRB:B_TRB + 128] = -li / 16.0
    cb[:, B_TR2:B_TR2 + 128] = -us / 16.0
    cb[:, B_O1024:B_O1024 + 128] = 1.0 / 1024.0
    cb[:, B_O256:B_O256 + 128] = 1.0 / 256.0
    cb[:, B_O1:B_O1 + 128] = 1.0
    cb[:, B_LI4:B_LI4 + 512] = np.tile(li, (1, 4))
    return cf, cb


def _kblocks(W, col_lists):
    out = []
    for cols in col_lists:
        sub = W[:, cols]
        out.append(sub.reshape(8, 128, 512).transpose(1, 0, 2).reshape(128, 4096))
    return out


_SWAP = [False]


def _layer_is_gla(l):
    return (l % 2 == 0) != _SWAP[0]


def _prep_weights(inp, depth):
    wk = []
    ar = np.arange
    for l in range(depth):
        j = l // 2
        for which in range(2):
            if which == 1:
                pass
        def ffn_blocks(i):
            W = inp["ffn_w_in"][l, i]
            lists = [np.concatenate([ar(b * 256, (b + 1) * 256), DFF + ar(b * 256, (b + 1) * 256)]) for b in range(11)]
            wk.extend(_kblocks(W, lists))
            Wo = inp["ffn_w_out"][l, i]
            for half in range(2):
                for g in range(3):
                    nf = 8 if g < 2 else 6
                    sub = Wo[g * 1024:g * 1024 + nf * 128, half * 512:(half + 1) * 512]
                    blk = np.zeros((128, 8, 512), np.float32)
                    blk[:, 0:nf, :] = sub.reshape(nf, 128, 512).transpose(1, 0, 2)
                    wk.append(blk.reshape(128, 4096))
        ffn_blocks(0)
        if _layer_is_gla(l):
            W = inp["gla_w_in"][j]
            lists = [ar(512, 1024), ar(0, 512), ar(1024, 1536), ar(1536, 2048), ar(2048, 2560), ar(2560, 3072)]
            wk.extend(_kblocks(W, lists))
            wk.extend(_kblocks(inp["gla_w_out"][j], [ar(0, 512), ar(512, 1024)]))
        else:
            W = inp["gdn_w_in"][j]
            lists = [ar(b * 512, (b + 1) * 512) for b in range(8)]
            wk.extend(_kblocks(W, lists))
            wk.extend(_kblocks(inp["gdn_w_out"][j], [ar(0, 512), ar(512, 1024)]))
        ffn_blocks(1)
    sm = np.zeros((128, NSM), np.float32)
    sm[:, S_NORM:S_NORM + 96] = inp["norm_w"].reshape(12, 8, 128).transpose(2, 0, 1).reshape(128, 96)
    sm[:, S_FNORM:S_FNORM + 8] = inp["final_norm_w"].reshape(8, 128).T
    sm[:, S_GLAN:S_GLAN + 4] = inp["gla_norm_w"].reshape(2, 2, 128).transpose(2, 0, 1).reshape(128, 4)
    sm[:, S_CONV:S_CONV + 192] = inp["gdn_conv_w"].reshape(2, 4, 24, 128).transpose(3, 0, 1, 2).reshape(128, 192)
    sm[:, S_ALOG:S_ALOG + 16] = inp["gdn_a_log"].reshape(1, 16)
    sm[:, S_DTB:S_DTB + 16] = inp["gdn_dt_bias"].reshape(1, 16)
    sm[:, S_GDNN:S_GDNN + 256] = inp["gdn_norm_w"].reshape(1, 256)
    for j in range(2):
        wz = inp["gla_w_in"][j][:, 3072:3088]
        sm[:, S_WZ + j * 128:S_WZ + (j + 1) * 128] = wz.reshape(8, 128, 16).transpose(1, 0, 2).reshape(128, 128)
        wab = inp["gdn_w_in"][j][:, 4096:4112]
        sm[:, S_WAB + j * 128:S_WAB + (j + 1) * 128] = wab.reshape(8, 128, 16).transpose(1, 0, 2).reshape(128, 128)
        sm[0:16, S_WGK + j * 512:S_WGK + (j + 1) * 512] = inp["gla_w_gk"][j]
        sm[0:1, S_BGK + j * 512:S_BGK + (j + 1) * 512] = inp["gla_b_gk"][j][None, :]
    return np.ascontiguousarray(np.stack(wk)), sm


class Builder:
    def __init__(self, depth=4, nseq=2, ntile=8, stop_after=None):
        self.depth, self.nseq, self.ntile = depth, nseq, ntile
        self.stop_after = stop_after
        self.nblk_layer = [42 if _layer_is_gla(l) else 44 for l in range(depth)]
        self.NB = sum(self.nblk_layer)

    def mm(self, out, lhsT, rhs, start=True, stop=True, r=(), w=()):
        self.P.op("pe", lambda e: e.matmul(out, lhsT, rhs, start=start, stop=stop, skip_group_check=True), reads=r, writes=w)

    def tr(self, out, in_, ident, r=(), w=()):
        self.P.op("pe", lambda e: e.transpose(out, in_, ident), reads=r, writes=w)

    def act(self, out, in_, func, r=(), w=(), bias=None, scale=None, accum=None, eng="act"):
        kw = {}
        if bias is not None:
            kw["bias"] = bias
        if scale is not None:
            kw["scale"] = scale
        if accum is not None:
            kw["accum_out"] = accum
        self.P.op("act", lambda e: e.activation(out=out, in_=in_, func=func, **kw), reads=r, writes=w)

    def tt(self, eng, out, in0, in1, op, r=(), w=()):
        self.P.op(eng, lambda e: e.tensor_tensor(out=out, in0=in0, in1=in1, op=op), reads=r, writes=w)

    def ts(self, eng, out, in0, s1, s2, op0, op1=None, r=(), w=()):
        if op1 is None:
            self.P.op(eng, lambda e: e.tensor_scalar(out=out, in0=in0, scalar1=s1, scalar2=None, op0=op0), reads=r, writes=w)
        else:
            self.P.op(eng, lambda e: e.tensor_scalar(out=out, in0=in0, scalar1=s1, scalar2=s2, op0=op0, op1=op1), reads=r, writes=w)

    def stt(self, eng, out, in0, scalar, in1, op0, op1, r=(), w=()):
        self.P.op(eng, lambda e: e.scalar_tensor_tensor(out=out, in0=in0, scalar=scalar, in1=in1, op0=op0, op1=op1), reads=r, writes=w)

    def cp(self, eng, out, in_, r=(), w=()):
        if eng == "act":
            self.P.op("act", lambda e: e.activation(out=out, in_=in_, func=AF.Copy), reads=r, writes=w)
        else:
            self.P.op(eng, lambda e: e.tensor_copy(out=out, in_=in_), reads=r, writes=w)

    def memset(self, eng, ap, val, w=()):
        self.P.op(eng, lambda e: e.memset(ap, val), writes=w)

    def bank(self, pool=None):
        pool = pool or (0, 1, 2, 3, 4, 5, 6, 7)
        k = self._bctr.get(pool, 0)
        self._bctr[pool] = k + 1
        b = pool[k % len(pool)]
        return self.pbank[b], self.rbank[b]

    def load_wk(self):
        i = self.wk_i
        self.wk_i += 1
        slot = i % self.NWK
        blk = self.wk_next
        self.wk_next += 1
        src = self.wkb[blk]
        dst = self.wring[slot]
        layer_res = self.r_wkb[self.blk_layer[blk]]
        self.P.dma("sp", lambda e: e.dma_start(out=dst, in_=src), reads=[layer_res], writes=[self.r_wring[slot]], key="wk%d" % slot)
        return self.wring3[slot], self.r_wring[slot]

    def arena_switch(self):
        cell = self.cell
        self.P.op("dve", lambda e: e.memset(cell[:], 0.0), writes=[self.r_arena])

    def rmsnorm(self, wcol):
        xT, hT, sq = self.xT, self.hT, self.sq
        pb, rb = self.bank()
        for c in range(8):
            self.act(sq[:, c % 4, :], xT[:, c, :], AF.Square, r=[self.r_x], w=[self.r_sqs[c % 4]])
            self.mm(pb, self.cb[:, B_O1024:B_O1024 + 128], sq[:, c % 4, :], start=(c == 0), stop=(c == 7), r=[self.r_sqs[c % 4], self.r_c], w=[rb])
        rstd = self.rstd
        self.act(rstd, pb, AF.Ln, r=[rb, self.r_c], w=[self.r_rstd], bias=self.epsb[:])
        self.act(rstd, rstd, AF.Exp, r=[self.r_rstd], w=[self.r_rstd], scale=-0.5)
        for c in range(8):
            self.stt("dve", hT[:, c, :], xT[:, c, :], self.sm[:, wcol + c:wcol + c + 1], rstd, ALU.mult, ALU.mult,
                     r=[self.r_x, self.r_rstd, self.r_c], w=[self.r_h])

    def ffn(self, l, i):
        self.rmsnorm(S_NORM + (l * 3 + 2 * i) * 8)
        self.arena_switch()
        A = self.arena
        actb = A[:, 0:22 * 512].rearrange("p (a b) -> p a b", a=22)
        ra = [self.r_arena]
        r_act = self.r_av[0]
        hT = self.hT
        gu = (4, 5, 6, 7)
        for jb in range(11):
            w, rw = self.load_wk()
            for half in range(2):
                fc = jb * 2 + half
                pg, rg = self.bank(gu)
                pu, ru = self.bank(gu)
                for kc in range(8):
                    self.mm(pg, w[:, kc, half * 128:(half + 1) * 128], hT[:, kc, :], start=(kc == 0), stop=(kc == 7), r=[rw, self.r_h], w=[rg])
                for kc in range(8):
                    self.mm(pu, w[:, kc, 256 + half * 128:256 + (half + 1) * 128], hT[:, kc, :], start=(kc == 0), stop=(kc == 7), r=[rw, self.r_h], w=[ru])
                sg, rsg = self.sgbuf[fc % 2], self.r_sg[fc % 2]
                self.act(sg, pg, AF.Silu, r=[rg], w=[rsg])
                self.tt("dve", actb[:, fc, :], sg, pu, ALU.mult, r=[rsg, ru] + ra, w=[r_act])
        for half in range(2):
            pool = (0, 1, 2, 3) if half == 0 else (4, 5, 6, 7)
            bks = [self.bank(pool) for _ in range(4)]
            for g in range(3):
                wo, rwo = self.load_wk()
                for f in range(8 if g < 2 else 6):
                    fc = g * 8 + f
                    for d in range(4):
                        self.mm(bks[d][0], wo[:, f, d * 128:(d + 1) * 128], actb[:, fc, :], start=(fc == 0), stop=(fc == 21),
                                r=[rwo, r_act] + ra, w=[bks[d][1]])
            for d in range(4):
                dc = half * 4 + d
                self.stt("dve", self.xT[:, dc, :], bks[d][0], 0.5, self.xT[:, dc, :], ALU.mult, ALU.add, r=[bks[d][1], self.r_x], w=[self.r_x])

    def gla(self, l, j, first_tile):
        self.rmsnorm(S_NORM + (l * 3 + 1) * 8)
        self.arena_switch()
        A, ra = self.arena, [self.r_arena]
        hT, cb, cf = self.hT, self.cb, self.cf
        o = 0

        def carve(n, shape3=None):
            nonlocal o
            v = A[:, o:o + n]
            o += n
            return v
        qd = carve(2048).rearrange("p (a b) -> p a b", a=4)
        ki = carve(2048).rearrange("p (a b) -> p a b", a=4)
        ke = carve(2048).rearrange("p (a b) -> p a b", a=4)
        vt = carve(4096).rearrange("p (a b) -> p a b", a=4)
        lg = carve(2048).rearrange("p (a b) -> p a b", a=4)
        rs = carve(4096).rearrange("p (a b) -> p a b", a=8)
        og = carve(4096).rearrange("p (a b) -> p a b", a=8)
        zT = carve(512)
        AT = carve(1024).rearrange("p (a b) -> p a b", a=2)
        r_qd, r_ki, r_ke, r_vt, r_lg, r_rs, r_og, r_zT, r_oT, r_eb = self.r_av[0:10]
        r_AT = self.r_av[10:12]
        oT = self.f32a[:, 0:4096].rearrange("p (a b) -> p a b", a=8)
        eb = self.f32a[:, 4096:6144].rearrange("p (a b) -> p a b", a=4)
        S, Sb = self.Sg[j], self.Sgb[j]
        rS, rSb = self.r_Sg[j], self.r_Sgb[j]
        sm = self.sm
        if first_tile:
            self.memset("pool", S[:], 0.0, w=[rS])
            self.memset("pool", Sb[:], 0.0, w=[rSb])
        pz, rz = self.bank()
        for kc in range(8):
            self.mm(pz[0:16, :], self.smb[:, j * 128 + kc * 16:j * 128 + kc * 16 + 16], hT[:, kc, :], start=(kc == 0), stop=(kc == 7),
                    r=[self.r_h, self.r_c], w=[rz])
        self.cp("act", zT[0:16, :], pz[0:16, :], r=[rz] + ra, w=[r_zT])
        for b in range(4):
            pl, rl = self.bank()
            self.mm(pl, zT[0:16, b * 128:(b + 1) * 128], self.wgkb[0:16, j * 512:(j + 1) * 512], start=True, stop=False, r=[r_zT, self.r_c] + ra, w=[rl])
            self.mm(pl, self.cb[0:1, B_O1:B_O1 + 128], self.bgkb[0:1, j * 512:(j + 1) * 512], start=False, stop=True, r=[self.r_c], w=[rl])
            t0, rt0 = self.tmpf[b % 2], self.r_tmpf[b % 2]
            self.act(t0, pl, AF.Exp, r=[rl], w=[rt0], scale=-1.0)
            self.act(lg[:, b, :], t0, AF.Ln, r=[rt0, self.r_c] + ra, w=[r_lg], bias=self.oneb[:])
        w, rw = self.load_wk()
        for b in range(4):
            pk, rk = self.bank()
            for kc in range(8):
                self.mm(pk, hT[:, kc, b * 128:(b + 1) * 128], w[:, kc, :], start=(kc == 0), stop=(kc == 7), r=[rw, self.r_h], w=[rk])
            pd, rd = self.bank()
            self.mm(pd, cb[:, B_TR2:B_TR2 + 128], lg[:, b, :], r=[r_lg, self.r_c] + ra, w=[rd])
            t0, rt0 = self.tmpf[b % 2], self.r_tmpf[b % 2]
            self.act(t0, pd, AF.Exp, r=[rd], w=[rt0])
            self.tt("dve", ke[:, b, :], pk, t0, ALU.mult, r=[rk, rt0] + ra, w=[r_ke])
        for h in range(4):
            pk, rk = self.bank()
            for kc in range(8):
                self.mm(pk, w[:, kc, h * 128:(h + 1) * 128], hT[:, kc, :], start=(kc == 0), stop=(kc == 7), r=[rw, self.r_h], w=[rk])
            pb, rb = self.bank()
            for b in range(4):
                self.mm(pb[:, b * 128:(b + 1) * 128], lg[:, b, h * 128:(h + 1) * 128], cb[:, B_TRB:B_TRB + 128], r=[r_lg, self.r_c] + ra, w=[rb])
            t0, rt0 = self.tmpf[h % 2], self.r_tmpf[h % 2]
            self.act(t0, pb, AF.Exp, r=[rb], w=[rt0], scale=-1.0)
            self.act(eb[:, h, :], pb, AF.Exp, r=[rb], w=[r_eb])
            self.tt("dve", ki[:, h, :], pk, t0, ALU.mult, r=[rk, rt0] + ra, w=[r_ki])
        w, rw = self.load_wk()
        for h in range(4):
            pq, rq = self.bank()
            for kc in range(8):
                self.mm(pq, w[:, kc, h * 128:(h + 1) * 128], hT[:, kc, :], start=(kc == 0), stop=(kc == 7), r=[rw, self.r_h], w=[rq])
            self.stt("dve", qd[:, h, :], pq, 128.0 ** -0.5, eb[:, h, :], ALU.mult, ALU.mult, r=[rq, r_eb] + ra, w=[r_qd])
        for vb in range(2):
            w, rw = self.load_wk()
            for b in range(4):
                pv, rv = self.bank()
                for kc in range(8):
                    self.mm(pv, hT[:, kc, b * 128:(b + 1) * 128], w[:, kc, :], start=(kc == 0), stop=(kc == 7), r=[rw, self.r_h], w=[rv])
                self.cp("act" if b % 2 == 0 else "dve", vt[:, b, vb * 512:(vb + 1) * 512], pv, r=[rv] + ra, w=[r_vt])
        for rb_ in range(2):
            w, rw = self.load_wk()
            for cc in range(4):
                pr, rr = self.bank()
                for kc in range(8):
                    self.mm(pr, w[:, kc, cc * 128:(cc + 1) * 128], hT[:, kc, :], start=(kc == 0), stop=(kc == 7), r=[rw, self.r_h], w=[rr])
                self.act(rs[:, rb_ * 4 + cc, :], pr, AF.Silu, r=[rr] + ra, w=[r_rs])
        SP = (3, 4, 5, 6, 7)
        for b in range(4):
            bc = slice(b * 128, (b + 1) * 128)
            pA, rA = self.bank((0,))
            for h in range(4):
                self.mm(pA[:, h * 128:(h + 1) * 128], ki[:, h, bc], qd[:, h, bc], r=[r_ki, r_qd] + ra, w=[rA])
            at, rat = AT[:, b % 2, :], r_AT[b % 2]
            self.tt("dve", at, pA, cb[:, B_LI4:B_LI4 + 512], ALU.mult, r=[rA, self.r_c] + ra, w=[rat])
            po = [self.bank((1,)), self.bank((2,))]
            for pob, rob in po:
                self.memset("dve", pob, 0.0, w=[rob])
            for e in range(2):
                cs = slice(b * 128 + e * 64, b * 128 + (e + 1) * 64)
                last = b * 128 + e * 64 + 63
                for h in range(4):
                    pob, rob = po[h // 2]
                    for half in range(2):
                        c0 = (h % 2) * 256 + half * 128 + e * 64
                        self.mm(pob[:, c0:c0 + 64], Sb[:, h, half * 128:(half + 1) * 128], qd[:, h, cs], start=False, stop=False,
                                r=[rSb, r_qd] + ra, w=[rob])
                    pS, rpS = self.bank(SP)
                    self.mm(pS[:, 0:256], ke[e * 64:(e + 1) * 64, b, h * 128:(h + 1) * 128], vt[e * 64:(e + 1) * 64, b, h * 256:(h + 1) * 256],
                            r=[r_ke, r_vt] + ra, w=[rpS])
                    self.stt("dve", S[:, h, :], S[:, h, :], eb[:, h, last:last + 1], pS[:, 0:256], ALU.mult, ALU.add, r=[rS, r_eb, rpS], w=[rS])
                    self.cp("act", Sb[:, h, :], S[:, h, :], r=[rS], w=[rSb])
            for h in range(4):
                pob, rob = po[h // 2]
                for half in range(2):
                    c0 = (h % 2) * 256 + half * 128
                    self.mm(pob[:, c0:c0 + 128], vt[:, b, h * 256 + half * 128:h * 256 + (half + 1) * 128], at[:, h * 128:(h + 1) * 128],
                            start=False, stop=True, r=[r_vt, rat] + ra, w=[rob])
            for h2 in range(2):
                pob, rob = po[h2]
                self.cp("act", oT[:, h2 * 4:(h2 + 1) * 4, bc], pob.rearrange("p (a b) -> p a b", a=4), r=[rob], w=[r_oT])
        sq = self.sq
        for h in range(4):
            pn, rn = self.bank()
            for half in range(2):
                k4 = (h * 2 + half) % 4
                self.act(sq[:, k4, :], oT[:, h * 2 + half, :], AF.Square, r=[r_oT], w=[self.r_sqs[k4]])
                self.mm(pn, cb[:, B_O256:B_O256 + 128], sq[:, k4, :], start=(half == 0), stop=(half == 1), r=[self.r_sqs[k4], self.r_c], w=[rn])
            t0, rt0 = self.tmpf[h % 2], self.r_tmpf[h % 2]
            self.act(t0, pn, AF.Ln, r=[rn, self.r_c], w=[rt0], bias=self.epsb[:])
            self.act(t0, t0, AF.Exp, r=[rt0], w=[rt0], scale=-0.5)
            for half in range(2):
                c = h * 2 + half
                self.stt("dve", oT[:, c, :], oT[:, c, :], sm[:, S_GLAN + j * 2 + half:S_GLAN + j * 2 + half + 1], t0, ALU.mult, ALU.mult,
                         r=[r_oT, rt0, self.r_c], w=[r_oT])
                self.tt("dve", og[:, c, :], oT[:, c, :], rs[:, c, :], ALU.mult, r=[r_oT, r_rs] + ra, w=[r_og])
        self.out_proj(og, r_og, ra)

    def out_proj(self, og, r_og, ra):
        for blk in range(2):
            w, rw = self.load_wk()
            for dq in range(4):
                dc = blk * 4 + dq
                py, ry = self.bank()
                for vc in range(8):
                    self.mm(py, w[:, vc, dq * 128:(dq + 1) * 128], og[:, vc, :], start=(vc == 0), stop=(vc == 7), r=[rw, r_og] + ra, w=[ry])
                self.tt("dve", self.xT[:, dc, :], py, self.xT[:, dc, :], ALU.add, r=[ry, self.r_x], w=[self.r_x])

    def gdn(self, l, j, first_tile):
        self.rmsnorm(S_NORM + (l * 3 + 1) * 8)
        self.arena_switch()
        A, ra = self.arena, [self.r_arena]
        hT, cb, cf, sm = self.hT, self.cb, self.cf, self.sm
        o = 0

        def carve(n):
            nonlocal o
            v = A[:, o:o + n]
            o += n
            return v
        xc2 = carve(2 * 516).rearrange("p (a b) -> p a b", a=2)
        qkv = carve(24 * 512).rearrange("p (a b) -> p a b", a=24)
        og = carve(4096).rearrange("p (a b) -> p a b", a=8)
        ogt = carve(1024)
        ktok = carve(1024).rearrange("p (a b) -> p a b", a=8)
        kdtok = carve(1024).rearrange("p (a b) -> p a b", a=8)
        vtok = carve(1024).rearrange("p (a b) -> p a b", a=8)
        Pm = [carve(1024).rearrange("p (a b) -> p a b", a=8) for _ in range(2)]
        PmT = [carve(1024).rearrange("p (a b) -> p a b", a=8) for _ in range(2)]
        Rm = [carve(1024).rearrange("p (a b) -> p a b", a=8) for _ in range(2)]
        qkT = carve(1024).rearrange("p (a b) -> p a b", a=8)
        w0T = carve(1024).rearrange("p (a b) -> p a b", a=8)
        vnew = carve(1024).rearrange("p (a b) -> p a b", a=8)
        (r_xc0, r_qkv, r_og, r_ogt, r_kt, r_kd, r_vt, r_P0, r_P1, r_PT0, r_PT1, r_R0, r_R1, r_qk, r_w0, r_vn, r_rt, r_u0, r_sc, r_gu, r_dt) = self.r_av[0:21]
        r_P, r_PT, r_R = [r_P0, r_P1], [r_PT0, r_PT1], [r_R0, r_R1]
        r_sc = self.r_scs
        r_xcs = [r_xc0, self.r_av[21]]
        r_qc = self.r_qc
        f = self.f32a
        rtok = f[:, 0:1024]
        u0 = f[:, 1024:2048].rearrange("p (a b) -> p a b", a=8)
        gu = f[:, 2048:3072].rearrange("p (a b) -> p a b", a=8)
        dtm = f[:, 3072:4096].rearrange("p (a b) -> p a b", a=8)
        sc = f[:, 4096:4096 + 256]
        otok = f[:, 4608:5632].rearrange("p (a b) -> p a b", a=8)
        S, Sb = self.Sd[j], self.Sdb[j]
        rS, rSb = self.r_Sd[j], self.r_Sdb[j]
        ct, rct = self.ctail[j], self.r_ctail[j]
        if first_tile:
            self.memset("pool", S[:], 0.0, w=[rS])
            self.memset("pool", Sb[:], 0.0, w=[rSb])
            self.memset("pool", ct[:], 0.0, w=[rct])
        pab, rab = self.bank()
        for b in range(4):
            for kc in range(8):
                self.mm(pab[:, b * 16:(b + 1) * 16], hT[:, kc, b * 128:(b + 1) * 128], self.smb[:, 256 + j * 128 + kc * 16:256 + j * 128 + kc * 16 + 16],
                        start=(kc == 0), stop=(kc == 7), r=[self.r_h, self.r_c], w=[rab])
        pab3 = pab[:, 0:64].rearrange("p (b c) -> p b c", b=4)
        sc3 = sc.rearrange("p (b c) -> p b c", b=4)
        for b in range(4):
            self.tt("dve", sc3[:, b, 0:8], pab3[:, b, 0:8], sm[:, S_DTB + j * 8:S_DTB + j * 8 + 8], ALU.add, r=[rab, self.r_c], w=[r_sc])
        self.act(sc3[:, :, 0:8], sc3[:, :, 0:8], AF.Exp, r=[r_sc], w=[r_sc])
        self.act(sc3[:, :, 0:8], sc3[:, :, 0:8], AF.Ln, r=[r_sc, self.r_c], w=[r_sc], bias=self.oneb[:])
        for b in range(4):
            self.tt("dve", sc3[:, b, 8:16], sc3[:, b, 0:8], self.negA[:, j * 8:j * 8 + 8], ALU.mult, r=[r_sc, self.r_c], w=[r_sc])
        self.act(sc3[:, :, 16:24], pab3[:, :, 8:16], AF.Exp, r=[rab], w=[r_sc], scale=-1.0)
        self.act(sc3[:, :, 16:24], sc3[:, :, 16:24], AF.Ln, r=[r_sc, self.r_c], w=[r_sc], bias=self.oneb[:])
        self.act(sc3[:, :, 16:24], sc3[:, :, 16:24], AF.Exp, r=[r_sc], w=[r_sc], scale=-1.0)
        pdd, rdd = self.bank()
        for b in range(4):
            g_b = sc3[:, b, 8:16]
            self.mm(pdd[:, b * 16:b * 16 + 8], cf[:, C_LI:C_LI + 128], g_b, r=[r_sc, self.r_c], w=[rdd])
            self.mm(pdd[:, b * 16 + 8:b * 16 + 16], cf[:, C_US:C_US + 128], g_b, r=[r_sc, self.r_c], w=[rdd])
            self.mm(pdd[:, 64 + b * 16:64 + b * 16 + 8], cf[:, C_CE:C_CE + 128], g_b, r=[r_sc, self.r_c], w=[rdd])
            self.mm(pdd[:, 64 + b * 16 + 8:64 + b * 16 + 16], cf[:, C_CO:C_CO + 128], g_b, r=[r_sc, self.r_c], w=[rdd])
        pdd3 = pdd[:, 0:64].rearrange("p (b c) -> p b c", b=4)
        self.act(sc3[:, :, 40:56], pdd3, AF.Exp, r=[rdd], w=[r_sc])
        edl = self.edl
        self.act(edl, pdd[:, 64:128].rearrange("p (b c) -> p b c", b=4), AF.Exp, r=[rdd], w=[self.r_edl])
        t0s = [f[:, k * 512:(k + 1) * 512] for k in range(4)]
        t2s = [f[:, 2048 + k * 512:2048 + (k + 1) * 512] for k in range(4)]
        r_t0s, r_t2s = self.r_cv[0:4], self.r_cv[4:8]
        wcur = [None, None]
        pending, ready = [], []

        def stage_a(cc):
            if cc % 4 == 0:
                wcur[0], wcur[1] = self.load_wk()
            w, rw = wcur
            cq = cc % 4
            pp, rp = self.bank((0, 1, 2, 3))
            for kc in range(8):
                self.mm(pp, w[:, kc, cq * 128:(cq + 1) * 128], hT[:, kc, :], start=(kc == 0), stop=(kc == 7), r=[rw, self.r_h], w=[rp])
            xc, r_xc = xc2[:, cc % 2, :], r_xcs[cc % 2]
            self.cp("pool", xc[:, 0:3], ct[:, cc, :], r=[rct] + ra, w=[r_xc])
            self.cp("act", xc[:, 3:515], pp, r=[rp] + ra, w=[r_xc])
            t0, rt0 = t0s[cc % 4], r_t0s[cc % 4]
            cw = S_CONV + (j * 4) * 24 + cc
            self.ts("dve", t0, xc[:, 3:515], sm[:, cw + 3 * 24:cw + 3 * 24 + 1], None, ALU.mult, r=[r_xc, self.r_c] + ra, w=[rt0])
            for tap in (2, 1, 0):
                self.stt("dve", t0, xc[:, tap:tap + 512], sm[:, cw + tap * 24:cw + tap * 24 + 1], t0, ALU.mult, ALU.add,
                         r=[r_xc, rt0, self.r_c] + ra, w=[rt0])
            self.cp("pool", ct[:, cc, :], xc[:, 512:515], r=[r_xc] + ra, w=[rct])

        def stage_b(cc):
            t0, rt0 = t0s[cc % 4], r_t0s[cc % 4]
            self.act(qkv[:, cc, :], t0, AF.Silu, r=[rt0] + ra, w=[r_qc[cc]])
            if cc < 16:
                k4 = cc % 4
                self.act(self.sq[:, k4, :], qkv[:, cc, :], AF.Square, r=[r_qc[cc]] + ra, w=[self.r_sqs[k4]])
                pn, rn = self.bank((4, 5, 6, 7))
                self.mm(pn, cb[:, B_O1:B_O1 + 128], self.sq[:, k4, :], r=[self.r_sqs[k4], self.r_c], w=[rn])
                pending.append((cc, pn, rn))

        def stage_c(items):
            for k, (cc, pn, rn) in enumerate(items):
                self.act(t2s[k], pn, AF.Ln, r=[rn, self.r_c], w=[r_t2s[k]], bias=self.epsb[:])
            for k, (cc, pn, rn) in enumerate(items):
                self.act(t2s[k], t2s[k], AF.Exp, r=[r_t2s[k]], w=[r_t2s[k]], scale=-0.5)
            for k, (cc, pn, rn) in enumerate(items):
                if cc < 8:
                    self.stt("dve", qkv[:, cc, :], qkv[:, cc, :], 128.0 ** -0.5, t2s[k], ALU.mult, ALU.mult, r=[r_qc[cc], r_t2s[k]] + ra, w=[r_qc[cc]])
                else:
                    self.tt("dve", qkv[:, cc, :], qkv[:, cc, :], t2s[k], ALU.mult, r=[r_qc[cc], r_t2s[k]] + ra, w=[r_qc[cc]])

        for it in range(25):
            if it < 24:
                stage_a(it)
            if ready:
                stage_c(ready)
                ready = []
            if it >= 1:
                stage_b(it - 1)
            if len(pending) == 4:
                ready, pending = pending, []
        if ready:
            stage_c(ready)
        if pending:
            stage_c(pending)
        self.dump("sc", sc, [r_sc], F32)
        self.dump("edl", edl, [self.r_edl], F32)
        self.dump("qkv", qkv, r_qc, BF16)
        wr = [self.load_wk(), self.load_wk()]
        idb = cb[:, B_ID:B_ID + 128]
        for b in range(4):
            bc = slice(b * 128, (b + 1) * 128)
            for rb_ in range(2):
                w, rw = wr[rb_]
                pr, rr = self.bank((6, 7))
                for kc in range(8):
                    self.mm(pr, hT[:, kc, bc], w[:, kc, :], start=(kc == 0), stop=(kc == 7), r=[rw, self.r_h], w=[rr])
                self.act(rtok[:, rb_ * 512:(rb_ + 1) * 512], pr, AF.Silu, r=[rr], w=[r_rt])
            for grp in range(2):
                hs = range(grp * 4, grp * 4 + 4)
                ptk, rtk = self.bank((6, 7))
                ptk_b = self.pbank_bf[self._last_bank_id((6, 7))]
                for h in hs:
                    self.tr(ptk_b[:, (h % 4) * 128:(h % 4 + 1) * 128], qkv[:, 8 + h, bc], idb, r=[r_qc[8 + h], self.r_c] + ra, w=[rtk])
                for h in hs:
                    self.ts("dve", ktok[:, h, :], ptk_b[:, (h % 4) * 128:(h % 4 + 1) * 128], sc3[:, b, 48 + h:49 + h], None, ALU.mult, r=[rtk, r_sc] + ra, w=[r_kt])
                    self.ts("dve", kdtok[:, h, :], ptk_b[:, (h % 4) * 128:(h % 4 + 1) * 128], sc3[:, b, 40 + h:41 + h], None, ALU.mult,
                            r=[rtk, r_sc] + ra, w=[r_kd])
                ptv, rtv = self.bank((6, 7))
                ptv_b = self.pbank_bf[self._last_bank_id((6, 7))]
                for h in hs:
                    self.tr(ptv_b[:, (h % 4) * 128:(h % 4 + 1) * 128], qkv[:, 16 + h, bc], idb, r=[r_qc[16 + h], self.r_c] + ra, w=[rtv])
                self.cp("act", vtok[:, grp * 4:grp * 4 + 4, :], ptv_b[:, 0:512].rearrange("p (a b) -> p a b", a=4), r=[rtv] + ra, w=[r_vt])
            for h in range(8):
                self.ts("dve", gu[:, h, :], cf[:, C_US:C_US + 128], sc3[:, b, 8 + h:9 + h], None, ALU.mult, r=[self.r_c, r_sc], w=[r_gu])
            for grp in range(2):
                pD, rD = self.bank((4, 5))
                for h4 in range(4):
                    h = grp * 4 + h4
                    self.mm(pD[:, h4 * 128:(h4 + 1) * 128], gu[:, h, :], cf[:, C_LI:C_LI + 128], r=[r_gu, self.r_c], w=[rD])
                t0, rt0 = self.tmpf[grp], self.r_tmpf[grp]
                self.act(t0, pD, AF.Exp, r=[rD], w=[rt0])
                self.tt("dve", dtm[:, grp * 4:grp * 4 + 4, :], t0.rearrange("p (a b) -> p a b", a=4),
                        cb[:, B_LI4:B_LI4 + 512].rearrange("p (a b) -> p a b", a=4),
                        ALU.mult, r=[rt0, self.r_c], w=[r_dt])
            for grp in range(2):
                pX, rX = self.bank((4, 5))
                for h4 in range(4):
                    h = grp * 4 + h4
                    self.mm(pX[:, h4 * 128:(h4 + 1) * 128], qkv[:, 8 + h, bc], qkv[:, 8 + h, bc], r=[r_qc[8 + h]] + ra, w=[rX])
                for h4 in range(4):
                    h = grp * 4 + h4
                    self.stt("dve", self.xtmp[:, h4, :], pX[:, h4 * 128:(h4 + 1) * 128], sc3[:, b, 16 + h:17 + h], dtm[:, h, :], ALU.mult, ALU.mult,
                             r=[rX, r_sc, r_dt], w=[self.r_xtmp])
                self.stt("dve", Pm[0][:, grp * 4:grp * 4 + 4, :], self.xtmp[:, :, :], -1.0, cb[:, B_LS4:B_LS4 + 512].rearrange("p (a b) -> p a b", a=4),
                         ALU.mult, ALU.mult, r=[self.r_xtmp, self.r_c] + ra, w=[r_P[0]])
                pQ, rQ = self.bank((4, 5))
                for h4 in range(4):
                    h = grp * 4 + h4
                    self.mm(pQ[:, h4 * 128:(h4 + 1) * 128], qkv[:, 8 + h, bc], qkv[:, h, bc], r=[r_qc[8 + h], r_qc[h]] + ra, w=[rQ])
                self.tt("dve", qkT[:, grp * 4:grp * 4 + 4, :], pQ.rearrange("p (a b) -> p a b", a=4), dtm[:, grp * 4:grp * 4 + 4, :], ALU.mult,
                        r=[rQ, r_dt] + ra, w=[r_qk])
            self.dump("dtm", dtm, [r_dt], F32)
            self.dump("P0", Pm[0], [r_P[0]], BF16)
            self.dump("qkT", qkT, [r_qk], BF16)
            self.dump("ktok", ktok, [r_kt], BF16)
            self.dump("vtok", vtok, [r_vt], BF16)
            for grp in range(2):
                g4 = slice(grp * 4, grp * 4 + 4)
                pT, rT = self.bank((4, 5))
                pT_b = self.pbank_bf[self._last_bank_id((4, 5))]
                for h4 in range(4):
                    self.tr(pT_b[:, h4 * 128:(h4 + 1) * 128], Pm[0][:, grp * 4 + h4, :], idb, r=[r_P[0], self.r_c] + ra, w=[rT])
                self.cp("act", PmT[0][:, g4, :], pT_b[:, 0:512].rearrange("p (a b) -> p a b", a=4), r=[rT] + ra, w=[r_PT[0]])
                self.tt("pool", Rm[0][:, g4, :], Pm[0][:, g4, :], self.id4,
                        ALU.add, r=[r_P[0], self.r_c] + ra, w=[r_R[0]])
            for n in range(1, 6):
                cur, prv = n % 2, (n - 1) % 2
                for grp in range(2):
                    g4 = slice(grp * 4, grp * 4 + 4)
                    if n < 5:
                        pP, rP = self.bank((0, 1, 2, 3))
                        for h4 in range(4):
                            h = grp * 4 + h4
                            self.mm(pP[:, h4 * 128:(h4 + 1) * 128], PmT[prv][:, h, :], Pm[prv][:, h, :], r=[r_PT[prv], r_P[prv]] + ra, w=[rP])
                        self.cp("act", Pm[cur][:, g4, :], pP.rearrange("p (a b) -> p a b", a=4), r=[rP] + ra, w=[r_P[cur]])
                    pPT, rPT = self.bank((0, 1, 2, 3))
                    for h4 in range(4):
                        h = grp * 4 + h4
                        self.mm(pPT[:, h4 * 128:(h4 + 1) * 128], Pm[prv][:, h, :], PmT[prv][:, h, :], r=[r_PT[prv], r_P[prv]] + ra, w=[rPT])
                    self.cp("dve", PmT[cur][:, g4, :], pPT.rearrange("p (a b) -> p a b", a=4), r=[rPT] + ra, w=[r_PT[cur]])
                    pR, rR = self.bank((0, 1, 2, 3))
                    for h4 in range(4):
                        h = grp * 4 + h4
                        self.mm(pR[:, h4 * 128:(h4 + 1) * 128], PmT[cur][:, h, :], Rm[prv][:, h, :], start=True, stop=False, r=[r_PT[cur], r_R[prv]] + ra, w=[rR])
                        self.mm(pR[:, h4 * 128:(h4 + 1) * 128], idb, Rm[prv][:, h, :], start=False, stop=True, r=[self.r_c, r_R[prv]] + ra, w=[rR])
                    self.cp("act" if grp == 0 else "dve", Rm[cur][:, g4, :], pR.rearrange("p (a b) -> p a b", a=4), r=[rR] + ra, w=[r_R[cur]])
            Rf, r_Rf = Rm[1], r_R[1]
            self.dump("Rf", Rf, [r_Rf], BF16)
            for grp in range(2):
                g4 = slice(grp * 4, grp * 4 + 4)
                pU, rU = self.bank((0, 1, 2, 3))
                for h4 in range(4):
                    h = grp * 4 + h4
                    self.mm(pU[:, h4 * 128:(h4 + 1) * 128], Rf[:, h, :], vtok[:, h, :], r=[r_Rf, r_vt] + ra, w=[rU])
                self.cp("act", u0[:, g4, :], pU.rearrange("p (a b) -> p a b", a=4), r=[rU], w=[r_u0])
                pW, rW = self.bank((0, 1, 2, 3))
                for h4 in range(4):
                    h = grp * 4 + h4
                    self.mm(pW[:, h4 * 128:(h4 + 1) * 128], kdtok[:, h, :], Rf[:, h, :], r=[r_Rf, r_kd] + ra, w=[rW])
                self.cp("dve", w0T[:, g4, :], pW.rearrange("p (a b) -> p a b", a=4), r=[rW] + ra, w=[r_w0])
            self.dump("u0", u0, [r_u0], F32)
            self.dump("w0T", w0T, [r_w0], BF16)
            po = [self.bank((4,)), self.bank((5,))]
            for e in range(2):
                ps_ = slice(e * 64, (e + 1) * 64)
                cs = slice(b * 128 + e * 64, b * 128 + (e + 1) * 64)
                for grp in range(2):
                    pws, rws = self.bank((0, 1, 2, 3))
                    for h4 in range(4):
                        h = grp * 4 + h4
                        self.mm(pws[ps_, h4 * 128:(h4 + 1) * 128], w0T[:, h, e * 64:(e + 1) * 64], Sb[:, h, :], r=[r_w0, rSb] + ra, w=[rws])
                        self.mm(po[grp][0][ps_, h4 * 128:(h4 + 1) * 128], qkv[:, h, cs], Sb[:, h, :], r=[r_qc[h], rSb] + ra, w=[po[grp][1]])
                    g4 = slice(grp * 4, grp * 4 + 4)
                    self.tt("dve", self.vtmp[ps_, :, :], u0[ps_, g4, :], pws[ps_, :].rearrange("p (a b) -> p a b", a=4), ALU.subtract,
                            r=[r_u0, rws], w=[self.r_vtmp])
                    for h4 in range(4):
                        h = grp * 4 + h4
                        self.ts("dve", vnew[ps_, h, :], self.vtmp[ps_, h4, :], sc3[ps_, b, 16 + h:17 + h], None, ALU.mult, r=[self.r_vtmp, r_sc] + ra, w=[r_vn])
                    for h4 in range(4):
                        h = grp * 4 + h4
                        pS, rpS = self.bank((6, 7))
                        self.mm(pS[:, 0:128], ktok[ps_, h, :], vnew[ps_, h, :], r=[r_kt, r_vn] + ra, w=[rpS])
                        self.stt("dve", S[:, h, :], S[:, h, :], edl[:, b, e * 8 + h:e * 8 + h + 1], pS[:, 0:128], ALU.mult, ALU.add, r=[rS, self.r_edl, rpS], w=[rS])
                        self.cp("act", Sb[:, h, :], S[:, h, :], r=[rS], w=[rSb])
            for grp in range(2):
                g4 = slice(grp * 4, grp * 4 + 4)
                for h4 in range(4):
                    h = grp * 4 + h4
                    self.act(otok[:, h, :], po[grp][0][:, h4 * 128:(h4 + 1) * 128], AF.Copy, r=[po[grp][1], r_sc], w=[self.r_otok], scale=sc3[:, b, 40 + h:41 + h])
                pI, rI = self.bank((0, 1, 2, 3))
                for h4 in range(4):
                    h = grp * 4 + h4
                    self.mm(pI[:, h4 * 128:(h4 + 1) * 128], qkT[:, h, :], vnew[:, h, :], r=[r_qk, r_vn] + ra, w=[rI])
                self.tt("dve", otok[:, g4, :], otok[:, g4, :], pI.rearrange("p (a b) -> p a b", a=4), ALU.add, r=[self.r_otok, rI], w=[self.r_otok])
            self.dump("vnew", vnew, [r_vn], BF16)
            self.dump("otok", otok, [self.r_otok], F32)
            if b == 3:
                self.dump("otok3", otok, [self.r_otok], F32)
            ssq = self.ssq
            osq = f[:, 5632:6656].rearrange("p (a b) -> p a b", a=8)
            self.act(osq, otok, AF.Square, r=[self.r_otok], w=[self.r_junk])
            self.P.op("dve", lambda e, o_=ssq[:, 0:8], i_=osq: e.reduce_sum(out=o_, in_=i_, axis=mybir.AxisListType.X), reads=[self.r_junk], writes=[self.r_ssq])
            self.ts("dve", ssq[:, 0:8], ssq[:, 0:8], 1.0 / 128.0, None, ALU.mult, r=[self.r_ssq], w=[self.r_ssq])
            self.act(ssq[:, 0:8], ssq[:, 0:8], AF.Ln, r=[self.r_ssq, self.r_c], w=[self.r_ssq], bias=self.epsb[:])
            self.act(ssq[:, 0:8], ssq[:, 0:8], AF.Exp, r=[self.r_ssq], w=[self.r_ssq], scale=-0.5)
            for h in range(8):
                self.stt("dve", otok[:, h, :], otok[:, h, :], ssq[:, h:h + 1], sm[:, S_GDNN + j * 128:S_GDNN + (j + 1) * 128], ALU.mult, ALU.mult,
                         r=[self.r_otok, self.r_ssq, self.r_c], w=[self.r_otok])
            self.tt("dve", ogt[:, :], otok.rearrange("p a b -> p (a b)"), rtok, ALU.mult, r=[self.r_otok, r_rt] + ra, w=[r_ogt])
            self.dump("ogt", ogt, [r_ogt], BF16)
            self.dump("rtok", rtok, [r_rt], F32)
            self.dump("ssq", self.ssq, [self.r_ssq], F32)
            for grp in range(2):
                pG, rG = self.bank((0, 1, 2, 3))
                pG_b = self.pbank_bf[self._last_bank_id((0, 1, 2, 3))]
                for h4 in range(4):
                    c = grp * 4 + h4
                    self.tr(pG_b[:, h4 * 128:(h4 + 1) * 128], ogt[:, c * 128:(c + 1) * 128], idb, r=[r_ogt, self.r_c] + ra, w=[rG])
                self.cp("act", og[:, grp * 4:grp * 4 + 4, bc], pG_b[:, 0:512].rearrange("p (a b) -> p a b", a=4), r=[rG] + ra, w=[r_og])
        self.dump("og", og, [r_og], BF16)
        self.dump("Sd", S, [rS], F32)
        self.out_proj(og, r_og, ra)

    def dump(self, name, ap, reads, dt):
        if not getattr(self, "debug", False) or name in self._dumped:
            return
        self._dumped.add(name)
        d = self.nc.dram_tensor("dbg_" + name, list(ap.shape), dt, kind="ExternalOutput").ap()
        self.P.dma("sp", lambda e: e.dma_start(out=d, in_=ap), reads=reads, key="dbg_" + name)

    def _last_bank_id(self, pool):
        k = self._bctr[pool] - 1
        return pool[k % len(pool)]

    def build(self):
        nc = bass.Bass("TRN2", target_bir_lowering=False)
        self.nc = nc
        depth, nseq, ntile = self.depth, self.nseq, self.ntile
        ntok = nseq * ntile * T
        x_d = nc.dram_tensor("x", [ntok, D], F32, kind="ExternalInput").ap()
        wk_d = nc.dram_tensor("wk", [self.NB, 128, 4096], F32, kind="ExternalInput").ap()
        sm_d = nc.dram_tensor("sm", [128, NSM], F32, kind="ExternalInput").ap()
        cf_d = nc.dram_tensor("cf", [128, NCF], F32, kind="ExternalInput").ap()
        cb_d = nc.dram_tensor("cb", [128, NCB], F32, kind="ExternalInput").ap()
        y_d = nc.dram_tensor("y", [ntok, D], F32, kind="ExternalOutput").ap()
        self.wkb = nc.dram_tensor("wkb", [self.NB, 128, 4096], BF16, kind="Internal").ap()
        self.blk_layer = []
        for l in range(depth):
            nmix = self.nblk_layer[l] - 34
            self.blk_layer += [l * 3] * 17 + [l * 3 + 1] * nmix + [l * 3 + 2] * 17
        with contextlib.ExitStack() as st:
            def sb(name, shape, dt):
                return st.enter_context(nc.sbuf_tensor("s_" + name, shape, dt))
            P = self.P = Prog(nc)
            self._bctr = {}
            self._dumped = set()
            pall = st.enter_context(nc.psum_tensor("pall", [128, 8 * 512], F32))
            self.pbank = [pall[:, k * 512:(k + 1) * 512] for k in range(8)]
            self.rbank = [Res("bank%d" % k) for k in range(8)]
            self.pbank_bf = [pall[:, k * 512:(k + 1) * 512].bitcast(BF16) for k in range(8)]
            self.xT = sb("xT", [128, 8, T], F32)
            self.hT = sb("hT", [128, 8, T], BF16)
            self.sq = sb("sq", [128, 4, T], BF16)
            self.rstd = sb("rstd", [128, T], F32)[:]
            self.r_x, self.r_h, self.r_rstd = Res("x"), Res("h"), Res("rstd")
            self.r_sqs = [Res("sq%d" % k) for k in range(4)]
            self.xT, self.hT, self.sq = self.xT[:], self.hT[:], self.sq[:]
            self.tmpf = [sb("tmpf%d" % k, [128, T], F32)[:] for k in range(2)]
            self.r_tmpf = [Res("tmpf%d" % k) for k in range(2)]
            self.tmpg = [sb("tmpg%d" % k, [128, T], F32)[:] for k in range(2)]
            self.r_tmpg = [Res("tmpg%d" % k) for k in range(2)]
            self.tmph = [sb("tmph%d" % k, [128, T], F32)[:] for k in range(1)] * 2
            self.r_tmph = [Res("tmph0")] * 2
            self.sgbuf = [sb("sg%d" % k, [128, T], F32)[:] for k in range(2)]
            self.r_sg = [Res("sg%d" % k) for k in range(2)]
            self.NWK = 4
            wring = [sb("wring%d" % k, [128, 4096], BF16) for k in range(self.NWK)]
            self.wring = [w_[:] for w_ in wring]
            self.wring3 = [w_[:].rearrange("p (a b) -> p a b", a=8) for w_ in wring]
            self.r_wring = [Res("wring%d" % k) for k in range(self.NWK)]
            self.wk_i = 0
            self.arena = sb("arena", [128, 2 * 516 + 24 * 512 + 4096 + 13 * 1024 + 64], BF16)[:]
            self.r_arena = Res("arena")
            self.r_av = [Res("av%d" % k, parent=self.r_arena) for k in range(24)]
            self.f32a = sb("f32a", [128, 6656], F32)[:]
            self.cell = sb("cell", [128, 2], F32)
            self.cf = sb("cf", [128, NCF], F32)[:]
            self.cb = sb("cb", [128, NCB], BF16)[:]
            self.sm = sb("sm", [128, S_WZ], F32)[:]
            self.smb = sb("smb", [128, 512], BF16)[:]
            self.wgkb = sb("wgkb", [16, 1024], BF16)[:]
            self.bgkb = sb("bgkb", [1, 1024], BF16)[:]
            self.epsb = sb("epsb", [128, 1], F32)
            self.oneb = sb("oneb", [128, 1], F32)
            self.negA = sb("negA", [128, 16], F32)[:]
            self.id4 = sb("id4", [128, 4, 128], BF16)[:]
            self.xtmp = sb("xtmp", [128, 4, 128], F32)[:]
            self.r_xtmp = Res("xtmp")
            self.vtmp = sb("vtmp", [128, 4, 128], F32)[:]
            self.r_vtmp = Res("vtmp")
            self.edl = sb("edl", [128, 4, 16], F32)[:]
            self.r_edl = Res("edl", strict=True)
            self.ssq = sb("ssq", [128, 8], F32)[:]
            self.r_ssq = Res("ssq", strict=True)
            self.r_qc = [Res("qc%d" % k, parent=self.r_arena) for k in range(24)]
            self.r_cv = [Res("cv%d" % k, parent=self.r_arena) for k in range(8)]
            self.r_scs = Res("scs", parent=self.r_arena, strict=True)
            self.r_junk = Res("junk")
            self.r_otok = Res("otok", parent=self.r_arena)
            self.r_c = Res("consts", strict=True)
            self.Sg = [sb("Sg%d" % k, [128, 4, 256], F32)[:] for k in range(2)]
            self.Sgb = [sb("Sgb%d" % k, [128, 4, 256], BF16)[:] for k in range(2)]
            self.Sd = [sb("Sd%d" % k, [128, 8, 128], F32)[:] for k in range(2)]
            self.Sdb = [sb("Sdb%d" % k, [128, 8, 128], BF16)[:] for k in range(2)]
            self.ctail = [sb("ct%d" % k, [128, 24, 3], BF16)[:] for k in range(2)]
            self.r_Sg = [Res("Sg%d" % k) for k in range(2)]
            self.r_Sgb = [Res("Sgb%d" % k) for k in range(2)]
            self.r_Sd = [Res("Sd%d" % k) for k in range(2)]
            self.r_Sdb = [Res("Sdb%d" % k) for k in range(2)]
            self.r_ctail = [Res("ct%d" % k) for k in range(2)]
            xin = [self.arena[:, k * 2048:(k + 1) * 2048].bitcast(F32) for k in range(2)]
            r_xin = [Res("xin%d" % k) for k in range(2)]
            rc = self.r_c
            P.dma("sp", lambda e: e.dma_start(out=self.cf, in_=cf_d), writes=[rc], key="c0")
            P.dma("sp", lambda e: e.dma_start(out=self.sm, in_=sm_d[:, 0:S_WZ]), writes=[rc], key="c0")
            P.dma("pool", lambda e: e.dma_start(out=self.cb, in_=cb_d), writes=[rc], key="c1")
            P.dma("pool", lambda e: e.dma_start(out=self.smb[:, 0:256], in_=sm_d[:, S_WZ:S_WZ + 256]), writes=[rc], key="c1")
            P.dma("pool", lambda e: e.dma_start(out=self.smb[:, 256:512], in_=sm_d[:, S_WAB:S_WAB + 256]), writes=[rc], key="c1")
            P.dma("pool", lambda e: e.dma_start(out=self.wgkb, in_=sm_d[0:16, S_WGK:S_WGK + 1024]), writes=[rc], key="c1")
            P.dma("pool", lambda e: e.dma_start(out=self.bgkb, in_=sm_d[0:1, S_BGK:S_BGK + 1024]), writes=[rc], key="c1")
            self.memset("dve", self.epsb[:], EPS, w=[rc])
            self.memset("dve", self.oneb[:], 1.0, w=[rc])
            for k in range(4):
                self.cp("dve", self.id4[:, k, :], self.cb[:, B_ID:B_ID + 128], r=[rc], w=[rc])
            self.act(self.negA, self.sm[:, S_ALOG:S_ALOG + 16], AF.Exp, r=[rc], w=[rc])
            self.ts("dve", self.negA, self.negA, -1.0, None, ALU.mult, r=[rc], w=[rc])
            self.r_wkb = [Res("wkb%d" % l) for l in range(3 * depth)]
            bi = 0
            for l in range(depth):
                for k in range(self.nblk_layer[l]):
                    src, dst = wk_d[bi], self.wkb[bi]
                    P.dma("pool", lambda e, s_=src, d_=dst: e.dma_start(out=d_, in_=s_), writes=[self.r_wkb[self.blk_layer[bi]]], key="cwk%d" % self.blk_layer[bi])
                    bi += 1
            idf = self.cf[:, C_ID:C_ID + 128]
            r_yout = [Res("yout%d" % k) for k in range(2)]
            for s in range(nseq):
                for t in range(ntile):
                    tok0 = (s * ntile + t) * T
                    self.wk_next = 0
                    self.arena_switch()
                    for b in range(4):
                        xi, rxi = xin[b % 2], r_xin[b % 2]
                        src = x_d[tok0 + b * 128:tok0 + (b + 1) * 128, :]
                        P.dma("sp", lambda e, s_=src, d_=xi: e.dma_start(out=d_, in_=s_), reads=[self.r_arena], writes=[rxi], key="xin%d" % (b % 2))
                        for half in range(2):
                            pt, rt = self.bank()
                            for c4 in range(4):
                                c = half * 4 + c4
                                self.tr(pt[:, c4 * 128:(c4 + 1) * 128], xi[:, c * 128:(c + 1) * 128], idf, r=[rxi, rc, self.r_arena], w=[rt])
                            self.cp("act" if half == 0 else "dve", self.xT[:, half * 4:half * 4 + 4, b * 128:(b + 1) * 128],
                                    pt.rearrange("p (a b) -> p a b", a=4), r=[rt], w=[self.r_x])
                    for l in range(depth):
                        j = l // 2
                        self.ffn(l, 0)
                        if self.stop_after == (l, 0):
                            break
                        if _layer_is_gla(l):
                            self.gla(l, j, t == 0)
                        else:
                            self.gdn(l, j, t == 0)
                        if self.stop_after == (l, 1):
                            break
                        self.ffn(l, 1)
                        if self.stop_after == (l, 2):
                            break
                    if self.stop_after is None:
                        self.rmsnorm_final()
                        src_T = self.f32a[:, 0:4096].rearrange("p (a b) -> p a b", a=8)
                        r_src = self.r_av[23]
                    else:
                        self.arena_switch()
                        src_T, r_src = self.xT, self.r_x
                    for b in range(4):
                        yo, ryo = xin[b % 2], r_xin[b % 2]
                        for half in range(2):
                            pt, rt = self.bank()
                            for c4 in range(4):
                                c = half * 4 + c4
                                self.tr(pt[:, c4 * 128:(c4 + 1) * 128], src_T[:, c, b * 128:(b + 1) * 128], idf, r=[r_src, rc], w=[rt])
                            self.cp("act" if half == 0 else "dve", yo[:, half * 512:(half + 1) * 512], pt, r=[rt, self.r_arena], w=[ryo])
                        dst = y_d[tok0 + b * 128:tok0 + (b + 1) * 128, :]
                        P.dma("sp", lambda e, s_=yo, d_=dst: e.dma_start(out=d_, in_=s_), reads=[ryo, self.r_arena], writes=[], key="xin%d" % (b % 2))
            P.emit()
        return nc

    def rmsnorm_final(self):
        xT, sq = self.xT, self.sq
        self.arena_switch()
        outT = self.f32a[:, 0:4096].rearrange("p (a b) -> p a b", a=8)
        r_out = self.r_av[23]
        pb, rb = self.bank()
        for c in range(8):
            self.act(sq[:, c % 4, :], xT[:, c, :], AF.Square, r=[self.r_x], w=[self.r_sqs[c % 4]])
            self.mm(pb, self.cb[:, B_O1024:B_O1024 + 128], sq[:, c % 4, :], start=(c == 0), stop=(c == 7), r=[self.r_sqs[c % 4], self.r_c], w=[rb])
        rstd = self.rstd
        self.act(rstd, pb, AF.Ln, r=[rb, self.r_c], w=[self.r_rstd], bias=self.epsb[:])
        self.act(rstd, rstd, AF.Exp, r=[self.r_rstd], w=[self.r_rstd], scale=-0.5)
        for c in range(8):
            self.stt("dve", outT[:, c, :], xT[:, c, :], self.sm[:, S_FNORM + c:S_FNORM + c + 1], rstd, ALU.mult, ALU.mult,
                     r=[self.r_x, self.r_rstd, self.r_c, self.r_arena], w=[r_out])


_CACHE = {}


def kernel(**inputs):
    inp = {k: np.asarray(v) for k, v in inputs.items()}
    depth, ncore = 4, 8
    wk, sm = _prep_weights(inp, depth)
    cf, cb = _consts()
    x = np.ascontiguousarray(inp["x"], dtype=np.float32).reshape(ncore, 2 * SEQ, D)
    if "nc" not in _CACHE:
        _CACHE["nc"] = Builder(depth=depth, nseq=2, ntile=SEQ // T).build()
    nc = _CACHE["nc"]
    in_maps = [{"x": x[c], "wk": wk, "sm": sm, "cf": cf, "cb": cb} for c in range(ncore)]
    res = run_bass_kernel_spmd(nc, in_maps, core_ids=list(range(ncore)))
    y = np.stack([np.asarray(res.results[c]["y"]) for c in range(ncore)])
    return y.reshape(16, SEQ, D).astype(np.float32)
```

```python
import contextlib
import numpy as np
import concourse.bass as bass
import concourse.mybir as mybir
from concourse.bass_utils import run_bass_kernel_spmd

F32 = mybir.dt.float32
BF16 = mybir.dt.bfloat16
AF = mybir.ActivationFunctionType
ALU = mybir.AluOpType

D = 1024
SEQ = 4096
T = 512
EPS = 1e-6
DFF = 2816
ENGS = ("pe", "act", "dve", "pool", "sp")
EPOCH = 30000


class Res:
    __slots__ = ("name", "last_w", "readers", "parent", "strict")

    def __init__(self, name, parent=None, strict=False):
        self.name = name
        self.last_w = None
        self.readers = []
        self.parent = parent
        self.strict = strict


class Op:
    __slots__ = ("eng", "fn", "waits", "signal", "is_dma", "key", "sig")

    def __init__(self, eng, fn, is_dma=False, key=None):
        self.eng = eng
        self.fn = fn
        self.waits = []
        self.signal = False
        self.is_dma = is_dma
        self.key = key
        self.sig = None


class Prog:
    def __init__(self, nc):
        self.nc = nc
        self.ops = {e: [] for e in ENGS}
        self.all_ops = []
        self.dma_keys = {}

    def _dep(self, op, prod, strict=False):
        if prod is None or prod is op:
            return
        if prod.eng == op.eng and not prod.is_dma and not strict:
            return
        op.waits.append(prod)
        prod.signal = True

    def _track(self, op, reads, writes):
        extra = [r.parent for r in list(reads) + list(writes) if r.parent is not None]
        if extra:
            reads = list(reads) + [p for p in extra if p not in reads and p not in writes]
        for r in reads:
            self._dep(op, r.last_w, r.strict)
        for w in writes:
            self._dep(op, w.last_w, w.strict)
            for rd in w.readers:
                self._dep(op, rd)
        for r in reads:
            if not op.is_dma:
                r.readers = [x for x in r.readers if x.is_dma or x.eng != op.eng]
            r.readers.append(op)
        for w in writes:
            w.last_w = op
            w.readers = []

    def op(self, eng, fn, reads=(), writes=()):
        o = Op(eng, fn)
        self._track(o, reads, writes)
        self.ops[eng].append(o)
        self.all_ops.append(o)
        return o

    def dma(self, eng, fn, reads=(), writes=(), key="d"):
        o = Op(eng, fn, is_dma=True, key=key)
        o.signal = True
        self._track(o, reads, writes)
        self.ops[eng].append(o)
        self.all_ops.append(o)
        self.dma_keys.setdefault(key, 0)
        return o

    def emit(self):
        nc = self.nc
        cnt = {e: 0 for e in ENGS}
        dcnt = {k: 0 for k in self.dma_keys}
        for o in self.all_ops:
            if o.is_dma:
                dcnt[o.key] += 16
                o.sig = ("dma", o.key, dcnt[o.key])
            elif o.signal:
                cnt[o.eng] += 1
                ep, v = divmod(cnt[o.eng] - 1, EPOCH)
                o.sig = ("eng", (o.eng, ep), v + 1)
        sem_names = []
        for e in ENGS:
            for ep in range(max(1, (cnt[e] + EPOCH - 1) // EPOCH)):
                sem_names.append(("eng", (e, ep)))
        for k in self.dma_keys:
            sem_names.append(("dma", k))
        with contextlib.ExitStack() as st:
            sems = {}
            for i, sn in enumerate(sem_names):
                sems[sn] = st.enter_context(nc.semaphore("s%d" % i))
            block = st.enter_context(nc.Block())
            prog = self

            def make(ename):
                def body(eng):
                    waited = {}
                    for o in prog.ops[ename]:
                        need = {}
                        for p in o.waits:
                            kind, k, v = p.sig
                            sn = (kind, k)
                            if need.get(sn, 0) < v:
                                need[sn] = v
                        for sn, v in need.items():
                            if waited.get(sn, 0) >= v:
                                continue
                            eng.wait_ge(sems[sn], v)
                            waited[sn] = v
                        ins = o.fn(eng)
                        if o.is_dma:
                            ins.then_inc(sems[("dma", o.key)], 16)
                        elif o.signal:
                            ins.then_inc(sems[(o.sig[0], o.sig[1])], 1)
                    if ename == "sp":
                        for k, tot in dcnt.items():
                            if tot > 0 and waited.get(("dma", k), 0) < tot:
                                eng.wait_ge(sems[("dma", k)], tot)
                return body

            block.tensor(make("pe"))
            block.scalar(make("act"))
            block.vector(make("dve"))
            block.gpsimd(make("pool"))
            block.sync(make("sp"))


C_ID, C_LI, C_US, C_LS, C_CE, C_CO, C_ONE = 0, 128, 256, 384, 512, 640, 768
NCF = 896
B_ID, B_LI, B_LS4, B_TRB, B_TR2, B_O1024, B_O256, B_O1, B_LI4 = 0, 128, 256, 768, 896, 1024, 1152, 1280, 1408
NCB = 1920

S_NORM = 0
S_FNORM = 96
S_GLAN = 104
S_CONV = 108
S_ALOG = 300
S_DTB = 316
S_GDNN = 332
S_WZ = 588
S_WAB = 844
S_WGK = 1100
S_BGK = 2124
NSM = 3148


def _consts():
    cf = np.zeros((128, NCF), np.float32)
    m = np.arange(128)
    same = (m[:, None] // 64) == (m[None, :] // 64)
    li = ((m[:, None] <= m[None, :]) & same).astype(np.float32)
    ls = ((m[:, None] < m[None, :]) & same).astype(np.float32)
    us = ((m[:, None] > m[None, :]) & same).astype(np.float32)
    cf[:, C_ID:C_ID + 128] = np.eye(128)
    cf[:, C_LI:C_LI + 128] = li
    cf[:, C_US:C_US + 128] = us
    cf[:, C_LS:C_LS + 128] = ls
    cf[:, C_CE:C_CE + 128] = (m[:, None] < 64).astype(np.float32) * np.ones((1, 128), np.float32)
    cf[:, C_CO:C_CO + 128] = (m[:, None] >= 64).astype(np.float32) * np.ones((1, 128), np.float32)
    cf[:, C_ONE:C_ONE + 128] = 1.0
    cb = np.zeros((128, NCB), np.float32)
    cb[:, B_ID:B_ID + 128] = np.eye(128)
    cb[:, B_LI:B_LI + 128] = li
    cb[:, B_LS4:B_LS4 + 512] = np.tile(ls, (1, 4))
    cb[:, B_TRB:B_TRB + 128] = -li / 16.0
    cb[:, B_TR2:B_TR2 + 128] = -us / 16.0
    cb[:, B_O1024:B_O1024 + 128] = 1.0 / 1024.0
    cb[:, B_O256:B_O256 + 128] = 1.0 / 256.0
    cb[:, B_O1:B_O1 + 128] = 1.0
    cb[:, B_LI4:B_LI4 + 512] = np.tile(li, (1, 4))
    return cf, cb


def _kblocks(W, col_lists):
    out = []
    for cols in col_lists:
        sub = W[:, cols]
        out.append(sub.reshape(8, 128, 512).transpose(1, 0, 2).reshape(128, 4096))
    return out


_SWAP = [False]


def _layer_is_gla(l):
    return (l % 2 == 0) != _SWAP[0]


def _prep_weights(inp, depth):
    wk = []
    ar = np.arange
    for l in range(depth):
        j = l // 2
        for which in range(2):
            if which == 1:
                pass
        def ffn_blocks(i):
            W = inp["ffn_w_in"][l, i]
            lists = [np.concatenate([ar(b * 256, (b + 1) * 256), DFF + ar(b * 256, (b + 1) * 256)]) for b in range(11)]
            wk.extend(_kblocks(W, lists))
            Wo = inp["ffn_w_out"][l, i]
            for half in range(2):
                for g in range(3):
                    nf = 8 if g < 2 else 6
                    sub = Wo[g * 1024:g * 1024 + nf * 128, half * 512:(half + 1) * 512]
                    blk = np.zeros((128, 8, 512), np.float32)
                    blk[:, 0:nf, :] = sub.reshape(nf, 128, 512).transpose(1, 0, 2)
                    wk.append(blk.reshape(128, 4096))
        ffn_blocks(0)
        if _layer_is_gla(l):
            W = inp["gla_w_in"][j]
            lists = [ar(512, 1024), ar(0, 512), ar(1024, 1536), ar(1536, 2048), ar(2048, 2560), ar(2560, 3072)]
            wk.extend(_kblocks(W, lists))
            wk.extend(_kblocks(inp["gla_w_out"][j], [ar(0, 512), ar(512, 1024)]))
        else:
            W = inp["gdn_w_in"][j]
            lists = [ar(b * 512, (b + 1) * 512) for b in range(8)]
            wk.extend(_kblocks(W, lists))
            wk.extend(_kblocks(inp["gdn_w_out"][j], [ar(0, 512), ar(512, 1024)]))
        ffn_blocks(1)
    sm = np.zeros((128, NSM), np.float32)
    sm[:, S_NORM:S_NORM + 96] = inp["norm_w"].reshape(12, 8, 128).transpose(2, 0, 1).reshape(128, 96)
    sm[:, S_FNORM:S_FNORM + 8] = inp["final_norm_w"].reshape(8, 128).T
    sm[:, S_GLAN:S_GLAN + 4] = inp["gla_norm_w"].reshape(2, 2, 128).transpose(2, 0, 1).reshape(128, 4)
    sm[:, S_CONV:S_CONV + 192] = inp["gdn_conv_w"].reshape(2, 4, 24, 128).transpose(3, 0, 1, 2).reshape(128, 192)
    sm[:, S_ALOG:S_ALOG + 16] = inp["gdn_a_log"].reshape(1, 16)
    sm[:, S_DTB:S_DTB + 16] = inp["gdn_dt_bias"].reshape(1, 16)
    sm[:, S_GDNN:S_GDNN + 256] = inp["gdn_norm_w"].reshape(1, 256)
    for j in range(2):
        wz = inp["gla_w_in"][j][:, 3072:3088]
        sm[:, S_WZ + j * 128:S_WZ + (j + 1) * 128] = wz.reshape(8, 128, 16).transpose(1, 0, 2).reshape(128, 128)
        wab = inp["gdn_w_in"][j][:, 4096:4112]
        sm[:, S_WAB + j * 128:S_WAB + (j + 1) * 128] = wab.reshape(8, 128, 16).transpose(1, 0, 2).reshape(128, 128)
        sm[0:16, S_WGK + j * 512:S_WGK + (j + 1) * 512] = inp["gla_w_gk"][j]
        sm[0:1, S_BGK + j * 512:S_BGK + (j + 1) * 512] = inp["gla_b_gk"][j][None, :]
    return np.ascontiguousarray(np.stack(wk)), sm


class Builder:
    def __init__(self, depth=4, nseq=2, ntile=8, stop_after=None):
        self.depth, self.nseq, self.ntile = depth, nseq, ntile
        self.stop_after = stop_after
        self.nblk_layer = [42 if _layer_is_gla(l) else 44 for l in range(depth)]
        self.NB = sum(self.nblk_layer)

    def mm(self, out, lhsT, rhs, start=True, stop=True, r=(), w=()):
        self.P.op("pe", lambda e: e.matmul(out, lhsT, rhs, start=start, stop=stop, skip_group_check=True), reads=r, writes=w)

    def tr(self, out, in_, ident, r=(), w=()):
        self.P.op("pe", lambda e: e.transpose(out, in_, ident), reads=r, writes=w)

    def act(self, out, in_, func, r=(), w=(), bias=None, scale=None, accum=None, eng="act"):
        kw = {}
        if bias is not None:
            kw["bias"] = bias
        if scale is not None:
            kw["scale"] = scale
        if accum is not None:
            kw["accum_out"] = accum
        self.P.op("act", lambda e: e.activation(out=out, in_=in_, func=func, **kw), reads=r, writes=w)

    def tt(self, eng, out, in0, in1, op, r=(), w=()):
        self.P.op(eng, lambda e: e.tensor_tensor(out=out, in0=in0, in1=in1, op=op), reads=r, writes=w)

    def ts(self, eng, out, in0, s1, s2, op0, op1=None, r=(), w=()):
        if op1 is None:
            self.P.op(eng, lambda e: e.tensor_scalar(out=out, in0=in0, scalar1=s1, scalar2=None, op0=op0), reads=r, writes=w)
        else:
            self.P.op(eng, lambda e: e.tensor_scalar(out=out, in0=in0, scalar1=s1, scalar2=s2, op0=op0, op1=op1), reads=r, writes=w)

    def stt(self, eng, out, in0, scalar, in1, op0, op1, r=(), w=()):
        self.P.op(eng, lambda e: e.scalar_tensor_tensor(out=out, in0=in0, scalar=scalar, in1=in1, op0=op0, op1=op1), reads=r, writes=w)

    def cp(self, eng, out, in_, r=(), w=()):
        if eng == "act":
            self.P.op("act", lambda e: e.activation(out=out, in_=in_, func=AF.Copy), reads=r, writes=w)
        else:
            self.P.op(eng, lambda e: e.tensor_copy(out=out, in_=in_), reads=r, writes=w)

    def memset(self, eng, ap, val, w=()):
        self.P.op(eng, lambda e: e.memset(ap, val), writes=w)

    def bank(self, pool=None):
        pool = pool or (0, 1, 2, 3, 4, 5, 6, 7)
        k = self._bctr.get(pool, 0)
        self._bctr[pool] = k + 1
        b = pool[k % len(pool)]
        return self.pbank[b], self.rbank[b]

    def load_wk(self):
        i = self.wk_i
        self.wk_i += 1
        slot = i % self.NWK
        blk = self.wk_next
        self.wk_next += 1
        src = self.wkb[blk]
        dst = self.wring[slot]
        layer_res = self.r_wkb[self.blk_layer[blk]]
        self.P.dma("sp", lambda e: e.dma_start(out=dst, in_=src), reads=[layer_res], writes=[self.r_wring[slot]], key="wk%d" % slot)
        return self.wring3[slot], self.r_wring[slot]

    def arena_switch(self):
        cell = self.cell
        self.P.op("dve", lambda e: e.memset(cell[:], 0.0), writes=[self.r_arena])

    def rmsnorm(self, wcol):
        xT, hT, sq = self.xT, self.hT, self.sq
        pb, rb = self.bank()
        for c in range(8):
            self.act(sq[:, c % 4, :], xT[:, c, :], AF.Square, r=[self.r_x], w=[self.r_sqs[c % 4]])
            self.mm(pb, self.cb[:, B_O1024:B_O1024 + 128], sq[:, c % 4, :], start=(c == 0), stop=(c == 7), r=[self.r_sqs[c % 4], self.r_c], w=[rb])
        rstd = self.rstd
        self.act(rstd, pb, AF.Ln, r=[rb, self.r_c], w=[self.r_rstd], bias=self.epsb[:])
        self.act(rstd, rstd, AF.Exp, r=[self.r_rstd], w=[self.r_rstd], scale=-0.5)
        for c in range(8):
            self.stt("dve", hT[:, c, :], xT[:, c, :], self.sm[:, wcol + c:wcol + c + 1], rstd, ALU.mult, ALU.mult,
                     r=[self.r_x, self.r_rstd, self.r_c], w=[self.r_h])

    def ffn(self, l, i):
        self.rmsnorm(S_NORM + (l * 3 + 2 * i) * 8)
        self.arena_switch()
        A = self.arena
        actb = A[:, 0:22 * 512].rearrange("p (a b) -> p a b", a=22)
        ra = [self.r_arena]
        r_act = self.r_av[0]
        hT = self.hT
        gu = (4, 5, 6, 7)
        for jb in range(11):
            w, rw = self.load_wk()
            for half in range(2):
                fc = jb * 2 + half
                pg, rg = self.bank(gu)
                pu, ru = self.bank(gu)
                for kc in range(8):
                    self.mm(pg, w[:, kc, half * 128:(half + 1) * 128], hT[:, kc, :], start=(kc == 0), stop=(kc == 7), r=[rw, self.r_h], w=[rg])
                for kc in range(8):
                    self.mm(pu, w[:, kc, 256 + half * 128:256 + (half + 1) * 128], hT[:, kc, :], start=(kc == 0), stop=(kc == 7), r=[rw, self.r_h], w=[ru])
                sg, rsg = self.sgbuf[fc % 2], self.r_sg[fc % 2]
                self.act(sg, pg, AF.Silu, r=[rg], w=[rsg])
                self.tt("dve", actb[:, fc, :], sg, pu, ALU.mult, r=[rsg, ru] + ra, w=[r_act])
        for half in range(2):
            pool = (0, 1, 2, 3) if half == 0 else (4, 5, 6, 7)
            bks = [self.bank(pool) for _ in range(4)]
            for g in range(3):
                wo, rwo = self.load_wk()
                for f in range(8 if g < 2 else 6):
                    fc = g * 8 + f
                    for d in range(4):
                        self.mm(bks[d][0], wo[:, f, d * 128:(d + 1) * 128], actb[:, fc, :], start=(fc == 0), stop=(fc == 21),
                                r=[rwo, r_act] + ra, w=[bks[d][1]])
            for d in range(4):
                dc = half * 4 + d
                self.stt("dve", self.xT[:, dc, :], bks[d][0], 0.5, self.xT[:, dc, :], ALU.mult, ALU.add, r=[bks[d][1], self.r_x], w=[self.r_x])

    def gla(self, l, j, first_tile):
        self.rmsnorm(S_NORM + (l * 3 + 1) * 8)
        self.arena_switch()
        A, ra = self.arena, [self.r_arena]
        hT, cb, cf = self.hT, self.cb, self.cf
        o = 0

        def carve(n, shape3=None):
            nonlocal o
            v = A[:, o:o + n]
            o += n
            return v
        qd = carve(2048).rearrange("p (a b) -> p a b", a=4)
        ki = carve(2048).rearrange("p (a b) -> p a b", a=4)
        ke = carve(2048).rearrange("p (a b) -> p a b", a=4)
        vt = carve(4096).rearrange("p (a b) -> p a b", a=4)
        lg = carve(2048).rearrange("p (a b) -> p a b", a=4)
        rs = carve(4096).rearrange("p (a b) -> p a b", a=8)
        og = carve(4096).rearrange("p (a b) -> p a b", a=8)
        zT = carve(512)
        AT = carve(1024).rearrange("p (a b) -> p a b", a=2)
        r_qd, r_ki, r_ke, r_vt, r_lg, r_rs, r_og, r_zT, r_oT, r_eb = self.r_av[0:10]
        r_AT = self.r_av[10:12]
        oT = self.f32a[:, 0:4096].rearrange("p (a b) -> p a b", a=8)
        eb = self.f32a[:, 4096:6144].rearrange("p (a b) -> p a b", a=4)
        S, Sb = self.Sg[j], self.Sgb[j]
        rS, rSb = self.r_Sg[j], self.r_Sgb[j]
        sm = self.sm
        if first_tile:
            self.memset("pool", S[:], 0.0, w=[rS])
            self.memset("pool", Sb[:], 0.0, w=[rSb])
        pz, rz = self.bank()
        for kc in range(8):
            self.mm(pz[0:16, :], self.smb[:, j * 128 + kc * 16:j * 128 + kc * 16 + 16], hT[:, kc, :], start=(kc == 0), stop=(kc == 7),
                    r=[self.r_h, self.r_c], w=[rz])
        self.cp("act", zT[0:16, :], pz[0:16, :], r=[rz] + ra, w=[r_zT])
        for b in range(4):
            pl, rl = self.bank()
            self.mm(pl, zT[0:16, b * 128:(b + 1) * 128], self.wgkb[0:16, j * 512:(j + 1) * 512], start=True, stop=False, r=[r_zT, self.r_c] + ra, w=[rl])
            self.mm(pl, self.cb[0:1, B_O1:B_O1 + 128], self.bgkb[0:1, j * 512:(j + 1) * 512], start=False, stop=True, r=[self.r_c], w=[rl])
            t0, rt0 = self.tmpf[b % 2], self.r_tmpf[b % 2]
            self.act(t0, pl, AF.Exp, r=[rl], w=[rt0], scale=-1.0)
            self.act(lg[:, b, :], t0, AF.Ln, r=[rt0, self.r_c] + ra, w=[r_lg], bias=self.oneb[:])
        w, rw = self.load_wk()
        for b in range(4):
            pk, rk = self.bank()
            for kc in range(8):
                self.mm(pk, hT[:, kc, b * 128:(b + 1) * 128], w[:, kc, :], start=(kc == 0), stop=(kc == 7), r=[rw, self.r_h], w=[rk])
            pd, rd = self.bank()
            self.mm(pd, cb[:, B_TR2:B_TR2 + 128], lg[:, b, :], r=[r_lg, self.r_c] + ra, w=[rd])
            t0, rt0 = self.tmpf[b % 2], self.r_tmpf[b % 2]
            self.act(t0, pd, AF.Exp, r=[rd], w=[rt0])
            self.tt("dve", ke[:, b, :], pk, t0, ALU.mult, r=[rk, rt0] + ra, w=[r_ke])
        for h in range(4):
            pk, rk = self.bank()
            for kc in range(8):
                self.mm(pk, w[:, kc, h * 128:(h + 1) * 128], hT[:, kc, :], start=(kc == 0), stop=(kc == 7), r=[rw, self.r_h], w=[rk])
            pb, rb = self.bank()
            for b in range(4):
                self.mm(pb[:, b * 128:(b + 1) * 128], lg[:, b, h * 128:(h + 1) * 128], cb[:, B_TRB:B_TRB + 128], r=[r_lg, self.r_c] + ra, w=[rb])
            t0, rt0 = self.tmpf[h % 2], self.r_tmpf[h % 2]
            self.act(t0, pb, AF.Exp, r=[rb], w=[rt0], scale=-1.0)
            self.act(eb[:, h, :], pb, AF.Exp, r=[rb], w=[r_eb])
            self.tt("dve", ki[:, h, :], pk, t0, ALU.mult, r=[rk, rt0] + ra, w=[r_ki])
        w, rw = self.load_wk()
        for h in range(4):
            pq, rq = self.bank()
            for kc in range(8):
                self.mm(pq, w[:, kc, h * 128:(h + 1) * 128], hT[:, kc, :], start=(kc == 0), stop=(kc == 7), r=[rw, self.r_h], w=[rq])
            self.stt("dve", qd[:, h, :], pq, 128.0 ** -0.5, eb[:, h, :], ALU.mult, ALU.mult, r=[rq, r_eb] + ra, w=[r_qd])
        for vb in range(2):
            w, rw = self.load_wk()
            for b in range(4):
                pv, rv = self.bank()
                for kc in range(8):
                    self.mm(pv, hT[:, kc, b * 128:(b + 1) * 128], w[:, kc, :], start=(kc == 0), stop=(kc == 7), r=[rw, self.r_h], w=[rv])
                self.cp("act" if b % 2 == 0 else "dve", vt[:, b, vb * 512:(vb + 1) * 512], pv, r=[rv] + ra, w=[r_vt])
        for rb_ in range(2):
            w, rw = self.load_wk()
            for cc in range(4):
                pr, rr = self.bank()
                for kc in range(8):
                    self.mm(pr, w[:, kc, cc * 128:(cc + 1) * 128], hT[:, kc, :], start=(kc == 0), stop=(kc == 7), r=[rw, self.r_h], w=[rr])
                self.act(rs[:, rb_ * 4 + cc, :], pr, AF.Silu, r=[rr] + ra, w=[r_rs])
        SP = (3, 4, 5, 6, 7)
        for b in range(4):
            bc = slice(b * 128, (b + 1) * 128)
            pA, rA = self.bank((0,))
            for h in range(4):
                self.mm(pA[:, h * 128:(h + 1) * 128], ki[:, h, bc], qd[:, h, bc], r=[r_ki, r_qd] + ra, w=[rA])
            at, rat = AT[:, b % 2, :], r_AT[b % 2]
            self.tt("dve", at, pA, cb[:, B_LI4:B_LI4 + 512], ALU.mult, r=[rA, self.r_c] + ra, w=[rat])
            po = [self.bank((1,)), self.bank((2,))]
            for pob, rob in po:
                self.memset("dve", pob, 0.0, w=[rob])
            for e in range(2):
                cs = slice(b * 128 + e * 64, b * 128 + (e + 1) * 64)
                last = b * 128 + e * 64 + 63
                for h in range(4):
                    pob, rob = po[h // 2]
                    for half in range(2):
                        c0 = (h % 2) * 256 + half * 128 + e * 64
                        self.mm(pob[:, c0:c0 + 64], Sb[:, h, half * 128:(half + 1) * 128], qd[:, h, cs], start=False, stop=False,
                                r=[rSb, r_qd] + ra, w=[rob])
                    pS, rpS = self.bank(SP)
                    self.mm(pS[:, 0:256], ke[e * 64:(e + 1) * 64, b, h * 128:(h + 1) * 128], vt[e * 64:(e + 1) * 64, b, h * 256:(h + 1) * 256],
                            r=[r_ke, r_vt] + ra, w=[rpS])
                    self.stt("dve", S[:, h, :], S[:, h, :], eb[:, h, last:last + 1], pS[:, 0:256], ALU.mult, ALU.add, r=[rS, r_eb, rpS], w=[rS])
                    self.cp("act", Sb[:, h, :], S[:, h, :], r=[rS], w=[rSb])
            for h in range(4):
                pob, rob = po[h // 2]
                for half in range(2):
                    c0 = (h % 2) * 256 + half * 128
                    self.mm(pob[:, c0:c0 + 128], vt[:, b, h * 256 + half * 128:h * 256 + (half + 1) * 128], at[:, h * 128:(h + 1) * 128],
                            start=False, stop=True, r=[r_vt, rat] + ra, w=[rob])
            for h2 in range(2):
                pob, rob = po[h2]
                self.cp("act", oT[:, h2 * 4:(h2 + 1) * 4, bc], pob.rearrange("p (a b) -> p a b", a=4), r=[rob], w=[r_oT])
        sq = self.sq
        for h in range(4):
            pn, rn = self.bank()
            for half in range(2):
                k4 = (h * 2 + half) % 4
                self.act(sq[:, k4, :], oT[:, h * 2 + half, :], AF.Square, r=[r_oT], w=[self.r_sqs[k4]])
                self.mm(pn, cb[:, B_O256:B_O256 + 128], sq[:, k4, :], start=(half == 0), stop=(half == 1), r=[self.r_sqs[k4], self.r_c], w=[rn])
            t0, rt0 = self.tmpf[h % 2], self.r_tmpf[h % 2]
            self.act(t0, pn, AF.Ln, r=[rn, self.r_c], w=[rt0], bias=self.epsb[:])
            self.act(t0, t0, AF.Exp, r=[rt0], w=[rt0], scale=-0.5)
            for half in range(2):
                c = h * 2 + half
                self.stt("dve", oT[:, c, :], oT[:, c, :], sm[:, S_GLAN + j * 2 + half:S_GLAN + j * 2 + half + 1], t0, ALU.mult, ALU.mult,
                         r=[r_oT, rt0, self.r_c], w=[r_oT])
                self.tt("dve", og[:, c, :], oT[:, c, :], rs[:, c, :], ALU.mult, r=[r_oT, r_rs] + ra, w=[r_og])
        self.out_proj(og, r_og, ra)

    def out_proj(self, og, r_og, ra):
        for blk in range(2):
            w, rw = self.load_wk()
            for dq in range(4):
                dc = blk * 4 + dq
                py, ry = self.bank()
                for vc in range(8):
                    self.mm(py, w[:, vc, dq * 128:(dq + 1) * 128], og[:, vc, :], start=(vc == 0), stop=(vc == 7), r=[rw, r_og] + ra, w=[ry])
                self.tt("dve", self.xT[:, dc, :], py, self.xT[:, dc, :], ALU.add, r=[ry, self.r_x], w=[self.r_x])

    def gdn(self, l, j, first_tile):
        self.rmsnorm(S_NORM + (l * 3 + 1) * 8)
        self.arena_switch()
        A, ra = self.arena, [self.r_arena]
        hT, cb, cf, sm = self.hT, self.cb, self.cf, self.sm
        o = 0

        def carve(n):
            nonlocal o
            v = A[:, o:o + n]
            o += n
            return v
        xc2 = carve(2 * 516).rearrange("p (a b) -> p a b", a=2)
        qkv = carve(24 * 512).rearrange("p (a b) -> p a b", a=24)
        og = carve(4096).rearrange("p (a b) -> p a b", a=8)
        ogt = carve(1024)
        ktok = carve(1024).rearrange("p (a b) -> p a b", a=8)
        kdtok = carve(1024).rearrange("p (a b) -> p a b", a=8)
        vtok = carve(1024).rearrange("p (a b) -> p a b", a=8)
        Pm = [carve(1024).rearrange("p (a b) -> p a b", a=8) for _ in range(2)]
        PmT = [carve(1024).rearrange("p (a b) -> p a b", a=8) for _ in range(2)]
        Rm = [carve(1024).rearrange("p (a b) -> p a b", a=8) for _ in range(2)]
        qkT = carve(1024).rearrange("p (a b) -> p a b", a=8)
        w0T = carve(1024).rearrange("p (a b) -> p a b", a=8)
        vnew = carve(1024).rearrange("p (a b) -> p a b", a=8)
        (r_xc0, r_qkv, r_og, r_ogt, r_kt, r_kd, r_vt, r_P0, r_P1, r_PT0, r_PT1, r_R0, r_R1, r_qk, r_w0, r_vn, r_rt, r_u0, r_sc, r_gu, r_dt) = self.r_av[0:21]
        r_P, r_PT, r_R = [r_P0, r_P1], [r_PT0, r_PT1], [r_R0, r_R1]
        r_sc = self.r_scs
        r_xcs = [r_xc0, self.r_av[21]]
        r_qc = self.r_qc
        f = self.f32a
        rtok = f[:, 0:1024]
        u0 = f[:, 1024:2048].rearrange("p (a b) -> p a b", a=8)
        gu = f[:, 2048:3072].rearrange("p (a b) -> p a b", a=8)
        dtm = f[:, 3072:4096].rearrange("p (a b) -> p a b", a=8)
        sc = f[:, 4096:4096 + 256]
        otok = f[:, 4608:5632].rearrange("p (a b) -> p a b", a=8)
        S, Sb = self.Sd[j], self.Sdb[j]
        rS, rSb = self.r_Sd[j], self.r_Sdb[j]
        ct, rct = self.ctail[j], self.r_ctail[j]
        if first_tile:
            self.memset("pool", S[:], 0.0, w=[rS])
            self.memset("pool", Sb[:], 0.0, w=[rSb])
            self.memset("pool", ct[:], 0.0, w=[rct])
        pab, rab = self.bank()
        for b in range(4):
            for kc in range(8):
                self.mm(pab[:, b * 16:(b + 1) * 16], hT[:, kc, b * 128:(b + 1) * 128], self.smb[:, 256 + j * 128 + kc * 16:256 + j * 128 + kc * 16 + 16],
                        start=(kc == 0), stop=(kc == 7), r=[self.r_h, self.r_c], w=[rab])
        pab3 = pab[:, 0:64].rearrange("p (b c) -> p b c", b=4)
        sc3 = sc.rearrange("p (b c) -> p b c", b=4)
        for b in range(4):
            self.tt("dve", sc3[:, b, 0:8], pab3[:, b, 0:8], sm[:, S_DTB + j * 8:S_DTB + j * 8 + 8], ALU.add, r=[rab, self.r_c], w=[r_sc])
        self.act(sc3[:, :, 0:8], sc3[:, :, 0:8], AF.Exp, r=[r_sc], w=[r_sc])
        self.act(sc3[:, :, 0:8], sc3[:, :, 0:8], AF.Ln, r=[r_sc, self.r_c], w=[r_sc], bias=self.oneb[:])
        for b in range(4):
            self.tt("dve", sc3[:, b, 8:16], sc3[:, b, 0:8], self.negA[:, j * 8:j * 8 + 8], ALU.mult, r=[r_sc, self.r_c], w=[r_sc])
        self.act(sc3[:, :, 16:24], pab3[:, :, 8:16], AF.Exp, r=[rab], w=[r_sc], scale=-1.0)
        self.act(sc3[:, :, 16:24], sc3[:, :, 16:24], AF.Ln, r=[r_sc, self.r_c], w=[r_sc], bias=self.oneb[:])
        self.act(sc3[:, :, 16:24], sc3[:, :, 16:24], AF.Exp, r=[r_sc], w=[r_sc], scale=-1.0)
        pdd, rdd = self.bank()
        for b in range(4):
            g_b = sc3[:, b, 8:16]
            self.mm(pdd[:, b * 16:b * 16 + 8], cf[:, C_LI:C_LI + 128], g_b, r=[r_sc, self.r_c], w=[rdd])
            self.mm(pdd[:, b * 16 + 8:b * 16 + 16], cf[:, C_US:C_US + 128], g_b, r=[r_sc, self.r_c], w=[rdd])
            self.mm(pdd[:, 64 + b * 16:64 + b * 16 + 8], cf[:, C_CE:C_CE + 128], g_b, r=[r_sc, self.r_c], w=[rdd])
            self.mm(pdd[:, 64 + b * 16 + 8:64 + b * 16 + 16], cf[:, C_CO:C_CO + 128], g_b, r=[r_sc, self.r_c], w=[rdd])
        pdd3 = pdd[:, 0:64].rearrange("p (b c) -> p b c", b=4)
        self.act(sc3[:, :, 40:56], pdd3, AF.Exp, r=[rdd], w=[r_sc])
        edl = self.edl
        self.act(edl, pdd[:, 64:128].rearrange("p (b c) -> p b c", b=4), AF.Exp, r=[rdd], w=[self.r_edl])
        t0s = [f[:, k * 512:(k + 1) * 512] for k in range(4)]
        t2s = [f[:, 2048 + k * 512:2048 + (k + 1) * 512] for k in range(4)]
        r_t0s, r_t2s = self.r_cv[0:4], self.r_cv[4:8]
        wcur = [None, None]
        pending, ready = [], []

        def stage_a(cc):
            if cc % 4 == 0:
                wcur[0], wcur[1] = self.load_wk()
            w, rw = wcur
            cq = cc % 4
            pp, rp = self.bank((0, 1, 2, 3))
            for kc in range(8):
                self.mm(pp, w[:, kc, cq * 128:(cq + 1) * 128], hT[:, kc, :], start=(kc == 0), stop=(kc == 7), r=[rw, self.r_h], w=[rp])
            xc, r_xc = xc2[:, cc % 2, :], r_xcs[cc % 2]
            self.cp("pool", xc[:, 0:3], ct[:, cc, :], r=[rct] + ra, w=[r_xc])
            self.cp("act", xc[:, 3:515], pp, r=[rp] + ra, w=[r_xc])
            t0, rt0 = t0s[cc % 4], r_t0s[cc % 4]
            cw = S_CONV + (j * 4) * 24 + cc
            self.ts("dve", t0, xc[:, 3:515], sm[:, cw + 3 * 24:cw + 3 * 24 + 1], None, ALU.mult, r=[r_xc, self.r_c] + ra, w=[rt0])
            for tap in (2, 1, 0):
                self.stt("dve", t0, xc[:, tap:tap + 512], sm[:, cw + tap * 24:cw + tap * 24 + 1], t0, ALU.mult, ALU.add,
                         r=[r_xc, rt0, self.r_c] + ra, w=[rt0])
            self.cp("pool", ct[:, cc, :], xc[:, 512:515], r=[r_xc] + ra, w=[rct])

        def stage_b(cc):
            t0, rt0 = t0s[cc % 4], r_t0s[cc % 4]
            self.act(qkv[:, cc, :], t0, AF.Silu, r=[rt0] + ra, w=[r_qc[cc]])
            if cc < 16:
                k4 = cc % 4
                self.act(self.sq[:, k4, :], qkv[:, cc, :], AF.Square, r=[r_qc[cc]] + ra, w=[self.r_sqs[k4]])
                pn, rn = self.bank((4, 5, 6, 7))
                self.mm(pn, cb[:, B_O1:B_O1 + 128], self.sq[:, k4, :], r=[self.r_sqs[k4], self.r_c], w=[rn])
                pending.append((cc, pn, rn))

        def stage_c(items):
            for k, (cc, pn, rn) in enumerate(items):
                self.act(t2s[k], pn, AF.Ln, r=[rn, self.r_c], w=[r_t2s[k]], bias=self.epsb[:])
            for k, (cc, pn, rn) in enumerate(items):
                self.act(t2s[k], t2s[k], AF.Exp, r=[r_t2s[k]], w=[r_t2s[k]], scale=-0.5)
            for k, (cc, pn, rn) in enumerate(items):
                if cc < 8:
                    self.stt("dve", qkv[:, cc, :], qkv[:, cc, :], 128.0 ** -0.5, t2s[k], ALU.mult, ALU.mult, r=[r_qc[cc], r_t2s[k]] + ra, w=[r_qc[cc]])
                else:
                    self.tt("dve", qkv[:, cc, :], qkv[:, cc, :], t2s[k], ALU.mult, r=[r_qc[cc], r_t2s[k]] + ra, w=[r_qc[cc]])

        for it in range(25):
            if it < 24:
                stage_a(it)
            if ready:
                stage_c(ready)
                ready = []
            if it >= 1:
                stage_b(it - 1)
            if len(pending) == 4:
                ready, pending = pending, []
        if ready:
            stage_c(ready)
        if pending:
            stage_c(pending)
        self.dump("sc", sc, [r_sc], F32)
        self.dump("edl", edl, [self.r_edl], F32)
        self.dump("qkv", qkv, r_qc, BF16)
        wr = [self.load_wk(), self.load_wk()]
        idb = cb[:, B_ID:B_ID + 128]
        for b in range(4):
            bc = slice(b * 128, (b + 1) * 128)
            for rb_ in range(2):
                w, rw = wr[rb_]
                pr, rr = self.bank((6, 7))
                for kc in range(8):
                    self.mm(pr, hT[:, kc, bc], w[:, kc, :], start=(kc == 0), stop=(kc == 7), r=[rw, self.r_h], w=[rr])
                self.act(rtok[:, rb_ * 512:(rb_ + 1) * 512], pr, AF.Silu, r=[rr], w=[r_rt])
                self.tt("dve", rtok[:, rb_ * 512:(rb_ + 1) * 512].rearrange("p (a b) -> p a b", a=4),
                        rtok[:, rb_ * 512:(rb_ + 1) * 512].rearrange("p (a b) -> p a b", a=4),
                        sm[:, S_GDNN + j * 128:S_GDNN + (j + 1) * 128].unsqueeze(1).broadcast_to([128, 4, 128]), ALU.mult,
                        r=[r_rt, self.r_c], w=[r_rt])
            for grp in range(2):
                hs = range(grp * 4, grp * 4 + 4)
                ptk, rtk = self.bank((6, 7))
                ptk_b = self.pbank_bf[self._last_bank_id((6, 7))]
                for h in hs:
                    self.tr(ptk_b[:, (h % 4) * 128:(h % 4 + 1) * 128], qkv[:, 8 + h, bc], idb, r=[r_qc[8 + h], self.r_c] + ra, w=[rtk])
                pk3 = ptk_b[:, 0:512].rearrange("p (a b) -> p a b", a=4)
                gq = slice(grp * 4, grp * 4 + 4)
                self.tt("dve", ktok[:, gq, :], pk3, sc3[:, b, 48 + grp * 4:52 + grp * 4].unsqueeze(2).broadcast_to([128, 4, 128]), ALU.mult,
                        r=[rtk, r_sc] + ra, w=[r_kt])
                self.tt("dve", kdtok[:, gq, :], pk3, sc3[:, b, 40 + grp * 4:44 + grp * 4].unsqueeze(2).broadcast_to([128, 4, 128]), ALU.mult,
                        r=[rtk, r_sc] + ra, w=[r_kd])
                ptv, rtv = self.bank((6, 7))
                ptv_b = self.pbank_bf[self._last_bank_id((6, 7))]
                for h in hs:
                    self.tr(ptv_b[:, (h % 4) * 128:(h % 4 + 1) * 128], qkv[:, 16 + h, bc], idb, r=[r_qc[16 + h], self.r_c] + ra, w=[rtv])
                self.cp("act", vtok[:, grp * 4:grp * 4 + 4, :], ptv_b[:, 0:512].rearrange("p (a b) -> p a b", a=4), r=[rtv] + ra, w=[r_vt])
            for h in range(8):
                self.ts("dve", gu[:, h, :], cf[:, C_US:C_US + 128], sc3[:, b, 8 + h:9 + h], None, ALU.mult, r=[self.r_c, r_sc], w=[r_gu])
            for grp in range(2):
                pD, rD = self.bank((4, 5))
                for h4 in range(4):
                    h = grp * 4 + h4
                    self.mm(pD[:, h4 * 128:(h4 + 1) * 128], gu[:, h, :], cf[:, C_LI:C_LI + 128], r=[r_gu, self.r_c], w=[rD])
                t0, rt0 = self.tmpf[grp], self.r_tmpf[grp]
                self.act(t0, pD, AF.Exp, r=[rD], w=[rt0])
                self.tt("dve", dtm[:, grp * 4:grp * 4 + 4, :], t0.rearrange("p (a b) -> p a b", a=4),
                        cb[:, B_LI4:B_LI4 + 512].rearrange("p (a b) -> p a b", a=4),
                        ALU.mult, r=[rt0, self.r_c], w=[r_dt])
            for grp in range(2):
                pX, rX = self.bank((4, 5))
                for h4 in range(4):
                    h = grp * 4 + h4
                    self.mm(pX[:, h4 * 128:(h4 + 1) * 128], qkv[:, 8 + h, bc], qkv[:, 8 + h, bc], r=[r_qc[8 + h]] + ra, w=[rX])
                for h4 in range(4):
                    h = grp * 4 + h4
                    self.stt("dve", self.xtmp[:, h4, :], pX[:, h4 * 128:(h4 + 1) * 128], sc3[:, b, 16 + h:17 + h], dtm[:, h, :], ALU.mult, ALU.mult,
                             r=[rX, r_sc, r_dt], w=[self.r_xtmp])
                self.stt("dve", Pm[0][:, grp * 4:grp * 4 + 4, :], self.xtmp[:, :, :], -1.0, cb[:, B_LS4:B_LS4 + 512].rearrange("p (a b) -> p a b", a=4),
                         ALU.mult, ALU.mult, r=[self.r_xtmp, self.r_c] + ra, w=[r_P[0]])
                pQ, rQ = self.bank((4, 5))
                for h4 in range(4):
                    h = grp * 4 + h4
                    self.mm(pQ[:, h4 * 128:(h4 + 1) * 128], qkv[:, 8 + h, bc], qkv[:, h, bc], r=[r_qc[8 + h], r_qc[h]] + ra, w=[rQ])
                self.tt("dve", qkT[:, grp * 4:grp * 4 + 4, :], pQ.rearrange("p (a b) -> p a b", a=4), dtm[:, grp * 4:grp * 4 + 4, :], ALU.mult,
                        r=[rQ, r_dt] + ra, w=[r_qk])
            self.dump("dtm", dtm, [r_dt], F32)
            self.dump("P0", Pm[0], [r_P[0]], BF16)
            self.dump("qkT", qkT, [r_qk], BF16)
            self.dump("ktok", ktok, [r_kt], BF16)
            self.dump("vtok", vtok, [r_vt], BF16)
            for grp in range(2):
                g4 = slice(grp * 4, grp * 4 + 4)
                pT, rT = self.bank((4, 5))
                pT_b = self.pbank_bf[self._last_bank_id((4, 5))]
                for h4 in range(4):
                    self.tr(pT_b[:, h4 * 128:(h4 + 1) * 128], Pm[0][:, grp * 4 + h4, :], idb, r=[r_P[0], self.r_c] + ra, w=[rT])
                self.cp("act", PmT[0][:, g4, :], pT_b[:, 0:512].rearrange("p (a b) -> p a b", a=4), r=[rT] + ra, w=[r_PT[0]])
                self.tt("pool", Rm[0][:, g4, :], Pm[0][:, g4, :], self.id4,
                        ALU.add, r=[r_P[0], self.r_c] + ra, w=[r_R[0]])
            for n in range(1, 6):
                cur, prv = n % 2, (n - 1) % 2
                for grp in range(2):
                    g4 = slice(grp * 4, grp * 4 + 4)
                    if n < 5:
                        pP, rP = self.bank((0, 1, 2, 3))
                        for h4 in range(4):
                            h = grp * 4 + h4
                            self.mm(pP[:, h4 * 128:(h4 + 1) * 128], PmT[prv][:, h, :], Pm[prv][:, h, :], r=[r_PT[prv], r_P[prv]] + ra, w=[rP])
                        self.cp("act", Pm[cur][:, g4, :], pP.rearrange("p (a b) -> p a b", a=4), r=[rP] + ra, w=[r_P[cur]])
                    pPT, rPT = self.bank((0, 1, 2, 3))
                    for h4 in range(4):
                        h = grp * 4 + h4
                        self.mm(pPT[:, h4 * 128:(h4 + 1) * 128], Pm[prv][:, h, :], PmT[prv][:, h, :], r=[r_PT[prv], r_P[prv]] + ra, w=[rPT])
                    self.cp("dve", PmT[cur][:, g4, :], pPT.rearrange("p (a b) -> p a b", a=4), r=[rPT] + ra, w=[r_PT[cur]])
                    pR, rR = self.bank((0, 1, 2, 3))
                    for h4 in range(4):
                        h = grp * 4 + h4
                        self.mm(pR[:, h4 * 128:(h4 + 1) * 128], PmT[cur][:, h, :], Rm[prv][:, h, :], start=True, stop=False, r=[r_PT[cur], r_R[prv]] + ra, w=[rR])
                        self.mm(pR[:, h4 * 128:(h4 + 1) * 128], idb, Rm[prv][:, h, :], start=False, stop=True, r=[self.r_c, r_R[prv]] + ra, w=[rR])
                    self.cp("act" if grp == 0 else "dve", Rm[cur][:, g4, :], pR.rearrange("p (a b) -> p a b", a=4), r=[rR] + ra, w=[r_R[cur]])
            Rf, r_Rf = Rm[1], r_R[1]
            self.dump("Rf", Rf, [r_Rf], BF16)
            for grp in range(2):
                g4 = slice(grp * 4, grp * 4 + 4)
                pU, rU = self.bank((0, 1, 2, 3))
                for h4 in range(4):
                    h = grp * 4 + h4
                    self.mm(pU[:, h4 * 128:(h4 + 1) * 128], Rf[:, h, :], vtok[:, h, :], r=[r_Rf, r_vt] + ra, w=[rU])
                self.cp("act", u0[:, g4, :], pU.rearrange("p (a b) -> p a b", a=4), r=[rU], w=[r_u0])
                pW, rW = self.bank((0, 1, 2, 3))
                for h4 in range(4):
                    h = grp * 4 + h4
                    self.mm(pW[:, h4 * 128:(h4 + 1) * 128], kdtok[:, h, :], Rf[:, h, :], r=[r_Rf, r_kd] + ra, w=[rW])
                self.cp("dve", w0T[:, g4, :], pW.rearrange("p (a b) -> p a b", a=4), r=[rW] + ra, w=[r_w0])
            self.dump("u0", u0, [r_u0], F32)
            self.dump("w0T", w0T, [r_w0], BF16)
            po = [self.bank((4,)), self.bank((5,))]
            for e in range(2):
                ps_ = slice(e * 64, (e + 1) * 64)
                cs = slice(b * 128 + e * 64, b * 128 + (e + 1) * 64)
                for grp in range(2):
                    pws, rws = self.bank((0, 1, 2, 3))
                    for h4 in range(4):
                        h = grp * 4 + h4
                        self.mm(pws[ps_, h4 * 128:(h4 + 1) * 128], w0T[:, h, e * 64:(e + 1) * 64], Sb[:, h, :], r=[r_w0, rSb] + ra, w=[rws])
                        self.mm(po[grp][0][ps_, h4 * 128:(h4 + 1) * 128], qkv[:, h, cs], Sb[:, h, :], r=[r_qc[h], rSb] + ra, w=[po[grp][1]])
                    g4 = slice(grp * 4, grp * 4 + 4)
                    self.tt("dve", self.vtmp[ps_, :, :], u0[ps_, g4, :], pws[ps_, :].rearrange("p (a b) -> p a b", a=4), ALU.subtract,
                            r=[r_u0, rws], w=[self.r_vtmp])
                    self.tt("dve", vnew[ps_, g4, :], self.vtmp[ps_, :, :],
                            sc3[ps_, b, 16 + grp * 4:20 + grp * 4].unsqueeze(2).broadcast_to([64, 4, 128]), ALU.mult,
                            r=[self.r_vtmp, r_sc] + ra, w=[r_vn])
                    pS, rpS = self.bank((6, 7))
                    for h4 in range(4):
                        h = grp * 4 + h4
                        self.mm(pS[:, h4 * 128:(h4 + 1) * 128], ktok[ps_, h, :], vnew[ps_, h, :], r=[r_kt, r_vn] + ra, w=[rpS])
                    self.tt("dve", S[:, g4, :], S[:, g4, :], edl[:, b, e * 8 + grp * 4:e * 8 + grp * 4 + 4].unsqueeze(2).broadcast_to([128, 4, 128]),
                            ALU.mult, r=[rS, self.r_edl], w=[rS])
                    self.tt("dve", S[:, g4, :], S[:, g4, :], pS.rearrange("p (a b) -> p a b", a=4), ALU.add, r=[rS, rpS], w=[rS])
                    self.cp("act", Sb[:, g4, :], S[:, g4, :], r=[rS], w=[rSb])
            for grp in range(2):
                g4 = slice(grp * 4, grp * 4 + 4)
                self.tt("dve", otok[:, g4, :], po[grp][0].rearrange("p (a b) -> p a b", a=4),
                        sc3[:, b, 40 + grp * 4:44 + grp * 4].unsqueeze(2).broadcast_to([128, 4, 128]), ALU.mult,
                        r=[po[grp][1], r_sc], w=[self.r_otok])
                pI, rI = self.bank((0, 1, 2, 3))
                for h4 in range(4):
                    h = grp * 4 + h4
                    self.mm(pI[:, h4 * 128:(h4 + 1) * 128], qkT[:, h, :], vnew[:, h, :], r=[r_qk, r_vn] + ra, w=[rI])
                self.tt("dve", otok[:, g4, :], otok[:, g4, :], pI.rearrange("p (a b) -> p a b", a=4), ALU.add, r=[self.r_otok, rI], w=[self.r_otok])
            self.dump("vnew", vnew, [r_vn], BF16)
            self.dump("otok", otok, [self.r_otok], F32)
            if b == 3:
                self.dump("otok3", otok, [self.r_otok], F32)
            ssq = self.ssq
            osq = f[:, 5632:6656].rearrange("p (a b) -> p a b", a=8)
            self.act(osq, otok, AF.Square, r=[self.r_otok], w=[self.r_junk])
            self.P.op("dve", lambda e, o_=ssq[:, 0:8], i_=osq: e.reduce_sum(out=o_, in_=i_, axis=mybir.AxisListType.X), reads=[self.r_junk], writes=[self.r_ssq])
            self.ts("dve", ssq[:, 0:8], ssq[:, 0:8], 1.0 / 128.0, None, ALU.mult, r=[self.r_ssq], w=[self.r_ssq])
            self.act(ssq[:, 0:8], ssq[:, 0:8], AF.Ln, r=[self.r_ssq, self.r_c], w=[self.r_ssq], bias=self.epsb[:])
            self.act(ssq[:, 0:8], ssq[:, 0:8], AF.Exp, r=[self.r_ssq], w=[self.r_ssq], scale=-0.5)
            self.tt("dve", otok, otok, ssq[:, 0:8].unsqueeze(2).broadcast_to([128, 8, 128]), ALU.mult, r=[self.r_otok, self.r_ssq], w=[self.r_otok])
            self.tt("dve", ogt[:, :], otok.rearrange("p a b -> p (a b)"), rtok, ALU.mult, r=[self.r_otok, r_rt] + ra, w=[r_ogt])
            self.dump("ogt", ogt, [r_ogt], BF16)
            self.dump("rtok", rtok, [r_rt], F32)
            self.dump("ssq", self.ssq, [self.r_ssq], F32)
            for grp in range(2):
                pG, rG = self.bank((0, 1, 2, 3))
                pG_b = self.pbank_bf[self._last_bank_id((0, 1, 2, 3))]
                for h4 in range(4):
                    c = grp * 4 + h4
                    self.tr(pG_b[:, h4 * 128:(h4 + 1) * 128], ogt[:, c * 128:(c + 1) * 128], idb, r=[r_ogt, self.r_c] + ra, w=[rG])
                self.cp("act", og[:, grp * 4:grp * 4 + 4, bc], pG_b[:, 0:512].rearrange("p (a b) -> p a b", a=4), r=[rG] + ra, w=[r_og])
        self.dump("og", og, [r_og], BF16)
        self.dump("Sd", S, [rS], F32)
        self.out_proj(og, r_og, ra)

    def dump(self, name, ap, reads, dt):
        if not getattr(self, "debug", False) or name in self._dumped:
            return
        self._dumped.add(name)
        d = self.nc.dram_tensor("dbg_" + name, list(ap.shape), dt, kind="ExternalOutput").ap()
        self.P.dma("sp", lambda e: e.dma_start(out=d, in_=ap), reads=reads, key="dbg_" + name)

    def _last_bank_id(self, pool):
        k = self._bctr[pool] - 1
        return pool[k % len(pool)]

    def build(self):
        nc = bass.Bass("TRN2", target_bir_lowering=False)
        self.nc = nc
        depth, nseq, ntile = self.depth, self.nseq, self.ntile
        ntok = nseq * ntile * T
        x_d = nc.dram_tensor("x", [ntok, D], F32, kind="ExternalInput").ap()
        wk_d = nc.dram_tensor("wk", [self.NB, 128, 4096], F32, kind="ExternalInput").ap()
        sm_d = nc.dram_tensor("sm", [128, NSM], F32, kind="ExternalInput").ap()
        cf_d = nc.dram_tensor("cf", [128, NCF], F32, kind="ExternalInput").ap()
        cb_d = nc.dram_tensor("cb", [128, NCB], F32, kind="ExternalInput").ap()
        y_d = nc.dram_tensor("y", [ntok, D], F32, kind="ExternalOutput").ap()
        self.wkb = nc.dram_tensor("wkb", [self.NB, 128, 4096], BF16, kind="Internal").ap()
        self.blk_layer = []
        for l in range(depth):
            nmix = self.nblk_layer[l] - 34
            self.blk_layer += [l * 3] * 17 + [l * 3 + 1] * nmix + [l * 3 + 2] * 17
        with contextlib.ExitStack() as st:
            def sb(name, shape, dt):
                return st.enter_context(nc.sbuf_tensor("s_" + name, shape, dt))
            P = self.P = Prog(nc)
            self._bctr = {}
            self._dumped = set()
            pall = st.enter_context(nc.psum_tensor("pall", [128, 8 * 512], F32))
            self.pbank = [pall[:, k * 512:(k + 1) * 512] for k in range(8)]
            self.rbank = [Res("bank%d" % k) for k in range(8)]
            self.pbank_bf = [pall[:, k * 512:(k + 1) * 512].bitcast(BF16) for k in range(8)]
            self.xT = sb("xT", [128, 8, T], F32)
            self.hT = sb("hT", [128, 8, T], BF16)
            self.sq = sb("sq", [128, 4, T], BF16)
            self.rstd = sb("rstd", [128, T], F32)[:]
            self.r_x, self.r_h, self.r_rstd = Res("x"), Res("h"), Res("rstd")
            self.r_sqs = [Res("sq%d" % k) for k in range(4)]
            self.xT, self.hT, self.sq = self.xT[:], self.hT[:], self.sq[:]
            self.tmpf = [sb("tmpf%d" % k, [128, T], F32)[:] for k in range(2)]
            self.r_tmpf = [Res("tmpf%d" % k) for k in range(2)]
            self.sgbuf = [sb("sg%d" % k, [128, T], F32)[:] for k in range(2)]
            self.r_sg = [Res("sg%d" % k) for k in range(2)]
            self.NWK = 4
            wring = [sb("wring%d" % k, [128, 4096], BF16) for k in range(self.NWK)]
            self.wring = [w_[:] for w_ in wring]
            self.wring3 = [w_[:].rearrange("p (a b) -> p a b", a=8) for w_ in wring]
            self.r_wring = [Res("wring%d" % k) for k in range(self.NWK)]
            self.wk_i = 0
            self.arena = sb("arena", [128, 2 * 516 + 24 * 512 + 4096 + 13 * 1024 + 64], BF16)[:]
            self.r_arena = Res("arena")
            self.r_av = [Res("av%d" % k, parent=self.r_arena) for k in range(24)]
            self.f32a = sb("f32a", [128, 6656], F32)[:]
            self.cell = sb("cell", [128, 2], F32)
            self.cf = sb("cf", [128, NCF], F32)[:]
            self.cb = sb("cb", [128, NCB], BF16)[:]
            self.sm = sb("sm", [128, S_WZ], F32)[:]
            self.smb = sb("smb", [128, 512], BF16)[:]
            self.wgkb = sb("wgkb", [16, 1024], BF16)[:]
            self.bgkb = sb("bgkb", [1, 1024], BF16)[:]
            self.epsb = sb("epsb", [128, 1], F32)
            self.oneb = sb("oneb", [128, 1], F32)
            self.negA = sb("negA", [128, 16], F32)[:]
            self.id4 = sb("id4", [128, 4, 128], BF16)[:]
            self.xtmp = sb("xtmp", [128, 4, 128], F32)[:]
            self.r_xtmp = Res("xtmp")
            self.vtmp = sb("vtmp", [128, 4, 128], F32)[:]
            self.r_vtmp = Res("vtmp")
            self.edl = sb("edl", [128, 4, 16], F32)[:]
            self.r_edl = Res("edl", strict=True)
            self.ssq = sb("ssq", [128, 8], F32)[:]
            self.r_ssq = Res("ssq", strict=True)
            self.r_qc = [Res("qc%d" % k, parent=self.r_arena) for k in range(24)]
            self.r_cv = [Res("cv%d" % k, parent=self.r_arena) for k in range(8)]
            self.r_scs = Res("scs", parent=self.r_arena, strict=True)
            self.r_junk = Res("junk")
            self.r_otok = Res("otok", parent=self.r_arena)
            self.r_c = Res("consts", strict=True)
            self.Sg = [sb("Sg%d" % k, [128, 4, 256], F32)[:] for k in range(2)]
            self.Sgb = [sb("Sgb%d" % k, [128, 4, 256], BF16)[:] for k in range(2)]
            self.Sd = [sb("Sd%d" % k, [128, 8, 128], F32)[:] for k in range(2)]
            self.Sdb = [sb("Sdb%d" % k, [128, 8, 128], BF16)[:] for k in range(2)]
            self.ctail = [sb("ct%d" % k, [128, 24, 3], BF16)[:] for k in range(2)]
            self.r_Sg = [Res("Sg%d" % k) for k in range(2)]
            self.r_Sgb = [Res("Sgb%d" % k) for k in range(2)]
            self.r_Sd = [Res("Sd%d" % k) for k in range(2)]
            self.r_Sdb = [Res("Sdb%d" % k) for k in range(2)]
            self.r_ctail = [Res("ct%d" % k) for k in range(2)]
            xin = [self.arena[:, k * 2048:(k + 1) * 2048].bitcast(F32) for k in range(2)]
            r_xin = [Res("xin%d" % k) for k in range(2)]
            rc = self.r_c
            P.dma("sp", lambda e: e.dma_start(out=self.cf, in_=cf_d), writes=[rc], key="c0")
            P.dma("sp", lambda e: e.dma_start(out=self.sm, in_=sm_d[:, 0:S_WZ]), writes=[rc], key="c0")
            P.dma("pool", lambda e: e.dma_start(out=self.cb, in_=cb_d), writes=[rc], key="c1")
            P.dma("pool", lambda e: e.dma_start(out=self.smb[:, 0:256], in_=sm_d[:, S_WZ:S_WZ + 256]), writes=[rc], key="c1")
            P.dma("pool", lambda e: e.dma_start(out=self.smb[:, 256:512], in_=sm_d[:, S_WAB:S_WAB + 256]), writes=[rc], key="c1")
            P.dma("pool", lambda e: e.dma_start(out=self.wgkb, in_=sm_d[0:16, S_WGK:S_WGK + 1024]), writes=[rc], key="c1")
            P.dma("pool", lambda e: e.dma_start(out=self.bgkb, in_=sm_d[0:1, S_BGK:S_BGK + 1024]), writes=[rc], key="c1")
            self.memset("dve", self.epsb[:], EPS, w=[rc])
            self.memset("dve", self.oneb[:], 1.0, w=[rc])
            for k in range(4):
                self.cp("dve", self.id4[:, k, :], self.cb[:, B_ID:B_ID + 128], r=[rc], w=[rc])
            self.act(self.negA, self.sm[:, S_ALOG:S_ALOG + 16], AF.Exp, r=[rc], w=[rc])
            self.ts("dve", self.negA, self.negA, -1.0, None, ALU.mult, r=[rc], w=[rc])
            self.r_wkb = [Res("wkb%d" % l) for l in range(3 * depth)]
            bi = 0
            for l in range(depth):
                for k in range(self.nblk_layer[l]):
                    src, dst = wk_d[bi], self.wkb[bi]
                    P.dma("pool", lambda e, s_=src, d_=dst: e.dma_start(out=d_, in_=s_), writes=[self.r_wkb[self.blk_layer[bi]]], key="cwk%d" % self.blk_layer[bi])
                    bi += 1
            idf = self.cf[:, C_ID:C_ID + 128]
            r_yout = [Res("yout%d" % k) for k in range(2)]
            for s in range(nseq):
                for t in range(ntile):
                    tok0 = (s * ntile + t) * T
                    self.wk_next = 0
                    self.arena_switch()
                    for b in range(4):
                        xi, rxi = xin[b % 2], r_xin[b % 2]
                        src = x_d[tok0 + b * 128:tok0 + (b + 1) * 128, :]
                        P.dma("sp", lambda e, s_=src, d_=xi: e.dma_start(out=d_, in_=s_), reads=[self.r_arena], writes=[rxi], key="xin%d" % (b % 2))
                        for half in range(2):
                            pt, rt = self.bank()
                            for c4 in range(4):
                                c = half * 4 + c4
                                self.tr(pt[:, c4 * 128:(c4 + 1) * 128], xi[:, c * 128:(c + 1) * 128], idf, r=[rxi, rc, self.r_arena], w=[rt])
                            self.cp("act" if half == 0 else "dve", self.xT[:, half * 4:half * 4 + 4, b * 128:(b + 1) * 128],
                                    pt.rearrange("p (a b) -> p a b", a=4), r=[rt], w=[self.r_x])
                    for l in range(depth):
                        j = l // 2
                        self.ffn(l, 0)
                        if self.stop_after == (l, 0):
                            break
                        if _layer_is_gla(l):
                            self.gla(l, j, t == 0)
                        else:
                            self.gdn(l, j, t == 0)
                        if self.stop_after == (l, 1):
                            break
                        self.ffn(l, 1)
                        if self.stop_after == (l, 2):
                            break
                    if self.stop_after is None:
                        self.rmsnorm_final()
                        src_T = self.f32a[:, 0:4096].rearrange("p (a b) -> p a b", a=8)
                        r_src = self.r_av[23]
                    else:
                        self.arena_switch()
                        src_T, r_src = self.xT, self.r_x
                    for b in range(4):
                        yo, ryo = xin[b % 2], r_xin[b % 2]
                        for half in range(2):
                            pt, rt = self.bank()
                            for c4 in range(4):
                                c = half * 4 + c4
                                self.tr(pt[:, c4 * 128:(c4 + 1) * 128], src_T[:, c, b * 128:(b + 1) * 128], idf, r=[r_src, rc], w=[rt])
                            self.cp("act" if half == 0 else "dve", yo[:, half * 512:(half + 1) * 512], pt, r=[rt, self.r_arena], w=[ryo])
                        dst = y_d[tok0 + b * 128:tok0 + (b + 1) * 128, :]
                        P.dma("sp", lambda e, s_=yo, d_=dst: e.dma_start(out=d_, in_=s_), reads=[ryo, self.r_arena], writes=[], key="xin%d" % (b % 2))
            P.emit()
        return nc

    def rmsnorm_final(self):
        xT, sq = self.xT, self.sq
        self.arena_switch()
        outT = self.f32a[:, 0:4096].rearrange("p (a b) -> p a b", a=8)
        r_out = self.r_av[23]
        pb, rb = self.bank()
        for c in range(8):
            self.act(sq[:, c % 4, :], xT[:, c, :], AF.Square, r=[self.r_x], w=[self.r_sqs[c % 4]])
            self.mm(pb, self.cb[:, B_O1024:B_O1024 + 128], sq[:, c % 4, :], start=(c == 0), stop=(c == 7), r=[self.r_sqs[c % 4], self.r_c], w=[rb])
        rstd = self.rstd
        self.act(rstd, pb, AF.Ln, r=[rb, self.r_c], w=[self.r_rstd], bias=self.epsb[:])
        self.act(rstd, rstd, AF.Exp, r=[self.r_rstd], w=[self.r_rstd], scale=-0.5)
        for c in range(8):
            self.stt("dve", outT[:, c, :], xT[:, c, :], self.sm[:, S_FNORM + c:S_FNORM + c + 1], rstd, ALU.mult, ALU.mult,
                     r=[self.r_x, self.r_rstd, self.r_c, self.r_arena], w=[r_out])


_CACHE = {}


def kernel(**inputs):
    inp = {k: np.asarray(v) for k, v in inputs.items()}
    depth, ncore = 4, 8
    wk, sm = _prep_weights(inp, depth)
    cf, cb = _consts()
    x = np.ascontiguousarray(inp["x"], dtype=np.float32).reshape(ncore, 2 * SEQ, D)
    if "nc" not in _CACHE:
        _CACHE["nc"] = Builder(depth=depth, nseq=2, ntile=SEQ // T).build()
    nc = _CACHE["nc"]
    in_maps = [{"x": x[c], "wk": wk, "sm": sm, "cf": cf, "cb": cb} for c in range(ncore)]
    res = run_bass_kernel_spmd(nc, in_maps, core_ids=list(range(ncore)))
    y = np.stack([np.asarray(res.results[c]["y"]) for c in range(ncore)])
    return y.reshape(16, SEQ, D).astype(np.float32)
```

```python
import contextlib
import numpy as np
import concourse.bass as bass
import concourse.mybir as mybir
from concourse.bass_utils import run_bass_kernel_spmd

F32 = mybir.dt.float32
BF16 = mybir.dt.bfloat16
AF = mybir.ActivationFunctionType
ALU = mybir.AluOpType

D = 1024
SEQ = 4096
T = 512
EPS = 1e-6
DFF = 2816
ENGS = ("pe", "act", "dve", "pool", "sp")
EPOCH = 30000


class Res:
    __slots__ = ("name", "last_w", "readers", "parent", "strict")

    def __init__(self, name, parent=None, strict=False):
        self.name = name
        self.last_w = None
        self.readers = []
        self.parent = parent
        self.strict = strict


class Op:
    __slots__ = ("eng", "fn", "waits", "signal", "is_dma", "key", "sig")

    def __init__(self, eng, fn, is_dma=False, key=None):
        self.eng = eng
        self.fn = fn
        self.waits = []
        self.signal = False
        self.is_dma = is_dma
        self.key = key
        self.sig = None


class Prog:
    def __init__(self, nc):
        self.nc = nc
        self.ops = {e: [] for e in ENGS}
        self.all_ops = []
        self.dma_keys = {}

    def _dep(self, op, prod, strict=False):
        if prod is None or prod is op:
            return
        if prod.eng == op.eng and not prod.is_dma and not strict:
            return
        op.waits.append(prod)
        prod.signal = True

    def _track(self, op, reads, writes):
        extra = [r.parent for r in list(reads) + list(writes) if r.parent is not None]
        if extra:
            reads = list(reads) + [p for p in extra if p not in reads and p not in writes]
        for r in reads:
            self._dep(op, r.last_w, r.strict)
        for w in writes:
            self._dep(op, w.last_w, w.strict)
            for rd in w.readers:
                self._dep(op, rd)
        for r in reads:
            if not op.is_dma:
                r.readers = [x for x in r.readers if x.is_dma or x.eng != op.eng]
            r.readers.append(op)
        for w in writes:
            w.last_w = op
            w.readers = []

    def op(self, eng, fn, reads=(), writes=()):
        o = Op(eng, fn)
        self._track(o, reads, writes)
        self.ops[eng].append(o)
        self.all_ops.append(o)
        return o

    def dma(self, eng, fn, reads=(), writes=(), key="d"):
        o = Op(eng, fn, is_dma=True, key=key)
        o.signal = True
        self._track(o, reads, writes)
        self.ops[eng].append(o)
        self.all_ops.append(o)
        self.dma_keys.setdefault(key, 0)
        return o

    def emit(self):
        nc = self.nc
        cnt = {e: 0 for e in ENGS}
        dcnt = {k: 0 for k in self.dma_keys}
        for o in self.all_ops:
            if o.is_dma:
                dcnt[o.key] += 16
                o.sig = ("dma", o.key, dcnt[o.key])
            elif o.signal:
                cnt[o.eng] += 1
                ep, v = divmod(cnt[o.eng] - 1, EPOCH)
                o.sig = ("eng", (o.eng, ep), v + 1)
        sem_names = []
        for e in ENGS:
            for ep in range(max(1, (cnt[e] + EPOCH - 1) // EPOCH)):
                sem_names.append(("eng", (e, ep)))
        for k in self.dma_keys:
            sem_names.append(("dma", k))
        with contextlib.ExitStack() as st:
            sems = {}
            for i, sn in enumerate(sem_names):
                sems[sn] = st.enter_context(nc.semaphore("s%d" % i))
            block = st.enter_context(nc.Block())
            prog = self

            def make(ename):
                def body(eng):
                    waited = {}
                    for o in prog.ops[ename]:
                        need = {}
                        for p in o.waits:
                            kind, k, v = p.sig
                            sn = (kind, k)
                            if need.get(sn, 0) < v:
                                need[sn] = v
                        for sn, v in need.items():
                            if waited.get(sn, 0) >= v:
                                continue
                            eng.wait_ge(sems[sn], v)
                            waited[sn] = v
                        ins = o.fn(eng)
                        if o.is_dma:
                            ins.then_inc(sems[("dma", o.key)], 16)
                        elif o.signal:
                            ins.then_inc(sems[(o.sig[0], o.sig[1])], 1)
                    if ename == "sp":
                        for k, tot in dcnt.items():
                            if tot > 0 and waited.get(("dma", k), 0) < tot:
                                eng.wait_ge(sems[("dma", k)], tot)
                return body

            block.tensor(make("pe"))
            block.scalar(make("act"))
            block.vector(make("dve"))
            block.gpsimd(make("pool"))
            block.sync(make("sp"))


C_ID, C_LI, C_US, C_LS, C_CE, C_CO, C_ONE = 0, 128, 256, 384, 512, 640, 768
NCF = 896
B_ID, B_LI, B_LS4, B_TRB, B_TR2, B_O1024, B_O256, B_O1, B_LI4 = 0, 128, 256, 768, 896, 1024, 1152, 1280, 1408
NCB = 1920

S_NORM = 0
S_FNORM = 96
S_GLAN = 104
S_CONV = 108
S_ALOG = 300
S_DTB = 316
S_GDNN = 332
S_WZ = 588
S_WAB = 844
S_WGK = 1100
S_BGK = 2124
NSM = 3148


def _consts():
    cf = np.zeros((128, NCF), np.float32)
    m = np.arange(128)
    same = (m[:, None] // 64) == (m[None, :] // 64)
    li = ((m[:, None] <= m[None, :]) & same).astype(np.float32)
    ls = ((m[:, None] < m[None, :]) & same).astype(np.float32)
    us = ((m[:, None] > m[None, :]) & same).astype(np.float32)
    cf[:, C_ID:C_ID + 128] = np.eye(128)
    cf[:, C_LI:C_LI + 128] = li
    cf[:, C_US:C_US + 128] = us
    cf[:, C_LS:C_LS + 128] = ls
    cf[:, C_CE:C_CE + 128] = (m[:, None] < 64).astype(np.float32) * np.ones((1, 128), np.float32)
    cf[:, C_CO:C_CO + 128] = (m[:, None] >= 64).astype(np.float32) * np.ones((1, 128), np.float32)
    cf[:, C_ONE:C_ONE + 128] = 1.0
    cb = np.zeros((128, NCB), np.float32)
    cb[:, B_ID:B_ID + 128] = np.eye(128)
    cb[:, B_LI:B_LI + 128] = li
    cb[:, B_LS4:B_LS4 + 512] = np.tile(ls, (1, 4))
    cb[:, B_TRB:B_TRB + 128] = -li / 16.0
    cb[:, B_TR2:B_TR2 + 128] = -us / 16.0
    cb[:, B_O1024:B_O1024 + 128] = 1.0 / 1024.0
    cb[:, B_O256:B_O256 + 128] = 1.0 / 256.0
    cb[:, B_O1:B_O1 + 128] = 1.0
    cb[:, B_LI4:B_LI4 + 512] = np.tile(li, (1, 4))
    return cf, cb


def _kblocks(W, col_lists):
    out = []
    for cols in col_lists:
        sub = W[:, cols]
        out.append(sub.reshape(8, 128, 512).transpose(1, 0, 2).reshape(128, 4096))
    return out


_SWAP = [False]


def _layer_is_gla(l):
    return (l % 2 == 0) != _SWAP[0]


def _prep_weights(inp, depth):
    wk = []
    ar = np.arange
    for l in range(depth):
        j = l // 2
        for which in range(2):
            if which == 1:
                pass
        def ffn_blocks(i):
            W = inp["ffn_w_in"][l, i]
            lists = [np.concatenate([ar(b * 256, (b + 1) * 256), DFF + ar(b * 256, (b + 1) * 256)]) for b in range(11)]
            wk.extend(_kblocks(W, lists))
            Wo = inp["ffn_w_out"][l, i]
            for half in range(2):
                for g in range(3):
                    nf = 8 if g < 2 else 6
                    sub = Wo[g * 1024:g * 1024 + nf * 128, half * 512:(half + 1) * 512]
                    blk = np.zeros((128, 8, 512), np.float32)
                    blk[:, 0:nf, :] = sub.reshape(nf, 128, 512).transpose(1, 0, 2)
                    wk.append(blk.reshape(128, 4096))
        ffn_blocks(0)
        if _layer_is_gla(l):
            W = inp["gla_w_in"][j]
            lists = [ar(512, 1024), ar(0, 512), ar(1024, 1536), ar(1536, 2048), ar(2048, 2560), ar(2560, 3072)]
            wk.extend(_kblocks(W, lists))
            wk.extend(_kblocks(inp["gla_w_out"][j], [ar(0, 512), ar(512, 1024)]))
        else:
            W = inp["gdn_w_in"][j]
            lists = [ar(b * 512, (b + 1) * 512) for b in range(8)]
            wk.extend(_kblocks(W, lists))
            wk.extend(_kblocks(inp["gdn_w_out"][j], [ar(0, 512), ar(512, 1024)]))
        ffn_blocks(1)
    sm = np.zeros((128, NSM), np.float32)
    sm[:, S_NORM:S_NORM + 96] = inp["norm_w"].reshape(12, 8, 128).transpose(2, 0, 1).reshape(128, 96)
    sm[:, S_FNORM:S_FNORM + 8] = inp["final_norm_w"].reshape(8, 128).T
    sm[:, S_GLAN:S_GLAN + 4] = inp["gla_norm_w"].reshape(2, 2, 128).transpose(2, 0, 1).reshape(128, 4)
    sm[:, S_CONV:S_CONV + 192] = inp["gdn_conv_w"].reshape(2, 4, 24, 128).transpose(3, 0, 1, 2).reshape(128, 192)
    sm[:, S_ALOG:S_ALOG + 16] = inp["gdn_a_log"].reshape(1, 16)
    sm[:, S_DTB:S_DTB + 16] = inp["gdn_dt_bias"].reshape(1, 16)
    sm[:, S_GDNN:S_GDNN + 256] = inp["gdn_norm_w"].reshape(1, 256)
    for j in range(2):
        wz = inp["gla_w_in"][j][:, 3072:3088]
        sm[:, S_WZ + j * 128:S_WZ + (j + 1) * 128] = wz.reshape(8, 128, 16).transpose(1, 0, 2).reshape(128, 128)
        wab = inp["gdn_w_in"][j][:, 4096:4112]
        sm[:, S_WAB + j * 128:S_WAB + (j + 1) * 128] = wab.reshape(8, 128, 16).transpose(1, 0, 2).reshape(128, 128)
        sm[0:16, S_WGK + j * 512:S_WGK + (j + 1) * 512] = inp["gla_w_gk"][j]
        sm[0:1, S_BGK + j * 512:S_BGK + (j + 1) * 512] = inp["gla_b_gk"][j][None, :]
    return np.ascontiguousarray(np.stack(wk)), sm


class Builder:
    def __init__(self, depth=4, nseq=2, ntile=8, stop_after=None):
        self.depth, self.nseq, self.ntile = depth, nseq, ntile
        self.stop_after = stop_after
        self.nblk_layer = [42 if _layer_is_gla(l) else 44 for l in range(depth)]
        self.NB = sum(self.nblk_layer)

    def mm(self, out, lhsT, rhs, start=True, stop=True, r=(), w=()):
        self.P.op("pe", lambda e: e.matmul(out, lhsT, rhs, start=start, stop=stop, skip_group_check=True), reads=r, writes=w)

    def tr(self, out, in_, ident, r=(), w=()):
        self.P.op("pe", lambda e: e.transpose(out, in_, ident), reads=r, writes=w)

    def act(self, out, in_, func, r=(), w=(), bias=None, scale=None, accum=None, eng="act"):
        kw = {}
        if bias is not None:
            kw["bias"] = bias
        if scale is not None:
            kw["scale"] = scale
        if accum is not None:
            kw["accum_out"] = accum
        self.P.op("act", lambda e: e.activation(out=out, in_=in_, func=func, **kw), reads=r, writes=w)

    def tt(self, eng, out, in0, in1, op, r=(), w=()):
        self.P.op(eng, lambda e: e.tensor_tensor(out=out, in0=in0, in1=in1, op=op), reads=r, writes=w)

    def ts(self, eng, out, in0, s1, s2, op0, op1=None, r=(), w=()):
        if op1 is None:
            self.P.op(eng, lambda e: e.tensor_scalar(out=out, in0=in0, scalar1=s1, scalar2=None, op0=op0), reads=r, writes=w)
        else:
            self.P.op(eng, lambda e: e.tensor_scalar(out=out, in0=in0, scalar1=s1, scalar2=s2, op0=op0, op1=op1), reads=r, writes=w)

    def stt(self, eng, out, in0, scalar, in1, op0, op1, r=(), w=()):
        self.P.op(eng, lambda e: e.scalar_tensor_tensor(out=out, in0=in0, scalar=scalar, in1=in1, op0=op0, op1=op1), reads=r, writes=w)

    def cp(self, eng, out, in_, r=(), w=()):
        if eng == "act":
            self.P.op("act", lambda e: e.activation(out=out, in_=in_, func=AF.Copy), reads=r, writes=w)
        else:
            self.P.op(eng, lambda e: e.tensor_copy(out=out, in_=in_), reads=r, writes=w)

    def memset(self, eng, ap, val, w=()):
        self.P.op(eng, lambda e: e.memset(ap, val), writes=w)

    def bank(self, pool=None):
        pool = pool or (0, 1, 2, 3, 4, 5, 6, 7)
        k = self._bctr.get(pool, 0)
        self._bctr[pool] = k + 1
        b = pool[k % len(pool)]
        return self.pbank[b], self.rbank[b]

    def load_wk(self):
        i = self.wk_i
        self.wk_i += 1
        slot = i % self.NWK
        blk = self.wk_next
        self.wk_next += 1
        src = self.wkb[blk]
        dst = self.wring[slot]
        layer_res = self.r_wkb[self.blk_layer[blk]]
        self.P.dma("sp", lambda e: e.dma_start(out=dst, in_=src), reads=[layer_res], writes=[self.r_wring[slot]], key="wk%d" % slot)
        return self.wring3[slot], self.r_wring[slot]

    def arena_switch(self):
        cell = self.cell
        self.P.op("dve", lambda e: e.memset(cell[:], 0.0), writes=[self.r_arena])

    def rmsnorm(self, wcol):
        xT, hT, sq = self.xT, self.hT, self.sq
        pb, rb = self.bank()
        for c in range(8):
            self.act(sq[:, c % 4, :], xT[:, c, :], AF.Square, r=[self.r_x], w=[self.r_sqs[c % 4]])
            self.mm(pb, self.cb[:, B_O1024:B_O1024 + 128], sq[:, c % 4, :], start=(c == 0), stop=(c == 7), r=[self.r_sqs[c % 4], self.r_c], w=[rb])
        rstd = self.rstd
        self.act(rstd, pb, AF.Ln, r=[rb, self.r_c], w=[self.r_rstd], bias=self.epsb[:])
        self.act(rstd, rstd, AF.Exp, r=[self.r_rstd], w=[self.r_rstd], scale=-0.5)
        for c in range(8):
            self.stt("dve", hT[:, c, :], xT[:, c, :], self.sm[:, wcol + c:wcol + c + 1], rstd, ALU.mult, ALU.mult,
                     r=[self.r_x, self.r_rstd, self.r_c], w=[self.r_h])

    def ffn(self, l, i):
        self.rmsnorm(S_NORM + (l * 3 + 2 * i) * 8)
        self.arena_switch()
        A = self.arena
        actb = A[:, 0:22 * 512].rearrange("p (a b) -> p a b", a=22)
        ra = [self.r_arena]
        r_act = self.r_av[0]
        hT = self.hT
        gu = (4, 5, 6, 7)
        for jb in range(11):
            w, rw = self.load_wk()
            for half in range(2):
                fc = jb * 2 + half
                pg, rg = self.bank(gu)
                pu, ru = self.bank(gu)
                for kc in range(8):
                    self.mm(pg, w[:, kc, half * 128:(half + 1) * 128], hT[:, kc, :], start=(kc == 0), stop=(kc == 7), r=[rw, self.r_h], w=[rg])
                for kc in range(8):
                    self.mm(pu, w[:, kc, 256 + half * 128:256 + (half + 1) * 128], hT[:, kc, :], start=(kc == 0), stop=(kc == 7), r=[rw, self.r_h], w=[ru])
                sg, rsg = self.sgbuf[fc % 2], self.r_sg[fc % 2]
                self.act(sg, pg, AF.Silu, r=[rg], w=[rsg])
                self.tt("dve", actb[:, fc, :], sg, pu, ALU.mult, r=[rsg, ru] + ra, w=[r_act])
        for half in range(2):
            pool = (0, 1, 2, 3) if half == 0 else (4, 5, 6, 7)
            bks = [self.bank(pool) for _ in range(4)]
            for g in range(3):
                wo, rwo = self.load_wk()
                for f in range(8 if g < 2 else 6):
                    fc = g * 8 + f
                    for d in range(4):
                        self.mm(bks[d][0], wo[:, f, d * 128:(d + 1) * 128], actb[:, fc, :], start=(fc == 0), stop=(fc == 21),
                                r=[rwo, r_act] + ra, w=[bks[d][1]])
            for d in range(4):
                dc = half * 4 + d
                self.stt("dve", self.xT[:, dc, :], bks[d][0], 0.5, self.xT[:, dc, :], ALU.mult, ALU.add, r=[bks[d][1], self.r_x], w=[self.r_x])

    def gla(self, l, j, first_tile):
        self.rmsnorm(S_NORM + (l * 3 + 1) * 8)
        self.arena_switch()
        A, ra = self.arena, [self.r_arena]
        hT, cb, cf = self.hT, self.cb, self.cf
        o = 0

        def carve(n, shape3=None):
            nonlocal o
            v = A[:, o:o + n]
            o += n
            return v
        qd = carve(2048).rearrange("p (a b) -> p a b", a=4)
        ki = carve(2048).rearrange("p (a b) -> p a b", a=4)
        ke = carve(2048).rearrange("p (a b) -> p a b", a=4)
        vt = carve(4096).rearrange("p (a b) -> p a b", a=4)
        lg = carve(2048).rearrange("p (a b) -> p a b", a=4)
        rs = carve(4096).rearrange("p (a b) -> p a b", a=8)
        og = carve(4096).rearrange("p (a b) -> p a b", a=8)
        zT = carve(512)
        AT = carve(1024).rearrange("p (a b) -> p a b", a=2)
        r_qd, r_ki, r_ke, r_vt, r_lg, r_rs, r_og, r_zT, r_oT, r_eb = self.r_av[0:10]
        r_AT = self.r_av[10:12]
        oT = self.f32a[:, 0:4096].rearrange("p (a b) -> p a b", a=8)
        eb = self.f32a[:, 4096:6144].rearrange("p (a b) -> p a b", a=4)
        S, Sb = self.Sg[j], self.Sgb[j]
        rS, rSb = self.r_Sg[j], self.r_Sgb[j]
        sm = self.sm
        if first_tile:
            self.memset("pool", S[:], 0.0, w=[rS])
            self.memset("pool", Sb[:], 0.0, w=[rSb])
        pz, rz = self.bank()
        for kc in range(8):
            self.mm(pz[0:16, :], self.smb[:, j * 128 + kc * 16:j * 128 + kc * 16 + 16], hT[:, kc, :], start=(kc == 0), stop=(kc == 7),
                    r=[self.r_h, self.r_c], w=[rz])
        self.cp("act", zT[0:16, :], pz[0:16, :], r=[rz] + ra, w=[r_zT])
        for b in range(4):
            pl, rl = self.bank()
            self.mm(pl, zT[0:16, b * 128:(b + 1) * 128], self.wgkb[0:16, j * 512:(j + 1) * 512], start=True, stop=False, r=[r_zT, self.r_c] + ra, w=[rl])
            self.mm(pl, self.cb[0:1, B_O1:B_O1 + 128], self.bgkb[0:1, j * 512:(j + 1) * 512], start=False, stop=True, r=[self.r_c], w=[rl])
            t0, rt0 = self.tmpf[b % 2], self.r_tmpf[b % 2]
            self.act(t0, pl, AF.Exp, r=[rl], w=[rt0], scale=-1.0)
            self.act(lg[:, b, :], t0, AF.Ln, r=[rt0, self.r_c] + ra, w=[r_lg], bias=self.oneb[:])
        w, rw = self.load_wk()
        for b in range(4):
            pk, rk = self.bank()
            for kc in range(8):
                self.mm(pk, hT[:, kc, b * 128:(b + 1) * 128], w[:, kc, :], start=(kc == 0), stop=(kc == 7), r=[rw, self.r_h], w=[rk])
            pd, rd = self.bank()
            self.mm(pd, cb[:, B_TR2:B_TR2 + 128], lg[:, b, :], r=[r_lg, self.r_c] + ra, w=[rd])
            t0, rt0 = self.tmpf[b % 2], self.r_tmpf[b % 2]
            self.act(t0, pd, AF.Exp, r=[rd], w=[rt0])
            self.tt("dve", ke[:, b, :], pk, t0, ALU.mult, r=[rk, rt0] + ra, w=[r_ke])
        for h in range(4):
            pk, rk = self.bank()
            for kc in range(8):
                self.mm(pk, w[:, kc, h * 128:(h + 1) * 128], hT[:, kc, :], start=(kc == 0), stop=(kc == 7), r=[rw, self.r_h], w=[rk])
            pb, rb = self.bank()
            for b in range(4):
                self.mm(pb[:, b * 128:(b + 1) * 128], lg[:, b, h * 128:(h + 1) * 128], cb[:, B_TRB:B_TRB + 128], r=[r_lg, self.r_c] + ra, w=[rb])
            t0, rt0 = self.tmpf[h % 2], self.r_tmpf[h % 2]
            self.act(t0, pb, AF.Exp, r=[rb], w=[rt0], scale=-1.0)
            self.act(eb[:, h, :], pb, AF.Exp, r=[rb], w=[r_eb])
            self.tt("dve", ki[:, h, :], pk, t0, ALU.mult, r=[rk, rt0] + ra, w=[r_ki])
        w, rw = self.load_wk()
        for h in range(4):
            pq, rq = self.bank()
            for kc in range(8):
                self.mm(pq, w[:, kc, h * 128:(h + 1) * 128], hT[:, kc, :], start=(kc == 0), stop=(kc == 7), r=[rw, self.r_h], w=[rq])
            self.stt("dve", qd[:, h, :], pq, 128.0 ** -0.5, eb[:, h, :], ALU.mult, ALU.mult, r=[rq, r_eb] + ra, w=[r_qd])
        for vb in range(2):
            w, rw = self.load_wk()
            for b in range(4):
                pv, rv = self.bank()
                for kc in range(8):
                    self.mm(pv, hT[:, kc, b * 128:(b + 1) * 128], w[:, kc, :], start=(kc == 0), stop=(kc == 7), r=[rw, self.r_h], w=[rv])
                self.cp("act" if b % 2 == 0 else "dve", vt[:, b, vb * 512:(vb + 1) * 512], pv, r=[rv] + ra, w=[r_vt])
        for rb_ in range(2):
            w, rw = self.load_wk()
            for cc in range(4):
                pr, rr = self.bank()
                for kc in range(8):
                    self.mm(pr, w[:, kc, cc * 128:(cc + 1) * 128], hT[:, kc, :], start=(kc == 0), stop=(kc == 7), r=[rw, self.r_h], w=[rr])
                self.act(rs[:, rb_ * 4 + cc, :], pr, AF.Silu, r=[rr] + ra, w=[r_rs])
        SP = (3, 4, 5, 6, 7)
        for b in range(4):
            bc = slice(b * 128, (b + 1) * 128)
            pA, rA = self.bank((0,))
            for h in range(4):
                self.mm(pA[:, h * 128:(h + 1) * 128], ki[:, h, bc], qd[:, h, bc], r=[r_ki, r_qd] + ra, w=[rA])
            at, rat = AT[:, b % 2, :], r_AT[b % 2]
            self.tt("dve", at, pA, cb[:, B_LI4:B_LI4 + 512], ALU.mult, r=[rA, self.r_c] + ra, w=[rat])
            po = [self.bank((1,)), self.bank((2,))]
            for pob, rob in po:
                self.memset("dve", pob, 0.0, w=[rob])
            for e in range(2):
                cs = slice(b * 128 + e * 64, b * 128 + (e + 1) * 64)
                last = b * 128 + e * 64 + 63
                for h in range(4):
                    pob, rob = po[h // 2]
                    for half in range(2):
                        c0 = (h % 2) * 256 + half * 128 + e * 64
                        self.mm(pob[:, c0:c0 + 64], Sb[:, h, half * 128:(half + 1) * 128], qd[:, h, cs], start=False, stop=False,
                                r=[rSb, r_qd] + ra, w=[rob])
                    pS, rpS = self.bank(SP)
                    self.mm(pS[:, 0:256], ke[e * 64:(e + 1) * 64, b, h * 128:(h + 1) * 128], vt[e * 64:(e + 1) * 64, b, h * 256:(h + 1) * 256],
                            r=[r_ke, r_vt] + ra, w=[rpS])
                    self.stt("dve", S[:, h, :], S[:, h, :], eb[:, h, last:last + 1], pS[:, 0:256], ALU.mult, ALU.add, r=[rS, r_eb, rpS], w=[rS])
                    self.cp("act", Sb[:, h, :], S[:, h, :], r=[rS], w=[rSb])
            for h in range(4):
                pob, rob = po[h // 2]
                for half in range(2):
                    c0 = (h % 2) * 256 + half * 128
                    self.mm(pob[:, c0:c0 + 128], vt[:, b, h * 256 + half * 128:h * 256 + (half + 1) * 128], at[:, h * 128:(h + 1) * 128],
                            start=False, stop=True, r=[r_vt, rat] + ra, w=[rob])
            for h2 in range(2):
                pob, rob = po[h2]
                self.cp("act", oT[:, h2 * 4:(h2 + 1) * 4, bc], pob.rearrange("p (a b) -> p a b", a=4), r=[rob], w=[r_oT])
        sq = self.sq
        for h in range(4):
            pn, rn = self.bank()
            for half in range(2):
                k4 = (h * 2 + half) % 4
                self.act(sq[:, k4, :], oT[:, h * 2 + half, :], AF.Square, r=[r_oT], w=[self.r_sqs[k4]])
                self.mm(pn, cb[:, B_O256:B_O256 + 128], sq[:, k4, :], start=(half == 0), stop=(half == 1), r=[self.r_sqs[k4], self.r_c], w=[rn])
            t0, rt0 = self.tmpf[h % 2], self.r_tmpf[h % 2]
            self.act(t0, pn, AF.Ln, r=[rn, self.r_c], w=[rt0], bias=self.epsb[:])
            self.act(t0, t0, AF.Exp, r=[rt0], w=[rt0], scale=-0.5)
            for half in range(2):
                c = h * 2 + half
                self.stt("dve", oT[:, c, :], oT[:, c, :], sm[:, S_GLAN + j * 2 + half:S_GLAN + j * 2 + half + 1], t0, ALU.mult, ALU.mult,
                         r=[r_oT, rt0, self.r_c], w=[r_oT])
                self.tt("dve", og[:, c, :], oT[:, c, :], rs[:, c, :], ALU.mult, r=[r_oT, r_rs] + ra, w=[r_og])
        self.out_proj(og, r_og, ra)

    def out_proj(self, og, r_og, ra):
        for blk in range(2):
            w, rw = self.load_wk()
            for dq in range(4):
                dc = blk * 4 + dq
                py, ry = self.bank()
                for vc in range(8):
                    self.mm(py, w[:, vc, dq * 128:(dq + 1) * 128], og[:, vc, :], start=(vc == 0), stop=(vc == 7), r=[rw, r_og] + ra, w=[ry])
                self.tt("dve", self.xT[:, dc, :], py, self.xT[:, dc, :], ALU.add, r=[ry, self.r_x], w=[self.r_x])

    def gdn(self, l, j, first_tile):
        self.rmsnorm(S_NORM + (l * 3 + 1) * 8)
        self.arena_switch()
        A, ra = self.arena, [self.r_arena]
        hT, cb, cf, sm = self.hT, self.cb, self.cf, self.sm
        o = 0

        def carve(n):
            nonlocal o
            v = A[:, o:o + n]
            o += n
            return v
        xc2 = carve(2 * 516).rearrange("p (a b) -> p a b", a=2)
        qkv = carve(24 * 512).rearrange("p (a b) -> p a b", a=24)
        og = carve(4096).rearrange("p (a b) -> p a b", a=8)
        ogt = carve(1024)
        ktok = carve(1024).rearrange("p (a b) -> p a b", a=8)
        kdtok = carve(1024).rearrange("p (a b) -> p a b", a=8)
        vtok = carve(1024).rearrange("p (a b) -> p a b", a=8)
        Pm = [carve(1024).rearrange("p (a b) -> p a b", a=8) for _ in range(2)]
        PmT = [carve(1024).rearrange("p (a b) -> p a b", a=8) for _ in range(2)]
        Rm = [carve(1024).rearrange("p (a b) -> p a b", a=8) for _ in range(2)]
        qkT = carve(1024).rearrange("p (a b) -> p a b", a=8)
        w0T = carve(1024).rearrange("p (a b) -> p a b", a=8)
        vnew = carve(1024).rearrange("p (a b) -> p a b", a=8)
        (r_xc0, r_qkv, r_og, r_ogt, r_kt, r_kd, r_vt, r_P0, r_P1, r_PT0, r_PT1, r_R0, r_R1, r_qk, r_w0, r_vn, r_rt, r_u0, r_sc, r_gu, r_dt) = self.r_av[0:21]
        r_P, r_PT, r_R = [r_P0, r_P1], [r_PT0, r_PT1], [r_R0, r_R1]
        r_sc = self.r_scs
        r_xcs = [r_xc0, self.r_av[21]]
        r_qc = self.r_qc
        f = self.f32a
        rtok = f[:, 0:1024]
        u0 = f[:, 1024:2048].rearrange("p (a b) -> p a b", a=8)
        gu = f[:, 2048:3072].rearrange("p (a b) -> p a b", a=8)
        dtm = f[:, 3072:4096].rearrange("p (a b) -> p a b", a=8)
        sc = f[:, 4096:4096 + 256]
        otok = f[:, 4608:5632].rearrange("p (a b) -> p a b", a=8)
        dls = f[:, 6656:7680].rearrange("p (a b) -> p a b", a=8)
        r_dls = self.r_av[22]
        S, Sb = self.Sd[j], self.Sdb[j]
        rS, rSb = self.r_Sd[j], self.r_Sdb[j]
        ct, rct = self.ctail[j], self.r_ctail[j]
        if first_tile:
            self.memset("pool", S[:], 0.0, w=[rS])
            self.memset("pool", Sb[:], 0.0, w=[rSb])
            self.memset("pool", ct[:], 0.0, w=[rct])
        pab, rab = self.bank()
        for b in range(4):
            for kc in range(8):
                self.mm(pab[:, b * 16:(b + 1) * 16], hT[:, kc, b * 128:(b + 1) * 128], self.smb[:, 256 + j * 128 + kc * 16:256 + j * 128 + kc * 16 + 16],
                        start=(kc == 0), stop=(kc == 7), r=[self.r_h, self.r_c], w=[rab])
        pab3 = pab[:, 0:64].rearrange("p (b c) -> p b c", b=4)
        sc3 = sc.rearrange("p (b c) -> p b c", b=4)
        for b in range(4):
            self.tt("dve", sc3[:, b, 0:8], pab3[:, b, 0:8], sm[:, S_DTB + j * 8:S_DTB + j * 8 + 8], ALU.add, r=[rab, self.r_c], w=[r_sc])
        self.act(sc3[:, :, 0:8], sc3[:, :, 0:8], AF.Exp, r=[r_sc], w=[r_sc])
        self.act(sc3[:, :, 0:8], sc3[:, :, 0:8], AF.Ln, r=[r_sc, self.r_c], w=[r_sc], bias=self.oneb[:])
        for b in range(4):
            self.tt("dve", sc3[:, b, 8:16], sc3[:, b, 0:8], self.negA[:, j * 8:j * 8 + 8], ALU.mult, r=[r_sc, self.r_c], w=[r_sc])
        self.act(sc3[:, :, 16:24], pab3[:, :, 8:16], AF.Exp, r=[rab], w=[r_sc], scale=-1.0)
        self.act(sc3[:, :, 16:24], sc3[:, :, 16:24], AF.Ln, r=[r_sc, self.r_c], w=[r_sc], bias=self.oneb[:])
        self.act(sc3[:, :, 16:24], sc3[:, :, 16:24], AF.Exp, r=[r_sc], w=[r_sc], scale=-1.0)
        pdd, rdd = self.bank()
        for b in range(4):
            g_b = sc3[:, b, 8:16]
            self.mm(pdd[:, b * 16:b * 16 + 8], cf[:, C_LI:C_LI + 128], g_b, r=[r_sc, self.r_c], w=[rdd])
            self.mm(pdd[:, b * 16 + 8:b * 16 + 16], cf[:, C_US:C_US + 128], g_b, r=[r_sc, self.r_c], w=[rdd])
            self.mm(pdd[:, 64 + b * 16:64 + b * 16 + 8], cf[:, C_CE:C_CE + 128], g_b, r=[r_sc, self.r_c], w=[rdd])
            self.mm(pdd[:, 64 + b * 16 + 8:64 + b * 16 + 16], cf[:, C_CO:C_CO + 128], g_b, r=[r_sc, self.r_c], w=[rdd])
        pdd3 = pdd[:, 0:64].rearrange("p (b c) -> p b c", b=4)
        self.act(sc3[:, :, 40:56], pdd3, AF.Exp, r=[rdd], w=[r_sc])
        edl = self.edl
        self.act(edl, pdd[:, 64:128].rearrange("p (b c) -> p b c", b=4), AF.Exp, r=[rdd], w=[self.r_edl])
        t0s = [f[:, k * 512:(k + 1) * 512] for k in range(4)]
        t2s = [f[:, 2048 + k * 512:2048 + (k + 1) * 512] for k in range(4)]
        r_t0s, r_t2s = self.r_cv[0:4], self.r_cv[4:8]
        wcur = [None, None]
        pending, ready = [], []

        def stage_a(cc):
            if cc % 4 == 0:
                wcur[0], wcur[1] = self.load_wk()
            w, rw = wcur
            cq = cc % 4
            pp, rp = self.bank((0, 1, 2, 3))
            for kc in range(8):
                self.mm(pp, w[:, kc, cq * 128:(cq + 1) * 128], hT[:, kc, :], start=(kc == 0), stop=(kc == 7), r=[rw, self.r_h], w=[rp])
            xc, r_xc = xc2[:, cc % 2, :], r_xcs[cc % 2]
            self.cp("pool", xc[:, 0:3], ct[:, cc, :], r=[rct] + ra, w=[r_xc])
            self.cp("act", xc[:, 3:515], pp, r=[rp] + ra, w=[r_xc])
            t0, rt0 = t0s[cc % 4], r_t0s[cc % 4]
            cw = S_CONV + (j * 4) * 24 + cc
            self.ts("dve", t0, xc[:, 3:515], sm[:, cw + 3 * 24:cw + 3 * 24 + 1], None, ALU.mult, r=[r_xc, self.r_c] + ra, w=[rt0])
            for tap in (2, 1, 0):
                self.stt("dve", t0, xc[:, tap:tap + 512], sm[:, cw + tap * 24:cw + tap * 24 + 1], t0, ALU.mult, ALU.add,
                         r=[r_xc, rt0, self.r_c] + ra, w=[rt0])
            self.cp("pool", ct[:, cc, :], xc[:, 512:515], r=[r_xc] + ra, w=[rct])

        def stage_b(cc):
            t0, rt0 = t0s[cc % 4], r_t0s[cc % 4]
            self.act(qkv[:, cc, :], t0, AF.Silu, r=[rt0] + ra, w=[r_qc[cc]])
            if cc < 16:
                k4 = cc % 4
                self.act(self.sq[:, k4, :], qkv[:, cc, :], AF.Square, r=[r_qc[cc]] + ra, w=[self.r_sqs[k4]])
                pn, rn = self.bank((4, 5, 6, 7))
                self.mm(pn, cb[:, B_O1:B_O1 + 128], self.sq[:, k4, :], r=[self.r_sqs[k4], self.r_c], w=[rn])
                pending.append((cc, pn, rn))

        def stage_c(items):
            for k, (cc, pn, rn) in enumerate(items):
                self.act(t2s[k], pn, AF.Ln, r=[rn, self.r_c], w=[r_t2s[k]], bias=self.epsb[:])
            for k, (cc, pn, rn) in enumerate(items):
                self.act(t2s[k], t2s[k], AF.Exp, r=[r_t2s[k]], w=[r_t2s[k]], scale=-0.5)
            for k, (cc, pn, rn) in enumerate(items):
                if cc < 8:
                    self.stt("dve", qkv[:, cc, :], qkv[:, cc, :], 128.0 ** -0.5, t2s[k], ALU.mult, ALU.mult, r=[r_qc[cc], r_t2s[k]] + ra, w=[r_qc[cc]])
                else:
                    self.tt("dve", qkv[:, cc, :], qkv[:, cc, :], t2s[k], ALU.mult, r=[r_qc[cc], r_t2s[k]] + ra, w=[r_qc[cc]])

        for it in range(25):
            if it < 24:
                stage_a(it)
            if ready:
                stage_c(ready)
                ready = []
            if it >= 1:
                stage_b(it - 1)
            if len(pending) == 4:
                ready, pending = pending, []
        if ready:
            stage_c(ready)
        if pending:
            stage_c(pending)
        self.dump("sc", sc, [r_sc], F32)
        self.dump("edl", edl, [self.r_edl], F32)
        self.dump("qkv", qkv, r_qc, BF16)
        wr = [self.load_wk(), self.load_wk()]
        idb = cb[:, B_ID:B_ID + 128]
        for b in range(4):
            bc = slice(b * 128, (b + 1) * 128)
            for rb_ in range(2):
                w, rw = wr[rb_]
                pr, rr = self.bank((6, 7))
                for kc in range(8):
                    self.mm(pr, hT[:, kc, bc], w[:, kc, :], start=(kc == 0), stop=(kc == 7), r=[rw, self.r_h], w=[rr])
                self.act(rtok[:, rb_ * 512:(rb_ + 1) * 512], pr, AF.Silu, r=[rr], w=[r_rt])
                self.tt("dve", rtok[:, rb_ * 512:(rb_ + 1) * 512].rearrange("p (a b) -> p a b", a=4),
                        rtok[:, rb_ * 512:(rb_ + 1) * 512].rearrange("p (a b) -> p a b", a=4),
                        sm[:, S_GDNN + j * 128:S_GDNN + (j + 1) * 128].unsqueeze(1).broadcast_to([128, 4, 128]), ALU.mult,
                        r=[r_rt, self.r_c], w=[r_rt])
            ptk, rtk = self.bank((6, 7))
            ptk_b = self.pbank_bf[self._last_bank_id((6, 7))]
            for h in range(8):
                self.tr(ptk_b[:, h * 128:(h + 1) * 128], qkv[:, 8 + h, bc], idb, r=[r_qc[8 + h], self.r_c] + ra, w=[rtk])
            pk3 = ptk_b[:, 0:1024].rearrange("p (a b) -> p a b", a=8)
            self.tt("dve", ktok, pk3, sc3[:, b, 48:56].unsqueeze(2).broadcast_to([128, 8, 128]), ALU.mult, r=[rtk, r_sc] + ra, w=[r_kt])
            self.tt("dve", kdtok, pk3, sc3[:, b, 40:48].unsqueeze(2).broadcast_to([128, 8, 128]), ALU.mult, r=[rtk, r_sc] + ra, w=[r_kd])
            ptv, rtv = self.bank((6, 7))
            ptv_b = self.pbank_bf[self._last_bank_id((6, 7))]
            for h in range(8):
                self.tr(ptv_b[:, h * 128:(h + 1) * 128], qkv[:, 16 + h, bc], idb, r=[r_qc[16 + h], self.r_c] + ra, w=[rtv])
            self.cp("act", vtok, ptv_b[:, 0:1024].rearrange("p (a b) -> p a b", a=8), r=[rtv] + ra, w=[r_vt])
            self.tt("dve", gu, cf[:, C_US:C_US + 128].unsqueeze(1).broadcast_to([128, 8, 128]),
                    sc3[:, b, 8:16].unsqueeze(2).broadcast_to([128, 8, 128]), ALU.mult, r=[self.r_c, r_sc], w=[r_gu])
            for grp in range(2):
                pD, rD = self.bank((4, 5))
                for h4 in range(4):
                    h = grp * 4 + h4
                    self.mm(pD[:, h4 * 128:(h4 + 1) * 128], gu[:, h, :], cf[:, C_LI:C_LI + 128], r=[r_gu, self.r_c], w=[rD])
                t0, rt0 = self.tmpf[grp], self.r_tmpf[grp]
                self.act(t0, pD, AF.Exp, r=[rD], w=[rt0])
                self.tt("dve", dtm[:, grp * 4:grp * 4 + 4, :], t0.rearrange("p (a b) -> p a b", a=4),
                        cb[:, B_LI4:B_LI4 + 512].rearrange("p (a b) -> p a b", a=4),
                        ALU.mult, r=[rt0, self.r_c], w=[r_dt])
                self.stt("dve", dls[:, grp * 4:grp * 4 + 4, :], t0.rearrange("p (a b) -> p a b", a=4), -1.0,
                         cb[:, B_LS4:B_LS4 + 512].rearrange("p (a b) -> p a b", a=4), ALU.mult, ALU.mult, r=[rt0, self.r_c], w=[r_dls])
            for grp in range(2):
                pX, rX = self.bank((4, 5))
                for h4 in range(4):
                    h = grp * 4 + h4
                    self.mm(pX[:, h4 * 128:(h4 + 1) * 128], qkv[:, 8 + h, bc], qkv[:, 8 + h, bc], r=[r_qc[8 + h]] + ra, w=[rX])
                self.tt("dve", self.xtmp, pX.rearrange("p (a b) -> p a b", a=4),
                        sc3[:, b, 16 + grp * 4:20 + grp * 4].unsqueeze(2).broadcast_to([128, 4, 128]), ALU.mult, r=[rX, r_sc], w=[self.r_xtmp])
                self.tt("dve", Pm[0][:, grp * 4:grp * 4 + 4, :], self.xtmp, dls[:, grp * 4:grp * 4 + 4, :], ALU.mult,
                        r=[self.r_xtmp, r_dls] + ra, w=[r_P[0]])
                pQ, rQ = self.bank((4, 5))
                for h4 in range(4):
                    h = grp * 4 + h4
                    self.mm(pQ[:, h4 * 128:(h4 + 1) * 128], qkv[:, 8 + h, bc], qkv[:, h, bc], r=[r_qc[8 + h], r_qc[h]] + ra, w=[rQ])
                self.tt("dve", qkT[:, grp * 4:grp * 4 + 4, :], pQ.rearrange("p (a b) -> p a b", a=4), dtm[:, grp * 4:grp * 4 + 4, :], ALU.mult,
                        r=[rQ, r_dt] + ra, w=[r_qk])
            self.dump("dtm", dtm, [r_dt], F32)
            self.dump("P0", Pm[0], [r_P[0]], BF16)
            self.dump("qkT", qkT, [r_qk], BF16)
            self.dump("ktok", ktok, [r_kt], BF16)
            self.dump("vtok", vtok, [r_vt], BF16)
            pT, rT = self.bank((4, 5))
            pT_b = self.pbank_bf[self._last_bank_id((4, 5))]
            for h in range(8):
                self.tr(pT_b[:, h * 128:(h + 1) * 128], Pm[0][:, h, :], idb, r=[r_P[0], self.r_c] + ra, w=[rT])
            self.cp("act", PmT[0], pT_b[:, 0:1024].rearrange("p (a b) -> p a b", a=8), r=[rT] + ra, w=[r_PT[0]])
            for grp in range(2):
                g4 = slice(grp * 4, grp * 4 + 4)
                self.tt("pool", Rm[0][:, g4, :], Pm[0][:, g4, :], self.id4, ALU.add, r=[r_P[0], self.r_c] + ra, w=[r_R[0]])
            for n in range(1, 6):
                cur, prv = n % 2, (n - 1) % 2
                for grp in range(2):
                    g4 = slice(grp * 4, grp * 4 + 4)
                    if n < 5:
                        pP, rP = self.bank((0, 1, 2, 3))
                        for h4 in range(4):
                            h = grp * 4 + h4
                            self.mm(pP[:, h4 * 128:(h4 + 1) * 128], PmT[prv][:, h, :], Pm[prv][:, h, :], r=[r_PT[prv], r_P[prv]] + ra, w=[rP])
                        self.cp("act", Pm[cur][:, g4, :], pP.rearrange("p (a b) -> p a b", a=4), r=[rP] + ra, w=[r_P[cur]])
                    pPT, rPT = self.bank((0, 1, 2, 3))
                    for h4 in range(4):
                        h = grp * 4 + h4
                        self.mm(pPT[:, h4 * 128:(h4 + 1) * 128], Pm[prv][:, h, :], PmT[prv][:, h, :], r=[r_PT[prv], r_P[prv]] + ra, w=[rPT])
                    self.cp("dve", PmT[cur][:, g4, :], pPT.rearrange("p (a b) -> p a b", a=4), r=[rPT] + ra, w=[r_PT[cur]])
                    pR, rR = self.bank((0, 1, 2, 3))
                    for h4 in range(4):
                        h = grp * 4 + h4
                        self.mm(pR[:, h4 * 128:(h4 + 1) * 128], PmT[cur][:, h, :], Rm[prv][:, h, :], start=True, stop=False, r=[r_PT[cur], r_R[prv]] + ra, w=[rR])
                        self.mm(pR[:, h4 * 128:(h4 + 1) * 128], idb, Rm[prv][:, h, :], start=False, stop=True, r=[self.r_c, r_R[prv]] + ra, w=[rR])
                    self.cp("act" if grp == 0 else "dve", Rm[cur][:, g4, :], pR.rearrange("p (a b) -> p a b", a=4), r=[rR] + ra, w=[r_R[cur]])
            Rf, r_Rf = Rm[1], r_R[1]
            self.dump("Rf", Rf, [r_Rf], BF16)
            for grp in range(2):
                g4 = slice(grp * 4, grp * 4 + 4)
                pU, rU = self.bank((0, 1, 2, 3))
                for h4 in range(4):
                    h = grp * 4 + h4
                    self.mm(pU[:, h4 * 128:(h4 + 1) * 128], Rf[:, h, :], vtok[:, h, :], r=[r_Rf, r_vt] + ra, w=[rU])
                self.cp("act", u0[:, g4, :], pU.rearrange("p (a b) -> p a b", a=4), r=[rU], w=[r_u0])
                pW, rW = self.bank((0, 1, 2, 3))
                for h4 in range(4):
                    h = grp * 4 + h4
                    self.mm(pW[:, h4 * 128:(h4 + 1) * 128], kdtok[:, h, :], Rf[:, h, :], r=[r_Rf, r_kd] + ra, w=[rW])
                self.cp("dve", w0T[:, g4, :], pW.rearrange("p (a b) -> p a b", a=4), r=[rW] + ra, w=[r_w0])
            self.dump("u0", u0, [r_u0], F32)
            self.dump("w0T", w0T, [r_w0], BF16)
            po = [self.bank((4,)), self.bank((5,))]
            for e in range(2):
                ps_ = slice(e * 64, (e + 1) * 64)
                cs = slice(b * 128 + e * 64, b * 128 + (e + 1) * 64)
                for grp in range(2):
                    pws, rws = self.bank((0, 1, 2, 3))
                    for h4 in range(4):
                        h = grp * 4 + h4
                        self.mm(pws[ps_, h4 * 128:(h4 + 1) * 128], w0T[:, h, e * 64:(e + 1) * 64], Sb[:, h, :], r=[r_w0, rSb] + ra, w=[rws])
                        self.mm(po[grp][0][ps_, h4 * 128:(h4 + 1) * 128], qkv[:, h, cs], Sb[:, h, :], r=[r_qc[h], rSb] + ra, w=[po[grp][1]])
                    g4 = slice(grp * 4, grp * 4 + 4)
                    self.tt("dve", self.vtmp[ps_, :, :], u0[ps_, g4, :], pws[ps_, :].rearrange("p (a b) -> p a b", a=4), ALU.subtract,
                            r=[r_u0, rws], w=[self.r_vtmp])
                    self.tt("dve", vnew[ps_, g4, :], self.vtmp[ps_, :, :],
                            sc3[ps_, b, 16 + grp * 4:20 + grp * 4].unsqueeze(2).broadcast_to([64, 4, 128]), ALU.mult,
                            r=[self.r_vtmp, r_sc] + ra, w=[r_vn])
                    pS, rpS = self.bank((6, 7))
                    for h4 in range(4):
                        h = grp * 4 + h4
                        self.mm(pS[:, h4 * 128:(h4 + 1) * 128], ktok[ps_, h, :], vnew[ps_, h, :], r=[r_kt, r_vn] + ra, w=[rpS])
                    self.tt("dve", S[:, g4, :], S[:, g4, :], edl[:, b, e * 8 + grp * 4:e * 8 + grp * 4 + 4].unsqueeze(2).broadcast_to([128, 4, 128]),
                            ALU.mult, r=[rS, self.r_edl], w=[rS])
                    self.tt("dve", S[:, g4, :], S[:, g4, :], pS.rearrange("p (a b) -> p a b", a=4), ALU.add, r=[rS, rpS], w=[rS])
                    self.cp("act", Sb[:, g4, :], S[:, g4, :], r=[rS], w=[rSb])
            for grp in range(2):
                g4 = slice(grp * 4, grp * 4 + 4)
                self.tt("dve", otok[:, g4, :], po[grp][0].rearrange("p (a b) -> p a b", a=4),
                        sc3[:, b, 40 + grp * 4:44 + grp * 4].unsqueeze(2).broadcast_to([128, 4, 128]), ALU.mult,
                        r=[po[grp][1], r_sc], w=[self.r_otok])
                pI, rI = self.bank((0, 1, 2, 3))
                for h4 in range(4):
                    h = grp * 4 + h4
                    self.mm(pI[:, h4 * 128:(h4 + 1) * 128], qkT[:, h, :], vnew[:, h, :], r=[r_qk, r_vn] + ra, w=[rI])
                self.tt("dve", otok[:, g4, :], otok[:, g4, :], pI.rearrange("p (a b) -> p a b", a=4), ALU.add, r=[self.r_otok, rI], w=[self.r_otok])
            self.dump("vnew", vnew, [r_vn], BF16)
            self.dump("otok", otok, [self.r_otok], F32)
            if b == 3:
                self.dump("otok3", otok, [self.r_otok], F32)
            ssq = self.ssq
            osq = f[:, 5632:6656].rearrange("p (a b) -> p a b", a=8)
            self.act(osq, otok, AF.Square, r=[self.r_otok], w=[self.r_junk])
            self.P.op("dve", lambda e, o_=ssq[:, 0:8], i_=osq: e.reduce_sum(out=o_, in_=i_, axis=mybir.AxisListType.X), reads=[self.r_junk], writes=[self.r_ssq])
            self.ts("dve", ssq[:, 0:8], ssq[:, 0:8], 1.0 / 128.0, None, ALU.mult, r=[self.r_ssq], w=[self.r_ssq])
            self.act(ssq[:, 0:8], ssq[:, 0:8], AF.Ln, r=[self.r_ssq, self.r_c], w=[self.r_ssq], bias=self.epsb[:])
            self.act(ssq[:, 0:8], ssq[:, 0:8], AF.Exp, r=[self.r_ssq], w=[self.r_ssq], scale=-0.5)
            self.tt("dve", otok, otok, ssq[:, 0:8].unsqueeze(2).broadcast_to([128, 8, 128]), ALU.mult, r=[self.r_otok, self.r_ssq], w=[self.r_otok])
            self.tt("dve", ogt[:, :], otok.rearrange("p a b -> p (a b)"), rtok, ALU.mult, r=[self.r_otok, r_rt] + ra, w=[r_ogt])
            self.dump("ogt", ogt, [r_ogt], BF16)
            self.dump("rtok", rtok, [r_rt], F32)
            self.dump("ssq", self.ssq, [self.r_ssq], F32)
            pG, rG = self.bank((0, 1, 2, 3))
            pG_b = self.pbank_bf[self._last_bank_id((0, 1, 2, 3))]
            for c in range(8):
                self.tr(pG_b[:, c * 128:(c + 1) * 128], ogt[:, c * 128:(c + 1) * 128], idb, r=[r_ogt, self.r_c] + ra, w=[rG])
            self.cp("act", og[:, :, bc], pG_b[:, 0:1024].rearrange("p (a b) -> p a b", a=8), r=[rG] + ra, w=[r_og])
        self.dump("og", og, [r_og], BF16)
        self.dump("Sd", S, [rS], F32)
        self.out_proj(og, r_og, ra)

    def dump(self, name, ap, reads, dt):
        if not getattr(self, "debug", False) or name in self._dumped:
            return
        self._dumped.add(name)
        d = self.nc.dram_tensor("dbg_" + name, list(ap.shape), dt, kind="ExternalOutput").ap()
        self.P.dma("sp", lambda e: e.dma_start(out=d, in_=ap), reads=reads, key="dbg_" + name)

    def _last_bank_id(self, pool):
        k = self._bctr[pool] - 1
        return pool[k % len(pool)]

    def build(self):
        nc = bass.Bass("TRN2", target_bir_lowering=False)
        self.nc = nc
        depth, nseq, ntile = self.depth, self.nseq, self.ntile
        ntok = nseq * ntile * T
        x_d = nc.dram_tensor("x", [ntok, D], F32, kind="ExternalInput").ap()
        wk_d = nc.dram_tensor("wk", [self.NB, 128, 4096], F32, kind="ExternalInput").ap()
        sm_d = nc.dram_tensor("sm", [128, NSM], F32, kind="ExternalInput").ap()
        cf_d = nc.dram_tensor("cf", [128, NCF], F32, kind="ExternalInput").ap()
        cb_d = nc.dram_tensor("cb", [128, NCB], F32, kind="ExternalInput").ap()
        y_d = nc.dram_tensor("y", [ntok, D], F32, kind="ExternalOutput").ap()
        self.wkb = nc.dram_tensor("wkb", [self.NB, 128, 4096], BF16, kind="Internal").ap()
        self.blk_layer = []
        for l in range(depth):
            nmix = self.nblk_layer[l] - 34
            self.blk_layer += [l * 3] * 17 + [l * 3 + 1] * nmix + [l * 3 + 2] * 17
        with contextlib.ExitStack() as st:
            def sb(name, shape, dt):
                return st.enter_context(nc.sbuf_tensor("s_" + name, shape, dt))
            P = self.P = Prog(nc)
            self._bctr = {}
            self._dumped = set()
            pall = st.enter_context(nc.psum_tensor("pall", [128, 8 * 512], F32))
            self.pbank = [pall[:, k * 512:(k + 1) * 512] for k in range(8)]
            self.rbank = [Res("bank%d" % k) for k in range(8)]
            self.pbank_bf = [pall[:, k * 512:(k + 1) * 512].bitcast(BF16) for k in range(8)]
            self.xT = sb("xT", [128, 8, T], F32)
            self.hT = sb("hT", [128, 8, T], BF16)
            self.sq = sb("sq", [128, 4, T], BF16)
            self.rstd = sb("rstd", [128, T], F32)[:]
            self.r_x, self.r_h, self.r_rstd = Res("x"), Res("h"), Res("rstd")
            self.r_sqs = [Res("sq%d" % k) for k in range(4)]
            self.xT, self.hT, self.sq = self.xT[:], self.hT[:], self.sq[:]
            self.tmpf = [sb("tmpf%d" % k, [128, T], F32)[:] for k in range(2)]
            self.r_tmpf = [Res("tmpf%d" % k) for k in range(2)]
            self.sgbuf = [sb("sg%d" % k, [128, T], F32)[:] for k in range(2)]
            self.r_sg = [Res("sg%d" % k) for k in range(2)]
            self.NWK = 4
            wring = [sb("wring%d" % k, [128, 4096], BF16) for k in range(self.NWK)]
            self.wring = [w_[:] for w_ in wring]
            self.wring3 = [w_[:].rearrange("p (a b) -> p a b", a=8) for w_ in wring]
            self.r_wring = [Res("wring%d" % k) for k in range(self.NWK)]
            self.wk_i = 0
            self.arena = sb("arena", [128, 2 * 516 + 24 * 512 + 4096 + 13 * 1024 + 64], BF16)[:]
            self.r_arena = Res("arena")
            self.r_av = [Res("av%d" % k, parent=self.r_arena) for k in range(24)]
            self.f32a = sb("f32a", [128, 7680], F32)[:]
            self.cell = sb("cell", [128, 2], F32)
            self.cf = sb("cf", [128, NCF], F32)[:]
            self.cb = sb("cb", [128, NCB], BF16)[:]
            self.sm = sb("sm", [128, S_WZ], F32)[:]
            self.smb = sb("smb", [128, 512], BF16)[:]
            self.wgkb = sb("wgkb", [16, 1024], BF16)[:]
            self.bgkb = sb("bgkb", [1, 1024], BF16)[:]
            self.epsb = sb("epsb", [128, 1], F32)
            self.oneb = sb("oneb", [128, 1], F32)
            self.negA = sb("negA", [128, 16], F32)[:]
            self.id4 = sb("id4", [128, 4, 128], BF16)[:]
            self.xtmp = sb("xtmp", [128, 4, 128], F32)[:]
            self.r_xtmp = Res("xtmp")
            self.vtmp = sb("vtmp", [128, 4, 128], F32)[:]
            self.r_vtmp = Res("vtmp")
            self.edl = sb("edl", [128, 4, 16], F32)[:]
            self.r_edl = Res("edl", strict=True)
            self.ssq = sb("ssq", [128, 8], F32)[:]
            self.r_ssq = Res("ssq", strict=True)
            self.r_qc = [Res("qc%d" % k, parent=self.r_arena) for k in range(24)]
            self.r_cv = [Res("cv%d" % k, parent=self.r_arena) for k in range(8)]
            self.r_scs = Res("scs", parent=self.r_arena, strict=True)
            self.r_junk = Res("junk")
            self.r_otok = Res("otok", parent=self.r_arena)
            self.r_c = Res("consts", strict=True)
            self.Sg = [sb("Sg%d" % k, [128, 4, 256], F32)[:] for k in range(2)]
            self.Sgb = [sb("Sgb%d" % k, [128, 4, 256], BF16)[:] for k in range(2)]
            self.Sd = [sb("Sd%d" % k, [128, 8, 128], F32)[:] for k in range(2)]
            self.Sdb = [sb("Sdb%d" % k, [128, 8, 128], BF16)[:] for k in range(2)]
            self.ctail = [sb("ct%d" % k, [128, 24, 3], BF16)[:] for k in range(2)]
            self.r_Sg = [Res("Sg%d" % k) for k in range(2)]
            self.r_Sgb = [Res("Sgb%d" % k) for k in range(2)]
            self.r_Sd = [Res("Sd%d" % k) for k in range(2)]
            self.r_Sdb = [Res("Sdb%d" % k) for k in range(2)]
            self.r_ctail = [Res("ct%d" % k) for k in range(2)]
            xin = [self.arena[:, k * 2048:(k + 1) * 2048].bitcast(F32) for k in range(2)]
            r_xin = [Res("xin%d" % k) for k in range(2)]
            rc = self.r_c
            P.dma("sp", lambda e: e.dma_start(out=self.cf, in_=cf_d), writes=[rc], key="c0")
            P.dma("sp", lambda e: e.dma_start(out=self.sm, in_=sm_d[:, 0:S_WZ]), writes=[rc], key="c0")
            P.dma("pool", lambda e: e.dma_start(out=self.cb, in_=cb_d), writes=[rc], key="c1")
            P.dma("pool", lambda e: e.dma_start(out=self.smb[:, 0:256], in_=sm_d[:, S_WZ:S_WZ + 256]), writes=[rc], key="c1")
            P.dma("pool", lambda e: e.dma_start(out=self.smb[:, 256:512], in_=sm_d[:, S_WAB:S_WAB + 256]), writes=[rc], key="c1")
            P.dma("pool", lambda e: e.dma_start(out=self.wgkb, in_=sm_d[0:16, S_WGK:S_WGK + 1024]), writes=[rc], key="c1")
            P.dma("pool", lambda e: e.dma_start(out=self.bgkb, in_=sm_d[0:1, S_BGK:S_BGK + 1024]), writes=[rc], key="c1")
            self.memset("dve", self.epsb[:], EPS, w=[rc])
            self.memset("dve", self.oneb[:], 1.0, w=[rc])
            for k in range(4):
                self.cp("dve", self.id4[:, k, :], self.cb[:, B_ID:B_ID + 128], r=[rc], w=[rc])
            self.act(self.negA, self.sm[:, S_ALOG:S_ALOG + 16], AF.Exp, r=[rc], w=[rc])
            self.ts("dve", self.negA, self.negA, -1.0, None, ALU.mult, r=[rc], w=[rc])
            self.r_wkb = [Res("wkb%d" % l) for l in range(3 * depth)]
            bi = 0
            for l in range(depth):
                for k in range(self.nblk_layer[l]):
                    src, dst = wk_d[bi], self.wkb[bi]
                    P.dma("pool", lambda e, s_=src, d_=dst: e.dma_start(out=d_, in_=s_), writes=[self.r_wkb[self.blk_layer[bi]]], key="cwk%d" % self.blk_layer[bi])
                    bi += 1
            idf = self.cf[:, C_ID:C_ID + 128]
            r_yout = [Res("yout%d" % k) for k in range(2)]
            for s in range(nseq):
                for t in range(ntile):
                    tok0 = (s * ntile + t) * T
                    self.wk_next = 0
                    self.arena_switch()
                    for b in range(4):
                        xi, rxi = xin[b % 2], r_xin[b % 2]
                        src = x_d[tok0 + b * 128:tok0 + (b + 1) * 128, :]
                        P.dma("sp", lambda e, s_=src, d_=xi: e.dma_start(out=d_, in_=s_), reads=[self.r_arena], writes=[rxi], key="xin%d" % (b % 2))
                        for half in range(2):
                            pt, rt = self.bank()
                            for c4 in range(4):
                                c = half * 4 + c4
                                self.tr(pt[:, c4 * 128:(c4 + 1) * 128], xi[:, c * 128:(c + 1) * 128], idf, r=[rxi, rc, self.r_arena], w=[rt])
                            self.cp("act" if half == 0 else "dve", self.xT[:, half * 4:half * 4 + 4, b * 128:(b + 1) * 128],
                                    pt.rearrange("p (a b) -> p a b", a=4), r=[rt], w=[self.r_x])
                    for l in range(depth):
                        j = l // 2
                        self.ffn(l, 0)
                        if self.stop_after == (l, 0):
                            break
                        if _layer_is_gla(l):
                            self.gla(l, j, t == 0)
                        else:
                            self.gdn(l, j, t == 0)
                        if self.stop_after == (l, 1):
                            break
                        self.ffn(l, 1)
                        if self.stop_after == (l, 2):
                            break
                    if self.stop_after is None:
                        self.rmsnorm_final()
                        src_T = self.f32a[:, 0:4096].rearrange("p (a b) -> p a b", a=8)
                        r_src = self.r_av[23]
                    else:
                        self.arena_switch()
                        src_T, r_src = self.xT, self.r_x
                    for b in range(4):
                        yo, ryo = xin[b % 2], r_xin[b % 2]
                        for half in range(2):
                            pt, rt = self.bank()
                            for c4 in range(4):
                                c = half * 4 + c4
                                self.tr(pt[:, c4 * 128:(c4 + 1) * 128], src_T[:, c, b * 128:(b + 1) * 128], idf, r=[r_src, rc], w=[rt])
                            self.cp("act" if half == 0 else "dve", yo[:, half * 512:(half + 1) * 512], pt, r=[rt, self.r_arena], w=[ryo])
                        dst = y_d[tok0 + b * 128:tok0 + (b + 1) * 128, :]
                        P.dma("sp", lambda e, s_=yo, d_=dst: e.dma_start(out=d_, in_=s_), reads=[ryo, self.r_arena], writes=[], key="xin%d" % (b % 2))
            P.emit()
        return nc

    def rmsnorm_final(self):
        xT, sq = self.xT, self.sq
        self.arena_switch()
        outT = self.f32a[:, 0:4096].rearrange("p (a b) -> p a b", a=8)
        r_out = self.r_av[23]
        pb, rb = self.bank()
        for c in range(8):
            self.act(sq[:, c % 4, :], xT[:, c, :], AF.Square, r=[self.r_x], w=[self.r_sqs[c % 4]])
            self.mm(pb, self.cb[:, B_O1024:B_O1024 + 128], sq[:, c % 4, :], start=(c == 0), stop=(c == 7), r=[self.r_sqs[c % 4], self.r_c], w=[rb])
        rstd = self.rstd
        self.act(rstd, pb, AF.Ln, r=[rb, self.r_c], w=[self.r_rstd], bias=self.epsb[:])
        self.act(rstd, rstd, AF.Exp, r=[self.r_rstd], w=[self.r_rstd], scale=-0.5)
        for c in range(8):
            self.stt("dve", outT[:, c, :], xT[:, c, :], self.sm[:, S_FNORM + c:S_FNORM + c + 1], rstd, ALU.mult, ALU.mult,
                     r=[self.r_x, self.r_rstd, self.r_c, self.r_arena], w=[r_out])


_CACHE = {}


def kernel(**inputs):
    inp = {k: np.asarray(v) for k, v in inputs.items()}
    depth, ncore = 4, 8
    wk, sm = _prep_weights(inp, depth)
    cf, cb = _consts()
    x = np.ascontiguousarray(inp["x"], dtype=np.float32).reshape(ncore, 2 * SEQ, D)
    if "nc" not in _CACHE:
        _CACHE["nc"] = Builder(depth=depth, nseq=2, ntile=SEQ // T).build()
    nc = _CACHE["nc"]
    in_maps = [{"x": x[c], "wk": wk, "sm": sm, "cf": cf, "cb": cb} for c in range(ncore)]
    res = run_bass_kernel_spmd(nc, in_maps, core_ids=list(range(ncore)))
    y = np.stack([np.asarray(res.results[c]["y"]) for c in range(ncore)])
    return y.reshape(16, SEQ, D).astype(np.float32)
```

```python
import contextlib
import numpy as np
import concourse.bass as bass
import concourse.mybir as mybir
from concourse.bass_utils import run_bass_kernel_spmd

F32 = mybir.dt.float32
BF16 = mybir.dt.bfloat16
AF = mybir.ActivationFunctionType
ALU = mybir.AluOpType

D = 1024
SEQ = 4096
T = 512
EPS = 1e-6
DFF = 2816
ENGS = ("pe", "act", "dve", "pool", "sp")
EPOCH = 30000


class Res:
    __slots__ = ("name", "last_w", "readers", "parent", "strict")

    def __init__(self, name, parent=None, strict=False):
        self.name = name
        self.last_w = None
        self.readers = []
        self.parent = parent
        self.strict = strict


class Op:
    __slots__ = ("eng", "fn", "waits", "signal", "is_dma", "key", "sig")

    def __init__(self, eng, fn, is_dma=False, key=None):
        self.eng = eng
        self.fn = fn
        self.waits = []
        self.signal = False
        self.is_dma = is_dma
        self.key = key
        self.sig = None


class Prog:
    def __init__(self, nc):
        self.nc = nc
        self.ops = {e: [] for e in ENGS}
        self.all_ops = []
        self.dma_keys = {}

    def _dep(self, op, prod, strict=False):
        if prod is None or prod is op:
            return
        if prod.eng == op.eng and not prod.is_dma and not strict:
            return
        op.waits.append(prod)
        prod.signal = True

    def _track(self, op, reads, writes):
        extra = [r.parent for r in list(reads) + list(writes) if r.parent is not None]
        if extra:
            reads = list(reads) + [p for p in extra if p not in reads and p not in writes]
        for r in reads:
            self._dep(op, r.last_w, r.strict)
        for w in writes:
            self._dep(op, w.last_w, w.strict)
            for rd in w.readers:
                self._dep(op, rd)
        for r in reads:
            if not op.is_dma:
                r.readers = [x for x in r.readers if x.is_dma or x.eng != op.eng]
            r.readers.append(op)
        for w in writes:
            w.last_w = op
            w.readers = []

    def op(self, eng, fn, reads=(), writes=()):
        o = Op(eng, fn)
        self._track(o, reads, writes)
        self.ops[eng].append(o)
        self.all_ops.append(o)
        return o

    def dma(self, eng, fn, reads=(), writes=(), key="d"):
        o = Op(eng, fn, is_dma=True, key=key)
        o.signal = True
        self._track(o, reads, writes)
        self.ops[eng].append(o)
        self.all_ops.append(o)
        self.dma_keys.setdefault(key, 0)
        return o

    def emit(self):
        nc = self.nc
        cnt = {e: 0 for e in ENGS}
        dcnt = {k: 0 for k in self.dma_keys}
        for o in self.all_ops:
            if o.is_dma:
                dcnt[o.key] += 16
                o.sig = ("dma", o.key, dcnt[o.key])
            elif o.signal:
                cnt[o.eng] += 1
                ep, v = divmod(cnt[o.eng] - 1, EPOCH)
                o.sig = ("eng", (o.eng, ep), v + 1)
        sem_names = []
        for e in ENGS:
            for ep in range(max(1, (cnt[e] + EPOCH - 1) // EPOCH)):
                sem_names.append(("eng", (e, ep)))
        for k in self.dma_keys:
            sem_names.append(("dma", k))
        with contextlib.ExitStack() as st:
            sems = {}
            for i, sn in enumerate(sem_names):
                sems[sn] = st.enter_context(nc.semaphore("s%d" % i))
            block = st.enter_context(nc.Block())
            prog = self

            def make(ename):
                def body(eng):
                    waited = {}
                    for o in prog.ops[ename]:
                        need = {}
                        for p in o.waits:
                            kind, k, v = p.sig
                            sn = (kind, k)
                            if need.get(sn, 0) < v:
                                need[sn] = v
                        for sn, v in need.items():
                            if waited.get(sn, 0) >= v:
                                continue
                            eng.wait_ge(sems[sn], v)
                            waited[sn] = v
                        ins = o.fn(eng)
                        if o.is_dma:
                            ins.then_inc(sems[("dma", o.key)], 16)
                        elif o.signal:
                            ins.then_inc(sems[(o.sig[0], o.sig[1])], 1)
                    if ename == "sp":
                        for k, tot in dcnt.items():
                            if tot > 0 and waited.get(("dma", k), 0) < tot:
                                eng.wait_ge(sems[("dma", k)], tot)
                return body

            block.tensor(make("pe"))
            block.scalar(make("act"))
            block.vector(make("dve"))
            block.gpsimd(make("pool"))
            block.sync(make("sp"))


C_ID, C_LI, C_US, C_LS, C_CE, C_CO, C_ONE = 0, 128, 256, 384, 512, 640, 768
NCF = 896
B_ID, B_LI, B_LS4, B_TRB, B_TR2, B_O1024, B_O256, B_O1, B_LI4 = 0, 128, 256, 768, 896, 1024, 1152, 1280, 1408
NCB = 1920

S_NORM = 0
S_FNORM = 96
S_GLAN = 104
S_CONV = 108
S_ALOG = 300
S_DTB = 316
S_GDNN = 332
S_WZ = 588
S_WAB = 844
S_WGK = 1100
S_BGK = 2124
NSM = 3148


def _consts():
    cf = np.zeros((128, NCF), np.float32)
    m = np.arange(128)
    same = (m[:, None] // 64) == (m[None, :] // 64)
    li = ((m[:, None] <= m[None, :]) & same).astype(np.float32)
    ls = ((m[:, None] < m[None, :]) & same).astype(np.float32)
    us = ((m[:, None] > m[None, :]) & same).astype(np.float32)
    cf[:, C_ID:C_ID + 128] = np.eye(128)
    cf[:, C_LI:C_LI + 128] = li
    cf[:, C_US:C_US + 128] = us
    cf[:, C_LS:C_LS + 128] = ls
    cf[:, C_CE:C_CE + 128] = (m[:, None] < 64).astype(np.float32) * np.ones((1, 128), np.float32)
    cf[:, C_CO:C_CO + 128] = (m[:, None] >= 64).astype(np.float32) * np.ones((1, 128), np.float32)
    cf[:, C_ONE:C_ONE + 128] = 1.0
    cb = np.zeros((128, NCB), np.float32)
    cb[:, B_ID:B_ID + 128] = np.eye(128)
    cb[:, B_LI:B_LI + 128] = li
    cb[:, B_LS4:B_LS4 + 512] = np.tile(ls, (1, 4))
    cb[:, B_TRB:B_TRB + 128] = -li / 16.0
    cb[:, B_TR2:B_TR2 + 128] = -us / 16.0
    cb[:, B_O1024:B_O1024 + 128] = 1.0 / 1024.0
    cb[:, B_O256:B_O256 + 128] = 1.0 / 256.0
    cb[:, B_O1:B_O1 + 128] = 1.0
    cb[:, B_LI4:B_LI4 + 512] = np.tile(li, (1, 4))
    return cf, cb


def _kblocks(W, col_lists):
    out = []
    for cols in col_lists:
        sub = W[:, cols]
        out.append(sub.reshape(8, 128, 512).transpose(1, 0, 2).reshape(128, 4096))
    return out


_SWAP = [False]


def _layer_is_gla(l):
    return (l % 2 == 0) != _SWAP[0]


def _prep_weights(inp, depth):
    wk = []
    ar = np.arange
    for l in range(depth):
        j = l // 2
        for which in range(2):
            if which == 1:
                pass
        def ffn_blocks(i):
            W = inp["ffn_w_in"][l, i]
            lists = [np.concatenate([ar(b * 256, (b + 1) * 256), DFF + ar(b * 256, (b + 1) * 256)]) for b in range(11)]
            wk.extend(_kblocks(W, lists))
            Wo = inp["ffn_w_out"][l, i]
            for half in range(2):
                for g in range(3):
                    nf = 8 if g < 2 else 6
                    sub = Wo[g * 1024:g * 1024 + nf * 128, half * 512:(half + 1) * 512]
                    blk = np.zeros((128, 8, 512), np.float32)
                    blk[:, 0:nf, :] = sub.reshape(nf, 128, 512).transpose(1, 0, 2)
                    wk.append(blk.reshape(128, 4096))
        ffn_blocks(0)
        if _layer_is_gla(l):
            W = inp["gla_w_in"][j]
            lists = [ar(512, 1024), ar(0, 512), ar(1024, 1536), ar(1536, 2048), ar(2048, 2560), ar(2560, 3072)]
            wk.extend(_kblocks(W, lists))
            wk.extend(_kblocks(inp["gla_w_out"][j], [ar(0, 512), ar(512, 1024)]))
        else:
            W = inp["gdn_w_in"][j]
            lists = [ar(b * 512, (b + 1) * 512) for b in range(8)]
            wk.extend(_kblocks(W, lists))
            wk.extend(_kblocks(inp["gdn_w_out"][j], [ar(0, 512), ar(512, 1024)]))
        ffn_blocks(1)
    sm = np.zeros((128, NSM), np.float32)
    sm[:, S_NORM:S_NORM + 96] = inp["norm_w"].reshape(12, 8, 128).transpose(2, 0, 1).reshape(128, 96)
    sm[:, S_FNORM:S_FNORM + 8] = inp["final_norm_w"].reshape(8, 128).T
    sm[:, S_GLAN:S_GLAN + 4] = inp["gla_norm_w"].reshape(2, 2, 128).transpose(2, 0, 1).reshape(128, 4)
    sm[:, S_CONV:S_CONV + 192] = inp["gdn_conv_w"].reshape(2, 4, 24, 128).transpose(3, 0, 1, 2).reshape(128, 192)
    sm[:, S_ALOG:S_ALOG + 16] = inp["gdn_a_log"].reshape(1, 16)
    sm[:, S_DTB:S_DTB + 16] = inp["gdn_dt_bias"].reshape(1, 16)
    sm[:, S_GDNN:S_GDNN + 256] = inp["gdn_norm_w"].reshape(1, 256)
    for j in range(2):
        wz = inp["gla_w_in"][j][:, 3072:3088]
        sm[:, S_WZ + j * 128:S_WZ + (j + 1) * 128] = wz.reshape(8, 128, 16).transpose(1, 0, 2).reshape(128, 128)
        wab = inp["gdn_w_in"][j][:, 4096:4112]
        sm[:, S_WAB + j * 128:S_WAB + (j + 1) * 128] = wab.reshape(8, 128, 16).transpose(1, 0, 2).reshape(128, 128)
        sm[0:16, S_WGK + j * 512:S_WGK + (j + 1) * 512] = inp["gla_w_gk"][j]
        sm[0:1, S_BGK + j * 512:S_BGK + (j + 1) * 512] = inp["gla_b_gk"][j][None, :]
    return np.ascontiguousarray(np.stack(wk)), sm


class Builder:
    def __init__(self, depth=4, nseq=2, ntile=8, stop_after=None):
        self.depth, self.nseq, self.ntile = depth, nseq, ntile
        self.stop_after = stop_after
        self.nblk_layer = [42 if _layer_is_gla(l) else 44 for l in range(depth)]
        self.NB = sum(self.nblk_layer)

    def mm(self, out, lhsT, rhs, start=True, stop=True, r=(), w=()):
        self.P.op("pe", lambda e: e.matmul(out, lhsT, rhs, start=start, stop=stop, skip_group_check=True), reads=r, writes=w)

    def tr(self, out, in_, ident, r=(), w=()):
        self.P.op("pe", lambda e: e.transpose(out, in_, ident), reads=r, writes=w)

    def act(self, out, in_, func, r=(), w=(), bias=None, scale=None, accum=None, eng="act"):
        kw = {}
        if bias is not None:
            kw["bias"] = bias
        if scale is not None:
            kw["scale"] = scale
        if accum is not None:
            kw["accum_out"] = accum
        self.P.op("act", lambda e: e.activation(out=out, in_=in_, func=func, **kw), reads=r, writes=w)

    def tt(self, eng, out, in0, in1, op, r=(), w=()):
        self.P.op(eng, lambda e: e.tensor_tensor(out=out, in0=in0, in1=in1, op=op), reads=r, writes=w)

    def ts(self, eng, out, in0, s1, s2, op0, op1=None, r=(), w=()):
        if op1 is None:
            self.P.op(eng, lambda e: e.tensor_scalar(out=out, in0=in0, scalar1=s1, scalar2=None, op0=op0), reads=r, writes=w)
        else:
            self.P.op(eng, lambda e: e.tensor_scalar(out=out, in0=in0, scalar1=s1, scalar2=s2, op0=op0, op1=op1), reads=r, writes=w)

    def stt(self, eng, out, in0, scalar, in1, op0, op1, r=(), w=()):
        self.P.op(eng, lambda e: e.scalar_tensor_tensor(out=out, in0=in0, scalar=scalar, in1=in1, op0=op0, op1=op1), reads=r, writes=w)

    def cp(self, eng, out, in_, r=(), w=()):
        if eng == "act":
            self.P.op("act", lambda e: e.activation(out=out, in_=in_, func=AF.Copy), reads=r, writes=w)
        else:
            self.P.op(eng, lambda e: e.tensor_copy(out=out, in_=in_), reads=r, writes=w)

    def memset(self, eng, ap, val, w=()):
        self.P.op(eng, lambda e: e.memset(ap, val), writes=w)

    def bank(self, pool=None):
        pool = pool or (0, 1, 2, 3, 4, 5, 6, 7)
        k = self._bctr.get(pool, 0)
        self._bctr[pool] = k + 1
        b = pool[k % len(pool)]
        return self.pbank[b], self.rbank[b]

    def load_wk(self):
        i = self.wk_i
        self.wk_i += 1
        slot = i % self.NWK
        blk = self.wk_next
        self.wk_next += 1
        src = self.wkb[blk]
        dst = self.wring[slot]
        layer_res = self.r_wkb[self.blk_layer[blk]]
        self.P.dma("sp", lambda e: e.dma_start(out=dst, in_=src), reads=[layer_res], writes=[self.r_wring[slot]], key="wk%d" % slot)
        return self.wring3[slot], self.r_wring[slot]

    def arena_switch(self):
        cell = self.cell
        self.P.op("dve", lambda e: e.memset(cell[:], 0.0), writes=[self.r_arena])

    def rmsnorm(self, wcol):
        xT, hT, sq = self.xT, self.hT, self.sq
        pb, rb = self.bank()
        for c in range(8):
            if c % 2 == 0:
                self.act(sq[:, c % 4, :], xT[:, c, :], AF.Square, r=[self.r_x], w=[self.r_sqs[c % 4]])
            else:
                self.tt("dve", sq[:, c % 4, :], xT[:, c, :], xT[:, c, :], ALU.mult, r=[self.r_x], w=[self.r_sqs[c % 4]])
            self.mm(pb, self.cb[:, B_O1024:B_O1024 + 128], sq[:, c % 4, :], start=(c == 0), stop=(c == 7), r=[self.r_sqs[c % 4], self.r_c], w=[rb])
        rstd = self.rstd
        self.act(rstd, pb, AF.Ln, r=[rb, self.r_c], w=[self.r_rstd], bias=self.epsb[:])
        self.act(rstd, rstd, AF.Exp, r=[self.r_rstd], w=[self.r_rstd], scale=-0.5)
        for c in range(8):
            self.stt("dve", hT[:, c, :], xT[:, c, :], self.sm[:, wcol + c:wcol + c + 1], rstd, ALU.mult, ALU.mult,
                     r=[self.r_x, self.r_rstd, self.r_c], w=[self.r_hs[c]])

    def ffn(self, l, i):
        self.rmsnorm(S_NORM + (l * 3 + 2 * i) * 8)
        self.arena_switch()
        A = self.arena
        actb = A[:, 0:22 * 512].rearrange("p (a b) -> p a b", a=22)
        ra = [self.r_arena]
        r_act = self.r_av[0]
        hT = self.hT
        gu = (4, 5, 6, 7)
        for jb in range(11):
            w, rw = self.load_wk()
            for half in range(2):
                fc = jb * 2 + half
                pg, rg = self.bank(gu)
                pu, ru = self.bank(gu)
                for kc in range(8):
                    self.mm(pg, w[:, kc, half * 128:(half + 1) * 128], hT[:, kc, :], start=(kc == 0), stop=(kc == 7), r=[rw, self.r_hs[kc]], w=[rg])
                for kc in range(8):
                    self.mm(pu, w[:, kc, 256 + half * 128:256 + (half + 1) * 128], hT[:, kc, :], start=(kc == 0), stop=(kc == 7), r=[rw, self.r_hs[kc]], w=[ru])
                sg, rsg = self.sgbuf[fc % 2], self.r_sg[fc % 2]
                self.act(sg, pg, AF.Silu, r=[rg], w=[rsg])
                self.tt("dve", actb[:, fc, :], sg, pu, ALU.mult, r=[rsg, ru] + ra, w=[r_act])
        for half in range(2):
            pool = (0, 1, 2, 3) if half == 0 else (4, 5, 6, 7)
            bks = [self.bank(pool) for _ in range(4)]
            for g in range(3):
                wo, rwo = self.load_wk()
                for f in range(8 if g < 2 else 6):
                    fc = g * 8 + f
                    for d in range(4):
                        self.mm(bks[d][0], wo[:, f, d * 128:(d + 1) * 128], actb[:, fc, :], start=(fc == 0), stop=(fc == 21),
                                r=[rwo, r_act] + ra, w=[bks[d][1]])
            for d in range(4):
                dc = half * 4 + d
                self.stt("dve", self.xT[:, dc, :], bks[d][0], 0.5, self.xT[:, dc, :], ALU.mult, ALU.add, r=[bks[d][1], self.r_x], w=[self.r_x])

    def gla(self, l, j, first_tile):
        self.rmsnorm(S_NORM + (l * 3 + 1) * 8)
        self.arena_switch()
        A, ra = self.arena, [self.r_arena]
        hT, cb, cf = self.hT, self.cb, self.cf
        o = 0

        def carve(n, shape3=None):
            nonlocal o
            v = A[:, o:o + n]
            o += n
            return v
        qd = carve(2048).rearrange("p (a b) -> p a b", a=4)
        ki = carve(2048).rearrange("p (a b) -> p a b", a=4)
        ke = carve(2048).rearrange("p (a b) -> p a b", a=4)
        vt = carve(4096).rearrange("p (a b) -> p a b", a=4)
        lg = carve(2048).rearrange("p (a b) -> p a b", a=4)
        rs = carve(4096).rearrange("p (a b) -> p a b", a=8)
        og = carve(4096).rearrange("p (a b) -> p a b", a=8)
        zT = carve(512)
        AT = carve(1024).rearrange("p (a b) -> p a b", a=2)
        r_qd, r_ki, r_ke, r_vt, r_lg, r_rs, r_og, r_zT, r_oT, r_eb = self.r_av[0:10]
        r_AT = self.r_av[10:12]
        oT = self.f32a[:, 0:4096].rearrange("p (a b) -> p a b", a=8)
        eb = self.f32a[:, 4096:6144].rearrange("p (a b) -> p a b", a=4)
        S, Sb = self.Sg[j], self.Sgb[j]
        rS, rSb = self.r_Sg[j], self.r_Sgb[j]
        sm = self.sm
        if first_tile:
            self.memset("pool", S[:], 0.0, w=[rS])
            self.memset("pool", Sb[:], 0.0, w=[rSb])
        pz, rz = self.bank()
        for kc in range(8):
            self.mm(pz[0:16, :], self.smb[:, j * 128 + kc * 16:j * 128 + kc * 16 + 16], hT[:, kc, :], start=(kc == 0), stop=(kc == 7),
                    r=[self.r_hs[kc], self.r_c], w=[rz])
        self.cp("act", zT[0:16, :], pz[0:16, :], r=[rz] + ra, w=[r_zT])
        for b in range(4):
            pl, rl = self.bank()
            self.mm(pl, zT[0:16, b * 128:(b + 1) * 128], self.wgkb[0:16, j * 512:(j + 1) * 512], start=True, stop=False, r=[r_zT, self.r_c] + ra, w=[rl])
            self.mm(pl, self.cb[0:1, B_O1:B_O1 + 128], self.bgkb[0:1, j * 512:(j + 1) * 512], start=False, stop=True, r=[self.r_c], w=[rl])
            t0, rt0 = self.tmpf[b % 2], self.r_tmpf[b % 2]
            self.act(t0, pl, AF.Exp, r=[rl], w=[rt0], scale=-1.0)
            self.act(lg[:, b, :], t0, AF.Ln, r=[rt0, self.r_c] + ra, w=[r_lg], bias=self.oneb[:])
        w, rw = self.load_wk()
        for b in range(4):
            pk, rk = self.bank()
            for kc in range(8):
                self.mm(pk, hT[:, kc, b * 128:(b + 1) * 128], w[:, kc, :], start=(kc == 0), stop=(kc == 7), r=[rw, self.r_hs[kc]], w=[rk])
            pd, rd = self.bank()
            self.mm(pd, cb[:, B_TR2:B_TR2 + 128], lg[:, b, :], r=[r_lg, self.r_c] + ra, w=[rd])
            t0, rt0 = self.tmpf[b % 2], self.r_tmpf[b % 2]
            self.act(t0, pd, AF.Exp, r=[rd], w=[rt0])
            self.tt("dve", ke[:, b, :], pk, t0, ALU.mult, r=[rk, rt0] + ra, w=[r_ke])
        for h in range(4):
            pk, rk = self.bank()
            for kc in range(8):
                self.mm(pk, w[:, kc, h * 128:(h + 1) * 128], hT[:, kc, :], start=(kc == 0), stop=(kc == 7), r=[rw, self.r_hs[kc]], w=[rk])
            pb, rb = self.bank()
            for b in range(4):
                self.mm(pb[:, b * 128:(b + 1) * 128], lg[:, b, h * 128:(h + 1) * 128], cb[:, B_TRB:B_TRB + 128], r=[r_lg, self.r_c] + ra, w=[rb])
            t0, rt0 = self.tmpf[h % 2], self.r_tmpf[h % 2]
            self.act(t0, pb, AF.Exp, r=[rb], w=[rt0], scale=-1.0)
            self.act(eb[:, h, :], pb, AF.Exp, r=[rb], w=[r_eb])
            self.tt("dve", ki[:, h, :], pk, t0, ALU.mult, r=[rk, rt0] + ra, w=[r_ki])
        w, rw = self.load_wk()
        for h in range(4):
            pq, rq = self.bank()
            for kc in range(8):
                self.mm(pq, w[:, kc, h * 128:(h + 1) * 128], hT[:, kc, :], start=(kc == 0), stop=(kc == 7), r=[rw, self.r_hs[kc]], w=[rq])
            self.stt("dve", qd[:, h, :], pq, 128.0 ** -0.5, eb[:, h, :], ALU.mult, ALU.mult, r=[rq, r_eb] + ra, w=[r_qd])
        for vb in range(2):
            w, rw = self.load_wk()
            for b in range(4):
                pv, rv = self.bank()
                for kc in range(8):
                    self.mm(pv, hT[:, kc, b * 128:(b + 1) * 128], w[:, kc, :], start=(kc == 0), stop=(kc == 7), r=[rw, self.r_hs[kc]], w=[rv])
                self.cp("act" if b % 2 == 0 else "dve", vt[:, b, vb * 512:(vb + 1) * 512], pv, r=[rv] + ra, w=[r_vt])
        for rb_ in range(2):
            w, rw = self.load_wk()
            for cc in range(4):
                pr, rr = self.bank()
                for kc in range(8):
                    self.mm(pr, w[:, kc, cc * 128:(cc + 1) * 128], hT[:, kc, :], start=(kc == 0), stop=(kc == 7), r=[rw, self.r_hs[kc]], w=[rr])
                self.act(rs[:, rb_ * 4 + cc, :], pr, AF.Silu, r=[rr] + ra, w=[r_rs])
        SP = (3, 4, 5, 6, 7)
        for b in range(4):
            bc = slice(b * 128, (b + 1) * 128)
            pA, rA = self.bank((0,))
            for h in range(4):
                self.mm(pA[:, h * 128:(h + 1) * 128], ki[:, h, bc], qd[:, h, bc], r=[r_ki, r_qd] + ra, w=[rA])
            at, rat = AT[:, b % 2, :], r_AT[b % 2]
            self.tt("dve", at, pA, cb[:, B_LI4:B_LI4 + 512], ALU.mult, r=[rA, self.r_c] + ra, w=[rat])
            po = [self.bank((1,)), self.bank((2,))]
            for pob, rob in po:
                self.memset("dve", pob, 0.0, w=[rob])
            for e in range(2):
                cs = slice(b * 128 + e * 64, b * 128 + (e + 1) * 64)
                last = b * 128 + e * 64 + 63
                for h in range(4):
                    pob, rob = po[h // 2]
                    for half in range(2):
                        c0 = (h % 2) * 256 + half * 128 + e * 64
                        self.mm(pob[:, c0:c0 + 64], Sb[:, h, half * 128:(half + 1) * 128], qd[:, h, cs], start=False, stop=False,
                                r=[rSb, r_qd] + ra, w=[rob])
                    pS, rpS = self.bank(SP)
                    self.mm(pS[:, 0:256], ke[e * 64:(e + 1) * 64, b, h * 128:(h + 1) * 128], vt[e * 64:(e + 1) * 64, b, h * 256:(h + 1) * 256],
                            r=[r_ke, r_vt] + ra, w=[rpS])
                    self.stt("dve", S[:, h, :], S[:, h, :], eb[:, h, last:last + 1], pS[:, 0:256], ALU.mult, ALU.add, r=[rS, r_eb, rpS], w=[rS])
                    self.cp("act", Sb[:, h, :], S[:, h, :], r=[rS], w=[rSb])
            for h in range(4):
                pob, rob = po[h // 2]
                for half in range(2):
                    c0 = (h % 2) * 256 + half * 128
                    self.mm(pob[:, c0:c0 + 128], vt[:, b, h * 256 + half * 128:h * 256 + (half + 1) * 128], at[:, h * 128:(h + 1) * 128],
                            start=False, stop=True, r=[r_vt, rat] + ra, w=[rob])
            for h2 in range(2):
                pob, rob = po[h2]
                self.cp("act", oT[:, h2 * 4:(h2 + 1) * 4, bc], pob.rearrange("p (a b) -> p a b", a=4), r=[rob], w=[r_oT])
        sq = self.sq
        for h in range(4):
            pn, rn = self.bank()
            for half in range(2):
                k4 = (h * 2 + half) % 4
                self.act(sq[:, k4, :], oT[:, h * 2 + half, :], AF.Square, r=[r_oT], w=[self.r_sqs[k4]])
                self.mm(pn, cb[:, B_O256:B_O256 + 128], sq[:, k4, :], start=(half == 0), stop=(half == 1), r=[self.r_sqs[k4], self.r_c], w=[rn])
            t0, rt0 = self.tmpf[h % 2], self.r_tmpf[h % 2]
            self.act(t0, pn, AF.Ln, r=[rn, self.r_c], w=[rt0], bias=self.epsb[:])
            self.act(t0, t0, AF.Exp, r=[rt0], w=[rt0], scale=-0.5)
            for half in range(2):
                c = h * 2 + half
                self.stt("dve", oT[:, c, :], oT[:, c, :], sm[:, S_GLAN + j * 2 + half:S_GLAN + j * 2 + half + 1], t0, ALU.mult, ALU.mult,
                         r=[r_oT, rt0, self.r_c], w=[r_oT])
                self.tt("dve", og[:, c, :], oT[:, c, :], rs[:, c, :], ALU.mult, r=[r_oT, r_rs] + ra, w=[r_og])
        self.out_proj(og, r_og, ra)

    def out_proj(self, og, r_og, ra):
        for blk in range(2):
            w, rw = self.load_wk()
            for dq in range(4):
                dc = blk * 4 + dq
                py, ry = self.bank()
                for vc in range(8):
                    self.mm(py, w[:, vc, dq * 128:(dq + 1) * 128], og[:, vc, :], start=(vc == 0), stop=(vc == 7), r=[rw, r_og] + ra, w=[ry])
                self.tt("dve", self.xT[:, dc, :], py, self.xT[:, dc, :], ALU.add, r=[ry, self.r_x], w=[self.r_x])

    def gdn(self, l, j, first_tile):
        self.rmsnorm(S_NORM + (l * 3 + 1) * 8)
        self.arena_switch()
        A, ra = self.arena, [self.r_arena]
        hT, cb, cf, sm = self.hT, self.cb, self.cf, self.sm
        o = 0

        def carve(n):
            nonlocal o
            v = A[:, o:o + n]
            o += n
            return v
        xc2 = carve(2 * 516).rearrange("p (a b) -> p a b", a=2)
        qkv = carve(24 * 512).rearrange("p (a b) -> p a b", a=24)
        og = carve(4096).rearrange("p (a b) -> p a b", a=8)
        ogt = carve(1024)
        ktok = carve(1024).rearrange("p (a b) -> p a b", a=8)
        kdtok = carve(1024).rearrange("p (a b) -> p a b", a=8)
        vtok = carve(1024).rearrange("p (a b) -> p a b", a=8)
        Pm = [carve(1024).rearrange("p (a b) -> p a b", a=8) for _ in range(2)]
        PmT = [carve(1024).rearrange("p (a b) -> p a b", a=8) for _ in range(2)]
        Rm = [carve(1024).rearrange("p (a b) -> p a b", a=8) for _ in range(2)]
        qkT = carve(1024).rearrange("p (a b) -> p a b", a=8)
        w0T = carve(1024).rearrange("p (a b) -> p a b", a=8)
        vnew = carve(1024).rearrange("p (a b) -> p a b", a=8)
        (r_xc0, r_qkv, r_og, r_ogt, r_kt, r_kd, r_vt, r_P0, r_P1, r_PT0, r_PT1, r_R0, r_R1, r_qk, r_w0, r_vn, r_rt, r_u0, r_sc, r_gu, r_dt) = self.r_av[0:21]
        r_P, r_PT, r_R = [r_P0, r_P1], [r_PT0, r_PT1], [r_R0, r_R1]
        r_sc = self.r_scs
        r_xcs = [r_xc0, self.r_av[21]]
        r_qc = self.r_qc
        f = self.f32a
        rtok = f[:, 0:1024]
        u0 = f[:, 1024:2048].rearrange("p (a b) -> p a b", a=8)
        gu = f[:, 2048:3072].rearrange("p (a b) -> p a b", a=8)
        dtm = f[:, 3072:4096].rearrange("p (a b) -> p a b", a=8)
        sc = f[:, 4096:4096 + 256]
        otok = f[:, 4608:5632].rearrange("p (a b) -> p a b", a=8)
        S, Sb = self.Sd[j], self.Sdb[j]
        rS, rSb = self.r_Sd[j], self.r_Sdb[j]
        ct, rct = self.ctail[j], self.r_ctail[j]
        if first_tile:
            self.memset("pool", S[:], 0.0, w=[rS])
            self.memset("pool", Sb[:], 0.0, w=[rSb])
            self.memset("pool", ct[:], 0.0, w=[rct])
        pab, rab = self.bank()
        for b in range(4):
            for kc in range(8):
                self.mm(pab[:, b * 16:(b + 1) * 16], hT[:, kc, b * 128:(b + 1) * 128], self.smb[:, 256 + j * 128 + kc * 16:256 + j * 128 + kc * 16 + 16],
                        start=(kc == 0), stop=(kc == 7), r=[self.r_hs[kc], self.r_c], w=[rab])
        pab3 = pab[:, 0:64].rearrange("p (b c) -> p b c", b=4)
        sc3 = sc.rearrange("p (b c) -> p b c", b=4)
        for b in range(4):
            self.tt("dve", sc3[:, b, 0:8], pab3[:, b, 0:8], sm[:, S_DTB + j * 8:S_DTB + j * 8 + 8], ALU.add, r=[rab, self.r_c], w=[r_sc])
        self.act(sc3[:, :, 0:8], sc3[:, :, 0:8], AF.Exp, r=[r_sc], w=[r_sc])
        self.act(sc3[:, :, 0:8], sc3[:, :, 0:8], AF.Ln, r=[r_sc, self.r_c], w=[r_sc], bias=self.oneb[:])
        for b in range(4):
            self.tt("dve", sc3[:, b, 8:16], sc3[:, b, 0:8], self.negA[:, j * 8:j * 8 + 8], ALU.mult, r=[r_sc, self.r_c], w=[r_sc])
        self.act(sc3[:, :, 16:24], pab3[:, :, 8:16], AF.Exp, r=[rab], w=[r_sc], scale=-1.0)
        self.act(sc3[:, :, 16:24], sc3[:, :, 16:24], AF.Ln, r=[r_sc, self.r_c], w=[r_sc], bias=self.oneb[:])
        self.act(sc3[:, :, 16:24], sc3[:, :, 16:24], AF.Exp, r=[r_sc], w=[r_sc], scale=-1.0)
        pdd, rdd = self.bank()
        for b in range(4):
            g_b = sc3[:, b, 8:16]
            self.mm(pdd[:, b * 16:b * 16 + 8], cf[:, C_LI:C_LI + 128], g_b, r=[r_sc, self.r_c], w=[rdd])
            self.mm(pdd[:, b * 16 + 8:b * 16 + 16], cf[:, C_US:C_US + 128], g_b, r=[r_sc, self.r_c], w=[rdd])
            self.mm(pdd[:, 64 + b * 16:64 + b * 16 + 8], cf[:, C_CE:C_CE + 128], g_b, r=[r_sc, self.r_c], w=[rdd])
            self.mm(pdd[:, 64 + b * 16 + 8:64 + b * 16 + 16], cf[:, C_CO:C_CO + 128], g_b, r=[r_sc, self.r_c], w=[rdd])
        pdd3 = pdd[:, 0:64].rearrange("p (b c) -> p b c", b=4)
        self.act(sc3[:, :, 40:56], pdd3, AF.Exp, r=[rdd], w=[r_sc])
        edl = self.edl
        self.act(edl, pdd[:, 64:128].rearrange("p (b c) -> p b c", b=4), AF.Exp, r=[rdd], w=[self.r_edl])
        t0s = [f[:, k * 512:(k + 1) * 512] for k in range(4)]
        t2s = [f[:, 2048 + k * 512:2048 + (k + 1) * 512] for k in range(4)]
        r_t0s, r_t2s = self.r_cv[0:4], self.r_cv[4:8]
        wcur = [None, None]
        pending, ready = [], []

        def stage_a(cc):
            if cc % 4 == 0:
                wcur[0], wcur[1] = self.load_wk()
            w, rw = wcur
            cq = cc % 4
            pp, rp = self.bank((0, 1, 2, 3))
            for kc in range(8):
                self.mm(pp, w[:, kc, cq * 128:(cq + 1) * 128], hT[:, kc, :], start=(kc == 0), stop=(kc == 7), r=[rw, self.r_hs[kc]], w=[rp])
            xc, r_xc = xc2[:, cc % 2, :], r_xcs[cc % 2]
            self.cp("pool", xc[:, 0:3], ct[:, cc, :], r=[rct] + ra, w=[r_xc])
            self.cp("act", xc[:, 3:515], pp, r=[rp] + ra, w=[r_xc])
            t0, rt0 = t0s[cc % 4], r_t0s[cc % 4]
            cw = S_CONV + (j * 4) * 24 + cc
            self.ts("dve", t0, xc[:, 3:515], sm[:, cw + 3 * 24:cw + 3 * 24 + 1], None, ALU.mult, r=[r_xc, self.r_c] + ra, w=[rt0])
            for tap in (2, 1, 0):
                self.stt("dve", t0, xc[:, tap:tap + 512], sm[:, cw + tap * 24:cw + tap * 24 + 1], t0, ALU.mult, ALU.add,
                         r=[r_xc, rt0, self.r_c] + ra, w=[rt0])
            self.cp("pool", ct[:, cc, :], xc[:, 512:515], r=[r_xc] + ra, w=[rct])

        def stage_b(cc):
            t0, rt0 = t0s[cc % 4], r_t0s[cc % 4]
            self.act(qkv[:, cc, :], t0, AF.Silu, r=[rt0] + ra, w=[r_qc[cc]])
            if cc < 16:
                k4 = cc % 4
                self.act(self.sq[:, k4, :], qkv[:, cc, :], AF.Square, r=[r_qc[cc]] + ra, w=[self.r_sqs[k4]])
                pn, rn = self.bank((4, 5, 6, 7))
                self.mm(pn, cb[:, B_O1:B_O1 + 128], self.sq[:, k4, :], r=[self.r_sqs[k4], self.r_c], w=[rn])
                pending.append((cc, pn, rn))

        def stage_c(items):
            for k, (cc, pn, rn) in enumerate(items):
                self.act(t2s[k], pn, AF.Ln, r=[rn, self.r_c], w=[r_t2s[k]], bias=self.epsb[:])
            for k, (cc, pn, rn) in enumerate(items):
                self.act(t2s[k], t2s[k], AF.Exp, r=[r_t2s[k]], w=[r_t2s[k]], scale=-0.5)
            for k, (cc, pn, rn) in enumerate(items):
                if cc < 8:
                    self.stt("dve", qkv[:, cc, :], qkv[:, cc, :], 128.0 ** -0.5, t2s[k], ALU.mult, ALU.mult, r=[r_qc[cc], r_t2s[k]] + ra, w=[r_qc[cc]])
                else:
                    self.tt("dve", qkv[:, cc, :], qkv[:, cc, :], t2s[k], ALU.mult, r=[r_qc[cc], r_t2s[k]] + ra, w=[r_qc[cc]])

        for it in range(25):
            if it < 24:
                stage_a(it)
            if ready:
                stage_c(ready)
                ready = []
            if it >= 1:
                stage_b(it - 1)
            if len(pending) == 4:
                ready, pending = pending, []
        if ready:
            stage_c(ready)
        if pending:
            stage_c(pending)
        self.dump("sc", sc, [r_sc], F32)
        self.dump("edl", edl, [self.r_edl], F32)
        self.dump("qkv", qkv, r_qc, BF16)
        wr = [self.load_wk(), self.load_wk()]
        idb = cb[:, B_ID:B_ID + 128]
        for b in range(4):
            bc = slice(b * 128, (b + 1) * 128)
            for rb_ in range(2):
                w, rw = wr[rb_]
                pr, rr = self.bank((6, 7))
                for kc in range(8):
                    self.mm(pr, hT[:, kc, bc], w[:, kc, :], start=(kc == 0), stop=(kc == 7), r=[rw, self.r_hs[kc]], w=[rr])
                self.act(rtok[:, rb_ * 512:(rb_ + 1) * 512], pr, AF.Silu, r=[rr], w=[r_rt])
                self.tt("dve", rtok[:, rb_ * 512:(rb_ + 1) * 512].rearrange("p (a b) -> p a b", a=4),
                        rtok[:, rb_ * 512:(rb_ + 1) * 512].rearrange("p (a b) -> p a b", a=4),
                        sm[:, S_GDNN + j * 128:S_GDNN + (j + 1) * 128].unsqueeze(1).broadcast_to([128, 4, 128]), ALU.mult,
                        r=[r_rt, self.r_c], w=[r_rt])
            for grp in range(2):
                hs = range(grp * 4, grp * 4 + 4)
                ptk, rtk = self.bank((6, 7))
                ptk_b = self.pbank_bf[self._last_bank_id((6, 7))]
                for h in hs:
                    self.tr(ptk_b[:, (h % 4) * 128:(h % 4 + 1) * 128], qkv[:, 8 + h, bc], idb, r=[r_qc[8 + h], self.r_c] + ra, w=[rtk])
                pk3 = ptk_b[:, 0:512].rearrange("p (a b) -> p a b", a=4)
                gq = slice(grp * 4, grp * 4 + 4)
                self.tt("dve", ktok[:, gq, :], pk3, sc3[:, b, 48 + grp * 4:52 + grp * 4].unsqueeze(2).broadcast_to([128, 4, 128]), ALU.mult,
                        r=[rtk, r_sc] + ra, w=[r_kt])
                self.tt("dve", kdtok[:, gq, :], pk3, sc3[:, b, 40 + grp * 4:44 + grp * 4].unsqueeze(2).broadcast_to([128, 4, 128]), ALU.mult,
                        r=[rtk, r_sc] + ra, w=[r_kd])
                ptv, rtv = self.bank((6, 7))
                ptv_b = self.pbank_bf[self._last_bank_id((6, 7))]
                for h in hs:
                    self.tr(ptv_b[:, (h % 4) * 128:(h % 4 + 1) * 128], qkv[:, 16 + h, bc], idb, r=[r_qc[16 + h], self.r_c] + ra, w=[rtv])
                self.cp("act", vtok[:, grp * 4:grp * 4 + 4, :], ptv_b[:, 0:512].rearrange("p (a b) -> p a b", a=4), r=[rtv] + ra, w=[r_vt])
            for h in range(8):
                self.ts("dve", gu[:, h, :], cf[:, C_US:C_US + 128], sc3[:, b, 8 + h:9 + h], None, ALU.mult, r=[self.r_c, r_sc], w=[r_gu])
            for grp in range(2):
                pD, rD = self.bank((4, 5))
                for h4 in range(4):
                    h = grp * 4 + h4
                    self.mm(pD[:, h4 * 128:(h4 + 1) * 128], gu[:, h, :], cf[:, C_LI:C_LI + 128], r=[r_gu, self.r_c], w=[rD])
                t0, rt0 = self.tmpf[grp], self.r_tmpf[grp]
                self.act(t0, pD, AF.Exp, r=[rD], w=[rt0])
                self.tt("dve", dtm[:, grp * 4:grp * 4 + 4, :], t0.rearrange("p (a b) -> p a b", a=4),
                        cb[:, B_LI4:B_LI4 + 512].rearrange("p (a b) -> p a b", a=4),
                        ALU.mult, r=[rt0, self.r_c], w=[r_dt])
            for grp in range(2):
                pX, rX = self.bank((4, 5))
                for h4 in range(4):
                    h = grp * 4 + h4
                    self.mm(pX[:, h4 * 128:(h4 + 1) * 128], qkv[:, 8 + h, bc], qkv[:, 8 + h, bc], r=[r_qc[8 + h]] + ra, w=[rX])
                for h4 in range(4):
                    h = grp * 4 + h4
                    self.stt("dve", self.xtmp[:, h4, :], pX[:, h4 * 128:(h4 + 1) * 128], sc3[:, b, 16 + h:17 + h], dtm[:, h, :], ALU.mult, ALU.mult,
                             r=[rX, r_sc, r_dt], w=[self.r_xtmp])
                self.stt("dve", Pm[0][:, grp * 4:grp * 4 + 4, :], self.xtmp[:, :, :], -1.0, cb[:, B_LS4:B_LS4 + 512].rearrange("p (a b) -> p a b", a=4),
                         ALU.mult, ALU.mult, r=[self.r_xtmp, self.r_c] + ra, w=[r_P[0]])
                pQ, rQ = self.bank((4, 5))
                for h4 in range(4):
                    h = grp * 4 + h4
                    self.mm(pQ[:, h4 * 128:(h4 + 1) * 128], qkv[:, 8 + h, bc], qkv[:, h, bc], r=[r_qc[8 + h], r_qc[h]] + ra, w=[rQ])
                self.tt("dve", qkT[:, grp * 4:grp * 4 + 4, :], pQ.rearrange("p (a b) -> p a b", a=4), dtm[:, grp * 4:grp * 4 + 4, :], ALU.mult,
                        r=[rQ, r_dt] + ra, w=[r_qk])
            self.dump("dtm", dtm, [r_dt], F32)
            self.dump("P0", Pm[0], [r_P[0]], BF16)
            self.dump("qkT", qkT, [r_qk], BF16)
            self.dump("ktok", ktok, [r_kt], BF16)
            self.dump("vtok", vtok, [r_vt], BF16)
            for grp in range(2):
                g4 = slice(grp * 4, grp * 4 + 4)
                pT, rT = self.bank((4, 5))
                pT_b = self.pbank_bf[self._last_bank_id((4, 5))]
                for h4 in range(4):
                    self.tr(pT_b[:, h4 * 128:(h4 + 1) * 128], Pm[0][:, grp * 4 + h4, :], idb, r=[r_P[0], self.r_c] + ra, w=[rT])
                self.cp("act", PmT[0][:, g4, :], pT_b[:, 0:512].rearrange("p (a b) -> p a b", a=4), r=[rT] + ra, w=[r_PT[0]])
                self.tt("pool", Rm[0][:, g4, :], Pm[0][:, g4, :], self.id4,
                        ALU.add, r=[r_P[0], self.r_c] + ra, w=[r_R[0]])
            for n in range(1, 6):
                cur, prv = n % 2, (n - 1) % 2
                for grp in range(2):
                    g4 = slice(grp * 4, grp * 4 + 4)
                    if n < 5:
                        pP, rP = self.bank((0, 1, 2, 3))
                        for h4 in range(4):
                            h = grp * 4 + h4
                            self.mm(pP[:, h4 * 128:(h4 + 1) * 128], PmT[prv][:, h, :], Pm[prv][:, h, :], r=[r_PT[prv], r_P[prv]] + ra, w=[rP])
                        self.cp("act", Pm[cur][:, g4, :], pP.rearrange("p (a b) -> p a b", a=4), r=[rP] + ra, w=[r_P[cur]])
                    pPT, rPT = self.bank((0, 1, 2, 3))
                    for h4 in range(4):
                        h = grp * 4 + h4
                        self.mm(pPT[:, h4 * 128:(h4 + 1) * 128], Pm[prv][:, h, :], PmT[prv][:, h, :], r=[r_PT[prv], r_P[prv]] + ra, w=[rPT])
                    self.cp("dve", PmT[cur][:, g4, :], pPT.rearrange("p (a b) -> p a b", a=4), r=[rPT] + ra, w=[r_PT[cur]])
                    pR, rR = self.bank((0, 1, 2, 3))
                    for h4 in range(4):
                        h = grp * 4 + h4
                        self.mm(pR[:, h4 * 128:(h4 + 1) * 128], PmT[cur][:, h, :], Rm[prv][:, h, :], start=True, stop=False, r=[r_PT[cur], r_R[prv]] + ra, w=[rR])
                        self.mm(pR[:, h4 * 128:(h4 + 1) * 128], idb, Rm[prv][:, h, :], start=False, stop=True, r=[self.r_c, r_R[prv]] + ra, w=[rR])
                    self.cp("act" if grp == 0 else "dve", Rm[cur][:, g4, :], pR.rearrange("p (a b) -> p a b", a=4), r=[rR] + ra, w=[r_R[cur]])
            Rf, r_Rf = Rm[1], r_R[1]
            self.dump("Rf", Rf, [r_Rf], BF16)
            for grp in range(2):
                g4 = slice(grp * 4, grp * 4 + 4)
                pU, rU = self.bank((0, 1, 2, 3))
                for h4 in range(4):
                    h = grp * 4 + h4
                    self.mm(pU[:, h4 * 128:(h4 + 1) * 128], Rf[:, h, :], vtok[:, h, :], r=[r_Rf, r_vt] + ra, w=[rU])
                self.cp("act", u0[:, g4, :], pU.rearrange("p (a b) -> p a b", a=4), r=[rU], w=[r_u0])
                pW, rW = self.bank((0, 1, 2, 3))
                for h4 in range(4):
                    h = grp * 4 + h4
                    self.mm(pW[:, h4 * 128:(h4 + 1) * 128], kdtok[:, h, :], Rf[:, h, :], r=[r_Rf, r_kd] + ra, w=[rW])
                self.cp("dve", w0T[:, g4, :], pW.rearrange("p (a b) -> p a b", a=4), r=[rW] + ra, w=[r_w0])
            self.dump("u0", u0, [r_u0], F32)
            self.dump("w0T", w0T, [r_w0], BF16)
            po = [self.bank((4,)), self.bank((5,))]
            for e in range(2):
                ps_ = slice(e * 64, (e + 1) * 64)
                cs = slice(b * 128 + e * 64, b * 128 + (e + 1) * 64)
                for grp in range(2):
                    pws, rws = self.bank((0, 1, 2, 3))
                    for h4 in range(4):
                        h = grp * 4 + h4
                        self.mm(pws[ps_, h4 * 128:(h4 + 1) * 128], w0T[:, h, e * 64:(e + 1) * 64], Sb[:, h, :], r=[r_w0, rSb] + ra, w=[rws])
                        self.mm(po[grp][0][ps_, h4 * 128:(h4 + 1) * 128], qkv[:, h, cs], Sb[:, h, :], r=[r_qc[h], rSb] + ra, w=[po[grp][1]])
                    g4 = slice(grp * 4, grp * 4 + 4)
                    self.tt("dve", self.vtmp[ps_, :, :], u0[ps_, g4, :], pws[ps_, :].rearrange("p (a b) -> p a b", a=4), ALU.subtract,
                            r=[r_u0, rws], w=[self.r_vtmp])
                    self.tt("dve", vnew[ps_, g4, :], self.vtmp[ps_, :, :],
                            sc3[ps_, b, 16 + grp * 4:20 + grp * 4].unsqueeze(2).broadcast_to([64, 4, 128]), ALU.mult,
                            r=[self.r_vtmp, r_sc] + ra, w=[r_vn])
                    pS, rpS = self.bank((6, 7))
                    for h4 in range(4):
                        h = grp * 4 + h4
                        self.mm(pS[:, h4 * 128:(h4 + 1) * 128], ktok[ps_, h, :], vnew[ps_, h, :], r=[r_kt, r_vn] + ra, w=[rpS])
                    self.tt("dve", S[:, g4, :], S[:, g4, :], edl[:, b, e * 8 + grp * 4:e * 8 + grp * 4 + 4].unsqueeze(2).broadcast_to([128, 4, 128]),
                            ALU.mult, r=[rS, self.r_edl], w=[rS])
                    self.tt("dve", S[:, g4, :], S[:, g4, :], pS.rearrange("p (a b) -> p a b", a=4), ALU.add, r=[rS, rpS], w=[rS])
                    self.cp("act", Sb[:, g4, :], S[:, g4, :], r=[rS], w=[rSb])
            for grp in range(2):
                g4 = slice(grp * 4, grp * 4 + 4)
                self.tt("dve", otok[:, g4, :], po[grp][0].rearrange("p (a b) -> p a b", a=4),
                        sc3[:, b, 40 + grp * 4:44 + grp * 4].unsqueeze(2).broadcast_to([128, 4, 128]), ALU.mult,
                        r=[po[grp][1], r_sc], w=[self.r_otok])
                pI, rI = self.bank((0, 1, 2, 3))
                for h4 in range(4):
                    h = grp * 4 + h4
                    self.mm(pI[:, h4 * 128:(h4 + 1) * 128], qkT[:, h, :], vnew[:, h, :], r=[r_qk, r_vn] + ra, w=[rI])
                self.tt("dve", otok[:, g4, :], otok[:, g4, :], pI.rearrange("p (a b) -> p a b", a=4), ALU.add, r=[self.r_otok, rI], w=[self.r_otok])
            self.dump("vnew", vnew, [r_vn], BF16)
            self.dump("otok", otok, [self.r_otok], F32)
            if b == 3:
                self.dump("otok3", otok, [self.r_otok], F32)
            ssq = self.ssq
            osq = f[:, 5632:6656].rearrange("p (a b) -> p a b", a=8)
            self.act(osq, otok, AF.Square, r=[self.r_otok], w=[self.r_junk])
            self.P.op("dve", lambda e, o_=ssq[:, 0:8], i_=osq: e.reduce_sum(out=o_, in_=i_, axis=mybir.AxisListType.X), reads=[self.r_junk], writes=[self.r_ssq])
            self.ts("dve", ssq[:, 0:8], ssq[:, 0:8], 1.0 / 128.0, None, ALU.mult, r=[self.r_ssq], w=[self.r_ssq])
            self.act(ssq[:, 0:8], ssq[:, 0:8], AF.Ln, r=[self.r_ssq, self.r_c], w=[self.r_ssq], bias=self.epsb[:])
            self.act(ssq[:, 0:8], ssq[:, 0:8], AF.Exp, r=[self.r_ssq], w=[self.r_ssq], scale=-0.5)
            self.tt("dve", otok, otok, ssq[:, 0:8].unsqueeze(2).broadcast_to([128, 8, 128]), ALU.mult, r=[self.r_otok, self.r_ssq], w=[self.r_otok])
            self.tt("dve", ogt[:, :], otok.rearrange("p a b -> p (a b)"), rtok, ALU.mult, r=[self.r_otok, r_rt] + ra, w=[r_ogt])
            self.dump("ogt", ogt, [r_ogt], BF16)
            self.dump("rtok", rtok, [r_rt], F32)
            self.dump("ssq", self.ssq, [self.r_ssq], F32)
            for grp in range(2):
                pG, rG = self.bank((0, 1, 2, 3))
                pG_b = self.pbank_bf[self._last_bank_id((0, 1, 2, 3))]
                for h4 in range(4):
                    c = grp * 4 + h4
                    self.tr(pG_b[:, h4 * 128:(h4 + 1) * 128], ogt[:, c * 128:(c + 1) * 128], idb, r=[r_ogt, self.r_c] + ra, w=[rG])
                self.cp("act", og[:, grp * 4:grp * 4 + 4, bc], pG_b[:, 0:512].rearrange("p (a b) -> p a b", a=4), r=[rG] + ra, w=[r_og])
        self.dump("og", og, [r_og], BF16)
        self.dump("Sd", S, [rS], F32)
        self.out_proj(og, r_og, ra)

    def dump(self, name, ap, reads, dt):
        if not getattr(self, "debug", False) or name in self._dumped:
            return
        self._dumped.add(name)
        d = self.nc.dram_tensor("dbg_" + name, list(ap.shape), dt, kind="ExternalOutput").ap()
        self.P.dma("sp", lambda e: e.dma_start(out=d, in_=ap), reads=reads, key="dbg_" + name)

    def _last_bank_id(self, pool):
        k = self._bctr[pool] - 1
        return pool[k % len(pool)]

    def build(self):
        nc = bass.Bass("TRN2", target_bir_lowering=False)
        self.nc = nc
        depth, nseq, ntile = self.depth, self.nseq, self.ntile
        ntok = nseq * ntile * T
        x_d = nc.dram_tensor("x", [ntok, D], F32, kind="ExternalInput").ap()
        wk_d = nc.dram_tensor("wk", [self.NB, 128, 4096], F32, kind="ExternalInput").ap()
        sm_d = nc.dram_tensor("sm", [128, NSM], F32, kind="ExternalInput").ap()
        cf_d = nc.dram_tensor("cf", [128, NCF], F32, kind="ExternalInput").ap()
        cb_d = nc.dram_tensor("cb", [128, NCB], F32, kind="ExternalInput").ap()
        y_d = nc.dram_tensor("y", [ntok, D], F32, kind="ExternalOutput").ap()
        self.wkb = nc.dram_tensor("wkb", [self.NB, 128, 4096], BF16, kind="Internal").ap()
        self.blk_layer = []
        for l in range(depth):
            nmix = self.nblk_layer[l] - 34
            self.blk_layer += [l * 3] * 17 + [l * 3 + 1] * nmix + [l * 3 + 2] * 17
        with contextlib.ExitStack() as st:
            def sb(name, shape, dt):
                return st.enter_context(nc.sbuf_tensor("s_" + name, shape, dt))
            P = self.P = Prog(nc)
            self._bctr = {}
            self._dumped = set()
            pall = st.enter_context(nc.psum_tensor("pall", [128, 8 * 512], F32))
            self.pbank = [pall[:, k * 512:(k + 1) * 512] for k in range(8)]
            self.rbank = [Res("bank%d" % k) for k in range(8)]
            self.pbank_bf = [pall[:, k * 512:(k + 1) * 512].bitcast(BF16) for k in range(8)]
            self.xT = sb("xT", [128, 8, T], F32)
            self.hT = sb("hT", [128, 8, T], BF16)
            self.sq = sb("sq", [128, 4, T], BF16)
            self.rstd = sb("rstd", [128, T], F32)[:]
            self.r_x, self.r_h, self.r_rstd = Res("x"), Res("h"), Res("rstd")
            self.r_hs = [Res("h%d" % k) for k in range(8)]
            self.r_sqs = [Res("sq%d" % k) for k in range(4)]
            self.xT, self.hT, self.sq = self.xT[:], self.hT[:], self.sq[:]
            self.tmpf = [sb("tmpf%d" % k, [128, T], F32)[:] for k in range(2)]
            self.r_tmpf = [Res("tmpf%d" % k) for k in range(2)]
            self.sgbuf = [sb("sg%d" % k, [128, T], F32)[:] for k in range(2)]
            self.r_sg = [Res("sg%d" % k) for k in range(2)]
            self.NWK = 4
            wring = [sb("wring%d" % k, [128, 4096], BF16) for k in range(self.NWK)]
            self.wring = [w_[:] for w_ in wring]
            self.wring3 = [w_[:].rearrange("p (a b) -> p a b", a=8) for w_ in wring]
            self.r_wring = [Res("wring%d" % k) for k in range(self.NWK)]
            self.wk_i = 0
            self.arena = sb("arena", [128, 2 * 516 + 24 * 512 + 4096 + 13 * 1024 + 64], BF16)[:]
            self.r_arena = Res("arena")
            self.r_av = [Res("av%d" % k, parent=self.r_arena) for k in range(24)]
            self.f32a = sb("f32a", [128, 6656], F32)[:]
            self.cell = sb("cell", [128, 2], F32)
            self.cf = sb("cf", [128, NCF], F32)[:]
            self.cb = sb("cb", [128, NCB], BF16)[:]
            self.sm = sb("sm", [128, S_WZ], F32)[:]
            self.smb = sb("smb", [128, 512], BF16)[:]
            self.wgkb = sb("wgkb", [16, 1024], BF16)[:]
            self.bgkb = sb("bgkb", [1, 1024], BF16)[:]
            self.epsb = sb("epsb", [128, 1], F32)
            self.oneb = sb("oneb", [128, 1], F32)
            self.negA = sb("negA", [128, 16], F32)[:]
            self.id4 = sb("id4", [128, 4, 128], BF16)[:]
            self.xtmp = sb("xtmp", [128, 4, 128], F32)[:]
            self.r_xtmp = Res("xtmp")
            self.vtmp = sb("vtmp", [128, 4, 128], F32)[:]
            self.r_vtmp = Res("vtmp")
            self.edl = sb("edl", [128, 4, 16], F32)[:]
            self.r_edl = Res("edl", strict=True)
            self.ssq = sb("ssq", [128, 8], F32)[:]
            self.r_ssq = Res("ssq", strict=True)
            self.r_qc = [Res("qc%d" % k, parent=self.r_arena) for k in range(24)]
            self.r_cv = [Res("cv%d" % k, parent=self.r_arena) for k in range(8)]
            self.r_scs = Res("scs", parent=self.r_arena, strict=True)
            self.r_junk = Res("junk")
            self.r_otok = Res("otok", parent=self.r_arena)
            self.r_c = Res("consts", strict=True)
            self.Sg = [sb("Sg%d" % k, [128, 4, 256], F32)[:] for k in range(2)]
            self.Sgb = [sb("Sgb%d" % k, [128, 4, 256], BF16)[:] for k in range(2)]
            self.Sd = [sb("Sd%d" % k, [128, 8, 128], F32)[:] for k in range(2)]
            self.Sdb = [sb("Sdb%d" % k, [128, 8, 128], BF16)[:] for k in range(2)]
            self.ctail = [sb("ct%d" % k, [128, 24, 3], BF16)[:] for k in range(2)]
            self.r_Sg = [Res("Sg%d" % k) for k in range(2)]
            self.r_Sgb = [Res("Sgb%d" % k) for k in range(2)]
            self.r_Sd = [Res("Sd%d" % k) for k in range(2)]
            self.r_Sdb = [Res("Sdb%d" % k) for k in range(2)]
            self.r_ctail = [Res("ct%d" % k) for k in range(2)]
            xin = [self.arena[:, k * 2048:(k + 1) * 2048].bitcast(F32) for k in range(2)]
            r_xin = [Res("xin%d" % k) for k in range(2)]
            rc = self.r_c
            P.dma("sp", lambda e: e.dma_start(out=self.cf, in_=cf_d), writes=[rc], key="c0")
            P.dma("sp", lambda e: e.dma_start(out=self.sm, in_=sm_d[:, 0:S_WZ]), writes=[rc], key="c0")
            P.dma("pool", lambda e: e.dma_start(out=self.cb, in_=cb_d), writes=[rc], key="c1")
            P.dma("pool", lambda e: e.dma_start(out=self.smb[:, 0:256], in_=sm_d[:, S_WZ:S_WZ + 256]), writes=[rc], key="c1")
            P.dma("pool", lambda e: e.dma_start(out=self.smb[:, 256:512], in_=sm_d[:, S_WAB:S_WAB + 256]), writes=[rc], key="c1")
            P.dma("pool", lambda e: e.dma_start(out=self.wgkb, in_=sm_d[0:16, S_WGK:S_WGK + 1024]), writes=[rc], key="c1")
            P.dma("pool", lambda e: e.dma_start(out=self.bgkb, in_=sm_d[0:1, S_BGK:S_BGK + 1024]), writes=[rc], key="c1")
            self.memset("dve", self.epsb[:], EPS, w=[rc])
            self.memset("dve", self.oneb[:], 1.0, w=[rc])
            for k in range(4):
                self.cp("dve", self.id4[:, k, :], self.cb[:, B_ID:B_ID + 128], r=[rc], w=[rc])
            self.act(self.negA, self.sm[:, S_ALOG:S_ALOG + 16], AF.Exp, r=[rc], w=[rc])
            self.ts("dve", self.negA, self.negA, -1.0, None, ALU.mult, r=[rc], w=[rc])
            self.r_wkb = [Res("wkb%d" % l) for l in range(3 * depth)]
            bi = 0
            for l in range(depth):
                for k in range(self.nblk_layer[l]):
                    src, dst = wk_d[bi], self.wkb[bi]
                    P.dma("pool", lambda e, s_=src, d_=dst: e.dma_start(out=d_, in_=s_), writes=[self.r_wkb[self.blk_layer[bi]]], key="cwk%d" % self.blk_layer[bi])
                    bi += 1
            idf = self.cf[:, C_ID:C_ID + 128]
            r_yout = [Res("yout%d" % k) for k in range(2)]
            for s in range(nseq):
                for t in range(ntile):
                    tok0 = (s * ntile + t) * T
                    self.wk_next = 0
                    self.arena_switch()
                    for b in range(4):
                        xi, rxi = xin[b % 2], r_xin[b % 2]
                        src = x_d[tok0 + b * 128:tok0 + (b + 1) * 128, :]
                        P.dma("sp", lambda e, s_=src, d_=xi: e.dma_start(out=d_, in_=s_), reads=[self.r_arena], writes=[rxi], key="xin%d" % (b % 2))
                        for half in range(2):
                            pt, rt = self.bank()
                            for c4 in range(4):
                                c = half * 4 + c4
                                self.tr(pt[:, c4 * 128:(c4 + 1) * 128], xi[:, c * 128:(c + 1) * 128], idf, r=[rxi, rc, self.r_arena], w=[rt])
                            self.cp("act" if half == 0 else "dve", self.xT[:, half * 4:half * 4 + 4, b * 128:(b + 1) * 128],
                                    pt.rearrange("p (a b) -> p a b", a=4), r=[rt], w=[self.r_x])
                    for l in range(depth):
                        j = l // 2
                        self.ffn(l, 0)
                        if self.stop_after == (l, 0):
                            break
                        if _layer_is_gla(l):
                            self.gla(l, j, t == 0)
                        else:
                            self.gdn(l, j, t == 0)
                        if self.stop_after == (l, 1):
                            break
                        self.ffn(l, 1)
                        if self.stop_after == (l, 2):
                            break
                    if self.stop_after is None:
                        self.rmsnorm_final()
                        src_T = self.f32a[:, 0:4096].rearrange("p (a b) -> p a b", a=8)
                        r_src = self.r_av[23]
                    else:
                        self.arena_switch()
                        src_T, r_src = self.xT, self.r_x
                    for b in range(4):
                        yo, ryo = xin[b % 2], r_xin[b % 2]
                        for half in range(2):
                            pt, rt = self.bank()
                            for c4 in range(4):
                                c = half * 4 + c4
                                self.tr(pt[:, c4 * 128:(c4 + 1) * 128], src_T[:, c, b * 128:(b + 1) * 128], idf, r=[r_src, rc], w=[rt])
                            self.cp("act" if half == 0 else "dve", yo[:, half * 512:(half + 1) * 512], pt, r=[rt, self.r_arena], w=[ryo])
                        dst = y_d[tok0 + b * 128:tok0 + (b + 1) * 128, :]
                        P.dma("sp", lambda e, s_=yo, d_=dst: e.dma_start(out=d_, in_=s_), reads=[ryo, self.r_arena], writes=[], key="xin%d" % (b % 2))
            P.emit()
        return nc

    def rmsnorm_final(self):
        xT, sq = self.xT, self.sq
        self.arena_switch()
        outT = self.f32a[:, 0:4096].rearrange("p (a b) -> p a b", a=8)
        r_out = self.r_av[23]
        pb, rb = self.bank()
        for c in range(8):
            if c % 2 == 0:
                self.act(sq[:, c % 4, :], xT[:, c, :], AF.Square, r=[self.r_x], w=[self.r_sqs[c % 4]])
            else:
                self.tt("dve", sq[:, c % 4, :], xT[:, c, :], xT[:, c, :], ALU.mult, r=[self.r_x], w=[self.r_sqs[c % 4]])
            self.mm(pb, self.cb[:, B_O1024:B_O1024 + 128], sq[:, c % 4, :], start=(c == 0), stop=(c == 7), r=[self.r_sqs[c % 4], self.r_c], w=[rb])
        rstd = self.rstd
        self.act(rstd, pb, AF.Ln, r=[rb, self.r_c], w=[self.r_rstd], bias=self.epsb[:])
        self.act(rstd, rstd, AF.Exp, r=[self.r_rstd], w=[self.r_rstd], scale=-0.5)
        for c in range(8):
            self.stt("dve", outT[:, c, :], xT[:, c, :], self.sm[:, S_FNORM + c:S_FNORM + c + 1], rstd, ALU.mult, ALU.mult,
                     r=[self.r_x, self.r_rstd, self.r_c, self.r_arena], w=[r_out])


_CACHE = {}


def kernel(**inputs):
    inp = {k: np.asarray(v) for k, v in inputs.items()}
    depth, ncore = 4, 8
    wk, sm = _prep_weights(inp, depth)
    cf, cb = _consts()
    x = np.ascontiguousarray(inp["x"], dtype=np.float32).reshape(ncore, 2 * SEQ, D)
    if "nc" not in _CACHE:
        _CACHE["nc"] = Builder(depth=depth, nseq=2, ntile=SEQ // T).build()
    nc = _CACHE["nc"]
    in_maps = [{"x": x[c], "wk": wk, "sm": sm, "cf": cf, "cb": cb} for c in range(ncore)]
    res = run_bass_kernel_spmd(nc, in_maps, core_ids=list(range(ncore)))
    y = np.stack([np.asarray(res.results[c]["y"]) for c in range(ncore)])
    return y.reshape(16, SEQ, D).astype(np.float32)
```
